# Optimizing a Trainium2 kernel written in Bass

```python
import math
import jax
import jax.numpy as jnp
from jax import lax
import numpy as np


D_MODEL = 4096
BATCH = 4
SEQ = 4096
DEPTH = 2

CTX_LEN = 256
GRID_W = 64
GROUP_W = D_MODEL // 4
MIX_WIDTH = 4 * GROUP_W
ROPE_DIM = 64
ROPE_BASE = 10000.0
Q_BLOCK = 128
NORM_EPS = 1e-6
DIFF_HEAD_DIM = 64
DIFF_HEADS = GROUP_W // (2 * DIFF_HEAD_DIM)
DIFF_QK = DIFF_HEADS * 2 * DIFF_HEAD_DIM
S5_CH = 16
S5_GROUPS = GROUP_W // S5_CH
S5_STATE = 64
MLA_NOPE = 128
MLA_ROPE = ROPE_DIM
MLA_V = 128
MLA_HEADS = GROUP_W // MLA_V
MLA_Q_RANK = 3 * D_MODEL // 16
MLA_KV_RANK = D_MODEL // 16
RET_K = ROPE_DIM
RET_V = 128
RET_HEADS = GROUP_W // RET_V
RET_QK = RET_HEADS * RET_K
RET_CHUNK = 128
MOE_GROUPS = 4
MOE_PER_GROUP = 4
MOE_EXPERTS = MOE_GROUPS * MOE_PER_GROUP
MOE_TOPK = 2
MOE_D_FF = D_MODEL // 4
IN_SPLITS = (DIFF_QK, DIFF_QK, GROUP_W, GROUP_W, MLA_Q_RANK, MLA_KV_RANK, MLA_ROPE,
             RET_QK, RET_QK, GROUP_W, GROUP_W)
IN_WIDTH = sum(IN_SPLITS)

kernel_name = "hybrid_headgroup_diffusion_trunk"


def rms_norm(x, g):
    xf = x.astype(jnp.float32)
    y = xf * lax.rsqrt(jnp.mean(xf * xf, axis=-1, keepdims=True) + NORM_EPS)
    return (y * g.astype(jnp.float32)).astype(x.dtype)


def modulate(h, shift, scale):
    return h * (1 + scale) + shift


def axial_rope_tables(n_lat):
    rows = n_lat // GRID_W
    row = jnp.repeat(jnp.arange(rows, dtype=jnp.float32), GRID_W)
    col = jnp.tile(jnp.arange(GRID_W, dtype=jnp.float32), rows)
    quarter = ROPE_DIM // 4
    inv = ROPE_BASE ** (-jnp.arange(quarter, dtype=jnp.float32) / quarter)
    ar = row[:, None] * inv
    ac = col[:, None] * inv
    ang = jnp.concatenate([ar, ar, ac, ac], axis=-1)
    return jnp.cos(ang), jnp.sin(ang)


def apply_rope(x, cos, sin):
    x1, x2, x3, x4 = jnp.split(x, 4, axis=-1)
    rot = jnp.concatenate([-x2, x1, -x4, x3], axis=-1)
    return x * cos[None, :, None, :].astype(x.dtype) + rot * sin[None, :, None, :].astype(x.dtype)


def sweep_query_blocks(fn, q):
    b, n = q.shape[:2]
    qb = jnp.moveaxis(q.reshape((b, n // Q_BLOCK, Q_BLOCK) + q.shape[2:]), 1, 0)
    out = jnp.moveaxis(lax.map(fn, qb), 0, 1)
    return out.reshape((b, n) + out.shape[3:])


def softmax_attend(q, k, v, scale):
    def block(qb):
        s = jnp.einsum('bqhd,bkhd->bhqk', qb, k, preferred_element_type=jnp.float32) * scale
        p = jax.nn.softmax(s, axis=-1)
        return jnp.einsum('bhqk,bkhe->bqhe', p.astype(v.dtype), v)
    return sweep_query_blocks(block, q)


def diff_attend(q, k, v, lam):
    b, _, h2, d = q.shape
    m = k.shape[1]
    scale = d ** -0.5
    def block(qb):
        s = jnp.einsum('bqhd,bkhd->bhqk', qb, k, preferred_element_type=jnp.float32) * scale
        p = jax.nn.softmax(s, axis=-1).reshape(b, h2 // 2, 2, qb.shape[1], m)
        p = p[:, :, 0] - lam * p[:, :, 1]
        return jnp.einsum('bhqk,bkhe->bqhe', p.astype(v.dtype), v)
    return sweep_query_blocks(block, q)


def diff_branch(q, k, v, q_c, k_c, v_c, cos, sin, lam_vec, subln, lam_init, need_ctx):
    hd = lambda t, nh: t.reshape(t.shape[0], t.shape[1], nh, -1)
    lv = lam_vec.astype(jnp.float32)
    lam = jnp.exp(jnp.sum(lv[0] * lv[1])) - jnp.exp(jnp.sum(lv[2] * lv[3])) + lam_init
    q = apply_rope(hd(q, 2 * DIFF_HEADS), cos, sin)
    k = apply_rope(hd(k, 2 * DIFF_HEADS), cos, sin)
    v = hd(v, DIFF_HEADS)
    q_c, k_c, v_c = hd(q_c, 2 * DIFF_HEADS), hd(k_c, 2 * DIFF_HEADS), hd(v_c, DIFF_HEADS)
    out = diff_attend(q, jnp.concatenate([k, k_c], axis=1), jnp.concatenate([v, v_c], axis=1), lam)
    finish = lambda o: (rms_norm(o, subln) * (1.0 - lam_init)).reshape(o.shape[0], o.shape[1], GROUP_W)
    out_c = finish(diff_attend(q_c, k_c, v_c, lam)) if need_ctx else None
    return finish(out), out_c


def _ssm_combine(e1, e2):
    a1, b1 = e1
    a2, b2 = e2
    return a1 * a2, a2 * b1 + b2


def s5_discretize(a_re, a_im, log_dt, b_re, b_im):
    a = lax.complex(a_re.astype(jnp.float32), a_im.astype(jnp.float32))
    dt = jnp.exp(log_dt.astype(jnp.float32))[:, None]
    abar = jnp.exp(a * dt)
    bmat = lax.complex(b_re.astype(jnp.float32), b_im.astype(jnp.float32))
    bbar = ((abar - 1.0) / a)[:, :, None] * bmat
    return abar, bbar


def s5_scan(u, abar, bbar, h0):
    bu = jnp.einsum('gpc,bngc->bngp', bbar, u.astype(jnp.complex64))
    if h0 is not None:
        bu = bu.at[:, 0].add(abar * h0)
    a = jnp.broadcast_to(abar, bu.shape)
    _, xs = lax.associative_scan(_ssm_combine, (a, bu), axis=1)
    return xs


def s5_readout(cmat, xs):
    return jnp.real(jnp.einsum('gcp,bngp->bngc', cmat, xs))


def s5_branch(u, u_c, a_re, a_im, log_dt, b_re, b_im, c_re, c_im, d_skip, glu_w, glu_b, need_ctx):
    b, n, _ = u.shape
    lc = u_c.shape[1]
    ug = u.astype(jnp.float32).reshape(b, n, S5_GROUPS, S5_CH)
    ucg = u_c.astype(jnp.float32).reshape(b, lc, S5_GROUPS, S5_CH)
    dsk = d_skip.astype(jnp.float32)
    y = dsk * ug
    yc = dsk * ucg if need_ctx else None
    for dr in range(2):
        flip = (lambda t: jnp.flip(t, 1)) if dr == 1 else (lambda t: t)
        abar, bbar = s5_discretize(a_re[dr], a_im[dr], log_dt[dr], b_re[dr], b_im[dr])
        cmat = lax.complex(c_re[dr].astype(jnp.float32), c_im[dr].astype(jnp.float32))
        xs_c = s5_scan(flip(ucg), abar, bbar, None)
        xs = s5_scan(flip(ug), abar, bbar, xs_c[:, -1])
        y = y + flip(s5_readout(cmat, xs))
        if need_ctx:
            yc = yc + flip(s5_readout(cmat, xs_c))
    def glu(t):
        g = jax.nn.gelu(t.reshape(t.shape[0], t.shape[1], GROUP_W))
        gate = jax.nn.sigmoid(g @ glu_w.astype(jnp.float32) + glu_b.astype(jnp.float32))
        return (g * gate).astype(u.dtype)
    return glu(y), (glu(yc) if need_ctx else None)


def mla_branch(cq, ckv, kr, cq_c, ckv_c, kr_c, cos, sin, q_norm, kv_norm, w_uq, w_ukv, need_ctx):
    def qkv(cq, ckv, kr, rotate):
        b, n, _ = cq.shape
        q = (rms_norm(cq, q_norm) @ w_uq).reshape(b, n, MLA_HEADS, MLA_NOPE + MLA_ROPE)
        kv = (rms_norm(ckv, kv_norm) @ w_ukv).reshape(b, n, MLA_HEADS, MLA_NOPE + MLA_V)
        q_nope, q_rope = q[..., :MLA_NOPE], q[..., MLA_NOPE:]
        k_nope, v = kv[..., :MLA_NOPE], kv[..., MLA_NOPE:]
        k_rope = kr[:, :, None, :]
        if rotate:
            q_rope = apply_rope(q_rope, cos, sin)
            k_rope = apply_rope(k_rope, cos, sin)
        k = jnp.concatenate([k_nope, jnp.broadcast_to(k_rope, (b, n, MLA_HEADS, MLA_ROPE))], axis=-1)
        return jnp.concatenate([q_nope, q_rope], axis=-1), k, v
    q, k, v = qkv(cq, ckv, kr, True)
    q_c, k_c, v_c = qkv(cq_c, ckv_c, kr_c, False)
    scale = (MLA_NOPE + MLA_ROPE) ** -0.5
    out = softmax_attend(q, jnp.concatenate([k, k_c], axis=1), jnp.concatenate([v, v_c], axis=1), scale)
    out = out.reshape(out.shape[0], out.shape[1], GROUP_W)
    out_c = None
    if need_ctx:
        out_c = softmax_attend(q_c, k_c, v_c, scale)
        out_c = out_c.reshape(out_c.shape[0], out_c.shape[1], GROUP_W)
    return out, out_c


def retention_chunked(q, k, v, log_g, s0, strict):
    b, h, n, dk = q.shape
    dv = v.shape[-1]
    cs = RET_CHUNK
    nc = n // cs
    pos = jnp.arange(cs, dtype=jnp.float32)
    diff = pos[:, None] - pos[None, :]
    keep = diff > 0 if strict else diff >= 0
    dmat = jnp.where(keep[None], jnp.exp(log_g[:, None, None] * jnp.maximum(diff, 0.0)[None]), 0.0)
    qc = q.reshape(b, h, nc, cs, dk)
    kc = k.reshape(b, h, nc, cs, dk)
    vc = v.reshape(b, h, nc, cs, dv)
    scores = jnp.einsum('bhncd,bhnmd->bhncm', qc, kc) * dmat[None, :, None]
    intra = jnp.einsum('bhncm,bhnme->bhnce', scores, vc)
    k_w = kc * jnp.exp(log_g[:, None] * (cs - 1 - pos))[None, :, None, :, None]
    u = jnp.einsum('bhncd,bhnce->nbhde', k_w, vc)
    chunk_decay = jnp.exp(log_g * cs)[None, :, None, None]
    def step(s, u_i):
        return chunk_decay * s + u_i, s
    s_final, s_prev = lax.scan(step, s0, u)
    q_w = qc * jnp.exp(log_g[:, None] * (pos + 1))[None, :, None, :, None]
    cross = jnp.einsum('bhncd,nbhde->bhnce', q_w, s_prev)
    return (intra + cross).reshape(b, h, n, dv), s_final


def retention_branch(q, k, v, g, q_c, k_c, v_c, g_c, cos, sin, decay_raw, norm_g, need_ctx):
    b = q.shape[0]
    log_g = jax.nn.log_sigmoid(decay_raw.astype(jnp.float32))
    bhnd = lambda t: jnp.transpose(t, (0, 2, 1, 3))
    flip = lambda t: jnp.flip(t, 2)
    hd = lambda t, dh: t.reshape(t.shape[0], t.shape[1], RET_HEADS, dh)
    ksc = RET_K ** -0.5
    q = bhnd(apply_rope(hd(q, RET_K), cos, sin))
    k = bhnd(apply_rope(hd(k, RET_K), cos, sin) * ksc)
    v = bhnd(hd(v, RET_V))
    q_c, k_c, v_c = bhnd(hd(q_c, RET_K)), bhnd(hd(k_c, RET_K) * ksc), bhnd(hd(v_c, RET_V))
    s0 = jnp.zeros((b, RET_HEADS, RET_K, RET_V), jnp.float32)
    yc_f, sc_f = retention_chunked(q_c, k_c, v_c, log_g[0], s0, False)
    yc_b, sc_b = retention_chunked(flip(q_c), flip(k_c), flip(v_c), log_g[1], s0, True)
    y_f, _ = retention_chunked(q, k, v, log_g[0], sc_f, False)
    y_b, _ = retention_chunked(flip(q), flip(k), flip(v), log_g[1], sc_b, True)
    def finish(y, gate):
        y = rms_norm(jnp.transpose(y, (0, 2, 1, 3)), norm_g.reshape(RET_HEADS, RET_V))
        return (jax.nn.silu(gate) * y.reshape(y.shape[0], y.shape[1], GROUP_W)).astype(gate.dtype)
    out = finish(y_f + flip(y_b), g)
    out_c = finish(yc_f + flip(yc_b), g_c) if need_ctx else None
    return out, out_c


def token_mixers(h, hc, cos, sin, need_ctx, lam_init, w_in, w_out, diff_lambda, diff_subln,
                 s5_a_re, s5_a_im, s5_log_dt, s5_b_re, s5_b_im, s5_c_re, s5_c_im, s5_d, s5_glu_w, s5_glu_b,
                 mla_q_norm, mla_kv_norm, mla_w_uq, mla_w_ukv, ret_decay, ret_norm):
    pts = np.cumsum(IN_SPLITS)[:-1].tolist()
    dq, dk, dv, su, mcq, mckv, mkr, rq, rk, rv, rg = jnp.split(h @ w_in, pts, axis=-1)
    dq_c, dk_c, dv_c, su_c, mcq_c, mckv_c, mkr_c, rq_c, rk_c, rv_c, rg_c = jnp.split(hc @ w_in, pts, axis=-1)
    a, a_c = diff_branch(dq, dk, dv, dq_c, dk_c, dv_c, cos, sin, diff_lambda, diff_subln, lam_init, need_ctx)
    s, s_c = s5_branch(su, su_c, s5_a_re, s5_a_im, s5_log_dt, s5_b_re, s5_b_im, s5_c_re, s5_c_im,
                       s5_d, s5_glu_w, s5_glu_b, need_ctx)
    m, m_c = mla_branch(mcq, mckv, mkr, mcq_c, mckv_c, mkr_c, cos, sin, mla_q_norm, mla_kv_norm,
                        mla_w_uq, mla_w_ukv, need_ctx)
    r, r_c = retention_branch(rq, rk, rv, rg, rq_c, rk_c, rv_c, rg_c, cos, sin, ret_decay, ret_norm, need_ctx)
    y = jnp.concatenate([a, s, m, r], axis=-1) @ w_out
    y_c = jnp.concatenate([a_c, s_c, m_c, r_c], axis=-1) @ w_out if need_ctx else None
    return y, y_c


def hier_moe(h, wg, bg, we, be, w_gate, w_up, w_down):
    shp = h.shape
    t = h.reshape(-1, shp[-1])
    g_prob = jax.nn.softmax((t @ wg + bg).astype(jnp.float32), axis=-1)
    g_p, g_idx = lax.top_k(g_prob, 1)
    e_logits = (t @ we + be).astype(jnp.float32).reshape(-1, MOE_GROUPS, MOE_PER_GROUP)
    e_logits = e_logits[jnp.arange(t.shape[0]), g_idx[:, 0]]
    e_p, e_idx = lax.top_k(jax.nn.softmax(e_logits, axis=-1), MOE_TOPK)
    w = g_p * e_p / jnp.sum(e_p, axis=-1, keepdims=True)
    ids = g_idx * MOE_PER_GROUP + e_idx
    dense_w = jnp.sum(jax.nn.one_hot(ids, MOE_EXPERTS, dtype=jnp.float32) * w[..., None], axis=1)
    dense_w = dense_w.astype(t.dtype)
    out = jnp.zeros_like(t)
    for e in range(MOE_EXPERTS):
        hid = jax.nn.silu(t @ w_gate[e]) * (t @ w_up[e])
        out = out + dense_w[:, e:e + 1] * (hid @ w_down[e])
    return out.reshape(shp)


def setup_inputs(seed: int = 0) -> dict:
    key = jax.random.key(seed)
    ks = list(jax.random.split(key, 40))
    f32 = jnp.float32
    L, D = DEPTH, D_MODEL
    G, P, CH, H = S5_GROUPS, S5_STATE, S5_CH, RET_HEADS

    def nrm(shape, scale=1.0):
        return scale * jax.random.normal(ks.pop(), shape, f32)

    def gain(shape):
        return 1.0 + nrm(shape, 0.1)

    s5_a_im = jnp.pi * jnp.arange(P, dtype=f32) + nrm((L, 2, G, P), 0.01)
    s5_log_dt = jax.random.uniform(ks.pop(), (L, 2, G), f32, math.log(1e-3), math.log(1e-1))
    ret_decay = jnp.log(2.0 ** (5.0 + jnp.arange(H, dtype=f32)) - 1.0) + nrm((L, 2, H), 0.05)
    return {
        "x": nrm((BATCH, SEQ, D)),
        "c": nrm((BATCH, D)),
        "ctx": nrm((BATCH, CTX_LEN, D)),
        "c_ctx": nrm((D,)),
        "ada_w": nrm((L, D, 6 * D), 0.5 * D ** -0.5),
        "ada_b": nrm((L, 6 * D), 0.01),
        "norm_mix": gain((L, D)),
        "norm_ffn": gain((L, D)),
        "w_in": nrm((L, D, IN_WIDTH), D ** -0.5),
        "w_out": nrm((L, MIX_WIDTH, D), MIX_WIDTH ** -0.5),
        "diff_lambda": nrm((L, 4, DIFF_HEAD_DIM), 0.1),
        "diff_subln": gain((L, 2 * DIFF_HEAD_DIM)),
        "s5_a_re": -0.5 + nrm((L, 2, G, P), 0.01),
        "s5_a_im": s5_a_im,
        "s5_log_dt": s5_log_dt,
        "s5_b_re": nrm((L, 2, G, P, CH), (2 * CH) ** -0.5),
        "s5_b_im": nrm((L, 2, G, P, CH), (2 * CH) ** -0.5),
        "s5_c_re": nrm((L, 2, G, CH, P), P ** -0.5),
        "s5_c_im": nrm((L, 2, G, CH, P), P ** -0.5),
        "s5_d": nrm((L, G, CH)),
        "s5_glu_w": nrm((L, GROUP_W, GROUP_W), GROUP_W ** -0.5),
        "s5_glu_b": nrm((L, GROUP_W), 0.01),
        "mla_q_norm": gain((L, MLA_Q_RANK)),
        "mla_kv_norm": gain((L, MLA_KV_RANK)),
        "mla_w_uq": nrm((L, MLA_Q_RANK, MLA_HEADS * (MLA_NOPE + MLA_ROPE)), MLA_Q_RANK ** -0.5),
        "mla_w_ukv": nrm((L, MLA_KV_RANK, MLA_HEADS * (MLA_NOPE + MLA_V)), MLA_KV_RANK ** -0.5),
        "ret_decay": ret_decay,
        "ret_norm": gain((L, GROUP_W)),
        "moe_wg": nrm((L, D, MOE_GROUPS), D ** -0.5),
        "moe_bg": nrm((L, MOE_GROUPS), 0.01),
        "moe_we": nrm((L, D, MOE_EXPERTS), D ** -0.5),
        "moe_be": nrm((L, MOE_EXPERTS), 0.01),
        "moe_w_gate": nrm((L, MOE_EXPERTS, D, MOE_D_FF), D ** -0.5),
        "moe_w_up": nrm((L, MOE_EXPERTS, D, MOE_D_FF), D ** -0.5),
        "moe_w_down": nrm((L, MOE_EXPERTS, MOE_D_FF, D), MOE_D_FF ** -0.5),
        "final_norm": gain((D,)),
    }


def reference(x, c, ctx, c_ctx, ada_w, ada_b, norm_mix, norm_ffn, w_in, w_out, diff_lambda, diff_subln,
              s5_a_re, s5_a_im, s5_log_dt, s5_b_re, s5_b_im, s5_c_re, s5_c_im, s5_d, s5_glu_w, s5_glu_b,
              mla_q_norm, mla_kv_norm, mla_w_uq, mla_w_ukv, ret_decay, ret_norm,
              moe_wg, moe_bg, moe_we, moe_be, moe_w_gate, moe_w_up, moe_w_down, final_norm):
    cos, sin = axial_rope_tables(x.shape[1])
    xc = ctx
    sc = jax.nn.silu(c)
    scc = jax.nn.silu(c_ctx)
    for l in range(DEPTH):
        need_ctx = l < DEPTH - 1
        lam_init = 0.8 - 0.6 * math.exp(-0.3 * l)
        mod = jnp.split((sc @ ada_w[l] + ada_b[l])[:, None, :], 6, axis=-1)
        mod_c = jnp.split(scc @ ada_w[l] + ada_b[l], 6, axis=-1)
        h = modulate(rms_norm(x, norm_mix[l]), mod[0], mod[1])
        hc = modulate(rms_norm(xc, norm_mix[l]), mod_c[0], mod_c[1])
        y, y_c = token_mixers(h, hc, cos, sin, need_ctx, lam_init, w_in[l], w_out[l], diff_lambda[l],
                              diff_subln[l], s5_a_re[l], s5_a_im[l], s5_log_dt[l], s5_b_re[l], s5_b_im[l],
                              s5_c_re[l], s5_c_im[l], s5_d[l], s5_glu_w[l], s5_glu_b[l],
                              mla_q_norm[l], mla_kv_norm[l], mla_w_uq[l], mla_w_ukv[l],
                              ret_decay[l], ret_norm[l])
        x = x + mod[2] * y
        h = modulate(rms_norm(x, norm_ffn[l]), mod[3], mod[4])
        x = x + mod[5] * hier_moe(h, moe_wg[l], moe_bg[l], moe_we[l], moe_be[l],
                                  moe_w_gate[l], moe_w_up[l], moe_w_down[l])
        if need_ctx:
            xc = xc + mod_c[2] * y_c
            hc = modulate(rms_norm(xc, norm_ffn[l]), mod_c[3], mod_c[4])
            xc = xc + mod_c[5] * hier_moe(hc, moe_wg[l], moe_bg[l], moe_we[l], moe_be[l],
                                          moe_w_gate[l], moe_w_up[l], moe_w_down[l])
    return rms_norm(x, final_norm)
```

```python
import math
import os
from contextlib import ExitStack
import numpy as np
import concourse.bass as bass
import concourse.mybir as mybir
from concourse.bass_utils import run_bass_kernel_spmd

F32 = mybir.dt.float32
BF16 = mybir.dt.bfloat16
AF = mybir.ActivationFunctionType
ALU = mybir.AluOpType
AX = mybir.AxisListType
NORM_EPS = 1e-6


class Cfg:
    def __init__(s, D=4096, N=4096, LC=256, DEPTH=2, B=4):
        s.D, s.N, s.LC, s.DEPTH, s.B = D, N, LC, DEPTH, B
        s.T = N + LC
        s.GW = D // 4
        s.DH = 64
        s.DIFF_HEADS = s.GW // 128
        s.S5_CH, s.S5_P = 16, 64
        s.S5_G = s.GW // 16
        s.MLA_HEADS = s.GW // 128
        s.QR = 3 * D // 16
        s.KVR = D // 16
        s.RET_HEADS = s.GW // 128
        s.RQK = s.RET_HEADS * 64
        s.E, s.FF = 16, D // 4
        s.splits = [s.GW, s.GW, s.GW, s.GW, s.QR, s.KVR, 64, s.RQK, s.RQK, s.GW, s.GW]
        s.names = ["dq", "dk", "dv", "su", "cq", "ckv", "kr", "rq", "rk", "rv", "rg"]
        s.off = {}
        o = 0
        for n, w in zip(s.names, s.splits):
            s.off[n] = (o, w)
            o += w
        s.INW = o
        s.KC = D // 128
        s.TB = 512
        s.blocks = [(i * 512, 512, False) for i in range(N // 512)]
        c0 = N
        while c0 < s.T:
            w = min(512, s.T - c0)
            s.blocks.append((c0, w, True))
            c0 += w
        s.NTB = len(s.blocks)
        s.KT = s.T // 128


class Buf:
    __slots__ = ("t", "w", "r", "name")

    def __init__(s, t, name=""):
        s.t, s.w, s.r, s.name = t, None, {}, name

    def __getitem__(s, idx):
        return V(s.t[idx], (s,))

    def sub(s):
        return Buf(s.t, s.name)


class V:
    __slots__ = ("ap", "bufs")

    def __init__(s, ap, bufs):
        s.ap, s.bufs = ap, bufs

    def __getitem__(s, idx):
        return V(s.ap[idx], s.bufs)

    def rearrange(s, pat, **kw):
        return V(s.ap.rearrange(pat, **kw), s.bufs)


class K:
    ND = 12

    def __init__(s, nc, st):
        s.nc, s.st = nc, st
        s.E = {"pe": nc.tensor, "act": nc.scalar, "dve": nc.vector, "pool": nc.gpsimd, "sp": nc.sync}
        s.esem = {e: st.enter_context(nc.semaphore("es_" + e)) for e in ("pe", "act", "dve", "pool")}
        s.cnt = {e: 0 for e in s.esem}
        s.known = {e: {} for e in s.E}
        s.dsl = {q: [[st.enter_context(nc.semaphore("ds_%s%d" % (q, i))), 0] for i in range(s.ND)] for q in ("sp", "pool", "act")}
        s.dnext = {"sp": 0, "pool": 0, "act": 0}
        s.nbuf = 0
        s.psb = None
        s.psi = 0
        s.held = []

    def sb(s, shape, dt=F32, name=None, stack=None):
        s.nbuf += 1
        nm = "%s_%d" % (name or "b", s.nbuf)
        t = (stack or s.st).enter_context(s.nc.sbuf_tensor(nm, list(shape), dt))
        return Buf(t, nm)

    def dram(s, shape, dt=F32, name=None):
        s.nbuf += 1
        nm = "%s_%d" % (name or "d", s.nbuf)
        return Buf(s.nc.dram_tensor(nm, list(shape), dt), nm)

    def init_psum(s):
        s.psb = [Buf(s.st.enter_context(s.nc.psum_tensor("ps%d" % i, [128, 512], F32)), "ps%d" % i) for i in range(8)]

    def ps(s, hold=False):
        while True:
            b = s.psb[s.psi]
            s.psi = (s.psi + 1) % 8
            if b not in s.held:
                break
        if hold:
            s.held.append(b)
        return b

    def release(s, b):
        s.held.remove(b)

    def _toks(s, reads, writes):
        toks = []
        for v in reads:
            for b in v.bufs:
                if b.w is not None:
                    toks.append(b.w)
        for v in writes:
            for b in v.bufs:
                if b.w is not None:
                    toks.append(b.w)
                toks.extend(b.r.values())
        return toks

    def _wait(s, eng, toks):
        kn = s.known[eng]
        for tok in toks:
            key = tok[0]
            val = tok[1]
            if key == eng and eng == "pe":
                continue
            if kn.get(key, 0) >= val:
                continue
            sem = s.esem[key] if isinstance(key, str) else s.dsl[key[0]][key[1]][0]
            s.E[eng].wait_ge(sem, val)
            kn[key] = val

    def _mark(s, tok, rkey, reads, writes):
        for v in writes:
            for b in v.bufs:
                b.w = tok
                b.r = {}
        for v in reads:
            for b in v.bufs:
                if b.w is tok:
                    continue
                b.r[rkey] = tok

    def op(s, eng, fn, reads, writes):
        reads = [v for v in reads if isinstance(v, V)]
        s._wait(eng, s._toks(reads, writes))
        ins = fn(s.E[eng])
        s.cnt[eng] += 1
        ins.then_inc(s.esem[eng], 1)
        tok = (eng, s.cnt[eng])
        s._mark(tok, eng, reads, writes)

    def dma(s, q, out, in_):
        si = s.dnext[q]
        s.dnext[q] = (si + 1) % s.ND
        slot = s.dsl[q][si]
        key = (q, si)
        toks = s._toks([in_], [out])
        if slot[1] > 0:
            toks.append((key, slot[1]))
        s._wait(q, toks)
        ins = s.E[q].dma_start(out=out.ap, in_=in_.ap)
        slot[1] += 16
        ins.then_inc(slot[0], 16)
        tok = (key, slot[1])
        s._mark(tok, key, [in_], [out])

    def barrier(s):
        toks = [(e, c) for e, c in s.cnt.items() if c > 0]
        for q in s.dsl:
            for i, (sem, val) in enumerate(s.dsl[q]):
                if val > 0:
                    toks.append(((q, i), val))
        for eng in s.E:
            s._wait(eng, toks)

    def scope(s):
        k = s

        class _Scope(ExitStack):
            def __exit__(self, *a):
                if a[0] is None:
                    k.barrier()
                return super().__exit__(*a)

        return _Scope()

    def finish(s, outs):
        toks = []
        for b in outs:
            if b.w is not None:
                toks.append(b.w)
        s._wait("sp", toks)

    @staticmethod
    def _a(x):
        return x.ap if isinstance(x, V) else x

    def mm(s, out, lhsT, rhs, start=True, stop=True):
        s.op("pe", lambda e: e.matmul(out.ap, lhsT.ap, rhs.ap, start=start, stop=stop), [lhsT, rhs] + ([] if start else [out]), [out])

    def tr(s, out, in_, ident):
        s.op("pe", lambda e: e.transpose(out.ap, in_.ap, ident.ap), [in_, ident], [out])

    def act(s, out, in_, func, bias=0.0, scale=1.0, accum=None):
        kw = {}
        if accum is not None:
            kw["accum_out"] = accum.ap
        s.op("act", lambda e: e.activation(out=out.ap, in_=in_.ap, func=func, bias=s._a(bias), scale=s._a(scale), **kw),
             [in_, bias, scale], [out] + ([accum] if accum is not None else []))

    def tt(s, out, a, b, op, eng="dve"):
        s.op(eng, lambda e: e.tensor_tensor(out=out.ap, in0=a.ap, in1=b.ap, op=op), [a, b], [out])

    def ts(s, out, a, s1, op0, s2=None, op1=None, eng="dve", accum=None):
        kw = {}
        if accum is not None:
            kw["accum_out"] = accum.ap
        if op1 is None:
            s.op(eng, lambda e: e.tensor_scalar(out=out.ap, in0=a.ap, scalar1=s._a(s1), scalar2=None, op0=op0, **kw), [a, s1], [out])
        else:
            s.op(eng, lambda e: e.tensor_scalar(out=out.ap, in0=a.ap, scalar1=s._a(s1), scalar2=s._a(s2), op0=op0, op1=op1, **kw),
                 [a, s1, s2], [out] + ([accum] if accum is not None else []))

    def stt(s, out, a, sc, b, op0, op1):
        s.op("dve", lambda e: e.scalar_tensor_tensor(out=out.ap, in0=a.ap, scalar=s._a(sc), in1=b.ap, op0=op0, op1=op1), [a, sc, b], [out])

    def copy(s, out, in_, eng="dve"):
        if eng == "act":
            s.op("act", lambda e: e.copy(out=out.ap, in_=in_.ap), [in_], [out])
        else:
            s.op(eng, lambda e: e.tensor_copy(out=out.ap, in_=in_.ap), [in_], [out])

    def memset(s, out, val, eng="dve"):
        s.op(eng, lambda e: e.memset(out.ap, val), [], [out])

    def recip(s, out, in_):
        s.op("dve", lambda e: e.reciprocal(out=out.ap, in_=in_.ap), [in_], [out])

    def scan(s, out, d0, d1, init, op0=ALU.mult, op1=ALU.add):
        s.op("dve", lambda e: e.tensor_tensor_scan(out.ap, d0.ap, d1.ap, s._a(init), op0, op1), [d0, d1, init], [out])

    def rmax(s, out, in_):
        s.op("dve", lambda e: e.reduce_max(out=out.ap, in_=in_.ap, axis=AX.X), [in_], [out])

    def rsum(s, out, in_):
        s.op("dve", lambda e: e.reduce_sum(out=out.ap, in_=in_.ap, axis=AX.X), [in_], [out])

    def rstd(s, out, ss, n, tmp):
        s.ts(tmp, ss, 1.0 / n, ALU.mult, NORM_EPS, ALU.add)
        s.act(tmp, tmp, AF.Sqrt)
        s.recip(out, tmp)


def chunks(n, c=128):
    return [(i, min(c, n - i)) for i in range(0, n, c)]


def build_program(cfg, debug=()):
    nc = bass.Bass("TRN2", target_bir_lowering=False)
    D, N, LC, T, KC = cfg.D, cfg.N, cfg.LC, cfg.T, cfg.KC
    L = cfg.DEPTH
    dbg = {}

    def ext(name, shape, dt=F32):
        return Buf(nc.dram_tensor(name, list(shape), dt, kind="ExternalInput"), name)

    I = {}
    I["x"] = ext("x", [N, D])
    I["ctx"] = ext("ctx", [LC, D])
    I["c"] = ext("c", [KC, 128])
    I["c_ctx"] = ext("c_ctx", [KC, 128])
    I["ada_w"] = ext("ada_w", [L, D, 6 * D])
    I["ada_b"] = ext("ada_b", [L, 6 * D])
    I["norm_mix"] = ext("norm_mix", [L, D])
    I["norm_ffn"] = ext("norm_ffn", [L, D])
    I["w_in"] = ext("w_in", [L, D, cfg.INW])
    I["w_out"] = ext("w_out", [L, D, D])
    I["diff_lambda"] = ext("diff_lambda", [L, 256])
    I["diff_subln"] = ext("diff_subln", [L, 128])
    I["s5_a_re"] = ext("s5_a_re", [L, 2, cfg.S5_G * 64])
    I["s5_a_im"] = ext("s5_a_im", [L, 2, cfg.S5_G * 64])
    I["s5_log_dt"] = ext("s5_log_dt", [L, 2, cfg.S5_G])
    I["s5_b_re"] = ext("s5_b_re", [L, 2, cfg.S5_G * 64, 16])
    I["s5_b_im"] = ext("s5_b_im", [L, 2, cfg.S5_G * 64, 16])
    I["s5_c_re"] = ext("s5_c_re", [L, 2, cfg.S5_G * 16, 64])
    I["s5_c_im"] = ext("s5_c_im", [L, 2, cfg.S5_G * 16, 64])
    I["s5_d"] = ext("s5_d", [L, cfg.GW])
    I["s5_glu_w"] = ext("s5_glu_w", [L, cfg.GW, cfg.GW])
    I["s5_glu_b"] = ext("s5_glu_b", [L, cfg.GW])
    I["mla_q_norm"] = ext("mla_q_norm", [L, cfg.QR])
    I["mla_kv_norm"] = ext("mla_kv_norm", [L, cfg.KVR])
    I["mla_w_uq"] = ext("mla_w_uq", [L, cfg.QR, cfg.MLA_HEADS * 192])
    I["mla_w_ukv"] = ext("mla_w_ukv", [L, cfg.KVR, cfg.MLA_HEADS * 256])
    I["ret_decay"] = ext("ret_decay", [L, 2 * cfg.RET_HEADS])
    I["ret_norm"] = ext("ret_norm", [L, cfg.GW])
    I["moe_wr"] = ext("moe_wr", [L, D, 20])
    I["moe_br"] = ext("moe_br", [L, 20])
    I["moe_w_gate"] = ext("moe_w_gate", [L, 16, D, cfg.FF])
    I["moe_w_up"] = ext("moe_w_up", [L, 16, D, cfg.FF])
    I["moe_w_down"] = ext("moe_w_down", [L, 16, cfg.FF, D])
    I["final_norm"] = ext("final_norm", [1, D])
    I["ident"] = ext("ident", [128, 128])
    I["rperm"] = ext("rperm", [128, 128])
    I["ropecs"] = ext("ropecs", [2, 128, N])
    OUT = Buf(nc.dram_tensor("out", [N, D], F32, kind="ExternalOutput"), "out")

    with ExitStack() as st:
        k = K(nc, st)
        k.init_psum()
        ident = k.sb([128, 128], F32, "ident")
        identb = k.sb([128, 128], BF16, "identb")
        ones = k.sb([128, 128], F32, "ones")
        onesb = k.sb([128, 128], BF16, "onesb")
        rperm = k.sb([128, 128], BF16, "rperm")
        k.dma("sp", ident[:, :], I["ident"][:, :])
        k.dma("pool", identb[:, :], I["ident"][:, :])
        k.dma("pool", rperm[:, :], I["rperm"][:, :])
        k.memset(ones[:, :], 1.0)
        k.memset(onesb[:, :], 1.0)

        xres = k.dram([T, D], F32, "xres")
        xres_t = [xres.sub() for _ in range(T // 128)]
        modbc = [[[k.dram([128, D], F32, "modbc") for j in range(6)] for r in range(2)] for l in range(L)]
        hT = k.dram([cfg.NTB, 128, KC, 512], BF16, "hT")
        hT_b = [hT.sub() for _ in range(cfg.NTB)]
        dwd = k.dram([T, 16], F32, "dw")
        dwd_t = [dwd.sub() for _ in range(T // 128)]

        for i in range(N // 128):
            k.dma("sp", V(xres.t[i * 128:(i + 1) * 128, :], (xres_t[i],)), I["x"][i * 128:(i + 1) * 128, :])
        for i in range(LC // 128):
            j = N // 128 + i
            k.dma("sp", V(xres.t[j * 128:(j + 1) * 128, :], (xres_t[j],)), I["ctx"][i * 128:(i + 1) * 128, :])

        def stage_ada():
            with k.scope() as ls:
                rep = [k.sb([128, KC, 128], F32, "rep", ls) for r in range(2)]
                crow = k.sb([KC, 128], F32, "crow", ls)
                colv = k.sb([128, KC], F32, "colv", ls)
                for r, src in enumerate((I["c"], I["c_ctx"])):
                    k.dma("sp", crow[:, :], src[:, :])
                    k.act(crow[:, :], crow[:, :], AF.Silu)
                    p = k.ps()
                    k.tr(p[:, 0:KC], crow[:, :], ident[0:KC, 0:KC])
                    k.copy(colv[:, :], p[:, 0:KC])
                    for c in range(KC):
                        k.ts(rep[r][:, c, :], ones[:, :], colv[:, c:c + 1], ALU.mult, eng=("dve" if c % 2 else "pool"))
                KB = min(8, KC)
                wt = [k.sb([128, KB, 512], F32, "adaw", ls) for _ in range(3)]
                brow = [k.sb([1, 512], F32, "brow", ls) for _ in range(2)]
                grow = [k.sb([1, 512], F32, "grow", ls) for _ in range(2)]
                stg = [k.sb([128, 512], F32, "stg", ls) for _ in range(2)]
                gbc = k.sb([128, 512], F32, "gbc", ls)
                wi = 0
                si = 0
                for l in range(L):
                    wv = I["ada_w"].t[l].rearrange("(c p) n -> p c n", p=128)
                    for ct in range(6 * D // 512):
                        j = (ct * 512) // D
                        col = ct * 512 - j * D
                        pp = [k.ps(), k.ps()]
                        br_ = brow[ct % 2]
                        k.dma("sp", br_[:, :], I["ada_b"][l:l + 1, ct * 512:(ct + 1) * 512])
                        for kb in range(0, KC, KB):
                            w = wt[wi % 3]
                            wi += 1
                            k.dma("sp", w[:, :, :], V(wv[:, kb:kb + KB, ct * 512:(ct + 1) * 512], (I["ada_w"],)))
                            for c in range(KB):
                                for r in range(2):
                                    k.mm(pp[r][:, :], rep[r][:, kb + c, :], w[:, c, :], start=(kb + c == 0), stop=False)
                        for r in range(2):
                            k.mm(pp[r][:, :], ones[0:1, :], br_[0:1, :], start=False, stop=True)
                        if j in (1, 4):
                            pg = k.ps()
                            gr_ = grow[ct % 2]
                            k.dma("sp", gr_[:, :], (I["norm_mix"] if j == 1 else I["norm_ffn"])[l:l + 1, col:col + 512])
                            k.mm(pg[:, :], ones[0:1, :], gr_[0:1, :])
                            k.copy(gbc[:, :], pg[:, :], eng="act")
                        for r in range(2):
                            sg = stg[si % 2]
                            si += 1
                            if j in (1, 4):
                                k.stt(sg[:, :], pp[r][:, :], 1.0, gbc[:, :], ALU.add, ALU.mult)
                            else:
                                k.copy(sg[:, :], pp[r][:, :], eng="act")
                            k.dma("sp", modbc[l][r][j][:, col:col + 512], sg[:, :])

        def stage_norm(l, which, blocks, router):
            jS, jA = (0, 1) if which == 0 else (3, 4)
            with k.scope() as ls:
                A = k.sb([128, D], F32, "A", ls)
                S = k.sb([128, D], F32, "S", ls)
                xt = [k.sb([128, D], F32, "xt", ls) for _ in range(2)]
                junk = k.sb([128, D], BF16, "junk", ls)
                hb = [k.sb([128, KC, 512], BF16, "hb", ls) for _ in range(2)]
                sm = k.sb([128, 8], F32, "sm", ls)
                if router:
                    hf = k.sb([128, KC * 128], F32, "hf", ls)
                    wr = k.sb([128, KC, 20], F32, "wr", ls)
                    brr = k.sb([1, 20], F32, "brr", ls)
                    k.dma("sp", wr[:, :, :], V(I["moe_wr"].t[l].rearrange("(c p) n -> p c n", p=128), (I["moe_wr"],)))
                    k.dma("sp", brr[:, :], I["moe_br"][l:l + 1, :])
                    rt = k.sb([128, 64], F32, "rt", ls)
                    dwt = k.sb([128, 16], F32, "dwt", ls)
                cur_r = None
                xi = 0
                for bi in blocks:
                    t0, bw, isc = cfg.blocks[bi]
                    r = 1 if isc else 0
                    if r != cur_r:
                        k.dma("sp", A[:, :], modbc[l][r][jA][:, :])
                        k.dma("sp", S[:, :], modbc[l][r][jS][:, :])
                        cur_r = r
                    hbb = hb[bi % 2]
                    for sub in range(bw // 128):
                        ti = (t0 + sub * 128) // 128
                        x = xt[xi % 2]
                        xi += 1
                        k.dma("sp", x[:, :], V(xres.t[ti * 128:(ti + 1) * 128, :], (xres_t[ti],)))
                        k.act(junk[:, :], x[:, :], AF.Square, accum=sm[:, 0:1])
                        k.rstd(sm[:, 1:2], sm[:, 0:1], D, sm[:, 2:3])
                        k.stt(x[:, :], x[:, :], sm[:, 1:2], A[:, :], ALU.mult, ALU.mult)
                        k.tt(x[:, :], x[:, :], S[:, :], ALU.add, eng="pool")
                        for c4 in range(0, KC, 4):
                            p = k.ps()
                            for c in range(c4, min(c4 + 4, KC)):
                                k.tr(p[:, (c - c4) * 128:(c - c4 + 1) * 128], x[:, c * 128:(c + 1) * 128], ident[:, :])
                            nn = min(4, KC - c4)
                            pv = p[:, 0:nn * 128].rearrange("p (c t) -> p c t", c=nn)
                            k.copy(hbb[:, c4:c4 + nn, sub * 128:(sub + 1) * 128], pv, eng=("dve" if router or (c4 // 4) % 2 else "act"))
                            if router:
                                k.ts(hf[:, c4 * 128:(c4 + nn) * 128], p[:, 0:nn * 128], 1.0, ALU.mult)
                        if router:
                            pr = k.ps()
                            for c in range(KC):
                                k.mm(pr[:, 0:20], hf[:, c * 128:(c + 1) * 128], wr[:, c, :], start=(c == 0), stop=False)
                            k.mm(pr[:, 0:20], ones[0:1, :], brr[0:1, :], start=False, stop=True)
                            lg = rt[:, 0:20]
                            k.copy(lg, pr[:, 0:20])
                            gmx = rt[:, 20:21]
                            k.rmax(gmx, rt[:, 0:4])
                            k.ts(rt[:, 24:28], rt[:, 0:4], gmx, ALU.subtract)
                            k.act(rt[:, 28:32], rt[:, 24:28], AF.Exp, accum=rt[:, 21:22])
                            k.recip(rt[:, 22:23], rt[:, 21:22])
                            k.ts(rt[:, 24:28], rt[:, 24:28], 0.0, ALU.is_ge)
                            k.ts(rt[:, 28:32], rt[:, 24:28], 1.0, ALU.subtract, 1e30, ALU.mult)
                            for g in range(4):
                                k.ts(rt[:, 32 + 4 * g:36 + 4 * g], rt[:, 4 + 4 * g:8 + 4 * g], rt[:, 28 + g:29 + g], ALU.add)
                            em = rt[:, 32:48]
                            k.rmax(rt[:, 48:49], em)
                            k.ts(dwt[:, :], em, rt[:, 48:49], ALU.is_ge)
                            k.stt(rt[:, 4:20], dwt[:, :], -1e30, em, ALU.mult, ALU.add)
                            k.rmax(rt[:, 49:50], rt[:, 4:20])
                            k.ts(rt[:, 32:48], rt[:, 4:20], rt[:, 49:50], ALU.is_ge)
                            k.tt(rt[:, 50:51], rt[:, 49:50], rt[:, 48:49], ALU.subtract)
                            k.act(rt[:, 51:52], rt[:, 50:51], AF.Exp)
                            k.ts(rt[:, 52:53], rt[:, 51:52], 1.0, ALU.add)
                            k.recip(rt[:, 52:53], rt[:, 52:53])
                            k.tt(rt[:, 53:54], rt[:, 52:53], rt[:, 22:23], ALU.mult)
                            k.tt(rt[:, 54:55], rt[:, 22:23], rt[:, 53:54], ALU.subtract)
                            k.ts(dwt[:, :], dwt[:, :], rt[:, 53:54], ALU.mult)
                            k.stt(dwt[:, :], rt[:, 32:48], rt[:, 54:55], dwt[:, :], ALU.mult, ALU.add)
                            k.dma("sp", V(dwd.t[ti * 128:(ti + 1) * 128, :], (dwd_t[ti],)), dwt[:, :])
                    k.dma("sp", V(hT.t[bi][:, :, 0:bw], (hT_b[bi],)), hbb[:, :, 0:bw])

        def fm_alloc(nrows, name, dt=BF16):
            nch = (nrows + 127) // 128
            d = k.dram([nch, cfg.NTB, 128, 512], dt, name)
            return d, [[d.sub() for _ in range(cfg.NTB)] for _ in range(nch)]

        FM = {}
        for nm in ("dq", "dk", "su", "cq", "ckv", "kr", "rq", "rk", "rg"):
            FM[nm] = fm_alloc(cfg.off[nm][1], "fm_" + nm)

        def fmv(nm, ch, tb, rows=128, w=512):
            d, subs = FM[nm]
            return V(d.t[ch, tb][0:rows, 0:w], (subs[ch][tb],))

        TMV = {}
        for nm, nh in (("dv", cfg.DIFF_HEADS), ("rv", cfg.RET_HEADS)):
            d = k.dram([nh, 128, cfg.KT, 128], BF16, "tm_" + nm)
            TMV[nm] = (d, [[d.sub() for _ in range(cfg.KT)] for _ in range(nh)])
        rq_bc = k.dram([cfg.NTB, 128, 512], F32, "rq_bc")
        rq_bc_s = [rq_bc.sub() for _ in range(cfg.NTB)]
        rkv_bc = k.dram([cfg.NTB, 128, 512], F32, "rkv_bc")
        rkv_bc_s = [rkv_bc.sub() for _ in range(cfg.NTB)]
        rkv_tm = k.dram([128, cfg.KT], F32, "rkv_tm")
        rkv_tm_s = [rkv_tm.sub() for _ in range(cfg.NTB)]

        def row_to_cols(dst, src_row, n, tmp_row):
            k.dma("sp", tmp_row[0:1, 0:n], src_row)
            p = k.ps()
            for i, (c0, cw) in enumerate(chunks(n)):
                k.tr(p[0:cw, i:i + 1], tmp_row[0:1, c0:c0 + cw], ident[0:1, 0:1])
                k.copy(dst[0:cw, i:i + 1], p[0:cw, i:i + 1])

        def stage_proj(l):
            wv = I["w_in"].t[l].rearrange("(c p) n -> p c n", p=128)
            csv = I["ropecs"].t.rearrange("a p n -> p a n")
            with k.scope() as ls:
                hb = [k.sb([128, KC, 512], BF16, "hb", ls) for _ in range(2)]
                wt = [k.sb([128, KC, 256], BF16, "wt", ls) for _ in range(3)]
                cs = [k.sb([128, 2, 512], F32, "cs", ls) for _ in range(2)]
                stg = [k.sb([128, 512], BF16, "stg", ls) for _ in range(3)]
                xs = [k.sb([128, 512], BF16, "xs", ls) for _ in range(2)]
                t1 = [k.sb([128, 512], F32, "t1", ls) for _ in range(2)]
                t2 = [k.sb([128, 512], F32, "t2", ls) for _ in range(2)]
                sqf = [k.sb([128, 512], F32, "sqf", ls) for _ in range(2)]
                rbc = k.sb([128, 512], F32, "rbc", ls)
                rtmp = k.sb([128, 512], F32, "rtmp", ls)
                qn = k.sb([128, 16], F32, "qn", ls)
                rtm = k.sb([128, 4], F32, "rtm", ls)
                trow = k.sb([1, max(cfg.QR, 128)], F32, "trow", ls)
                row_to_cols(qn[:, 0:8], I["mla_q_norm"][l:l + 1, :], cfg.QR, trow)
                row_to_cols(qn[:, 8:16], I["mla_kv_norm"][l:l + 1, :], cfg.KVR, trow)
                cnt = {"w": 0, "s": 0, "x": 0}

                def load_w(c0, w):
                    t = wt[cnt["w"] % 3]
                    cnt["w"] += 1
                    k.dma("pool", t[:, :, 0:w], V(wv[:, :, c0:c0 + w], (I["w_in"],)))
                    return t

                for bi, (t0, bw, isc) in enumerate(cfg.blocks):
                    h = hb[bi % 2]
                    k.dma("sp", h[:, :, 0:bw], V(hT.t[bi][:, :, 0:bw], (hT_b[bi],)))
                    c_ = cs[bi % 2]
                    if not isc:
                        k.dma("sp", c_[:, :, :], V(csv[:, :, t0:t0 + 512], (I["ropecs"],)))
                    for nm in cfg.names:
                        g0, gw = cfg.off[nm]
                        if nm in ("dv", "rv"):
                            d, subs = TMV[nm]
                            for c0 in range(0, gw, 256):
                                w = min(256, gw - c0)
                                t = load_w(g0 + c0, w)
                                for sub in range(bw // 128):
                                    p = k.ps()
                                    for c in range(KC):
                                        k.mm(p[:, 0:w], h[:, c, sub * 128:(sub + 1) * 128], t[:, c, 0:w], start=(c == 0), stop=(c == KC - 1))
                                    sg = stg[cnt["s"] % 3]
                                    cnt["s"] += 1
                                    k.copy(sg[:, 0:w], p[:, 0:w], eng=("act" if cnt["s"] % 2 else "dve"))
                                    kt = (t0 + sub * 128) // 128
                                    for hh in range(w // 128):
                                        head = (c0 + hh * 128) // 128
                                        k.dma("sp", V(d.t[head][:, kt, :], (subs[head][kt],)), sg[:, hh * 128:(hh + 1) * 128])
                            continue
                        nch = (gw + 127) // 128
                        pss = k.ps(hold=True) if nm in ("cq", "ckv") else None
                        for c0 in range(0, gw, 256):
                            w = min(256, gw - c0)
                            t = load_w(g0 + c0, w)
                            for j0 in range(0, w, 128):
                                cw = min(128, w - j0)
                                ch = (c0 + j0) // 128
                                p = k.ps()
                                for c in range(KC):
                                    k.mm(p[0:cw, 0:bw], t[:, c, j0:j0 + cw], h[:, c, 0:bw], start=(c == 0), stop=(c == KC - 1))
                                sg = stg[cnt["s"] % 3]
                                cnt["s"] += 1
                                if nm in ("dq", "dk", "kr", "rq", "rk") and not isc:
                                    x_ = xs[cnt["x"] % 2]
                                    a1 = t1[cnt["x"] % 2]
                                    a2 = t2[cnt["x"] % 2]
                                    cnt["x"] += 1
                                    k.copy(x_[0:cw, 0:bw], p[0:cw, 0:bw], eng="act")
                                    p2 = k.ps()
                                    k.mm(p2[0:cw, 0:bw], rperm[0:cw, 0:cw], x_[0:cw, 0:bw])
                                    k.tt(a1[0:cw, 0:bw], x_[0:cw, 0:bw], c_[0:cw, 0, 0:bw], ALU.mult, eng="pool")
                                    k.tt(a2[0:cw, 0:bw], p2[0:cw, 0:bw], c_[0:cw, 1, 0:bw], ALU.mult)
                                    k.tt(sg[0:cw, 0:bw], a1[0:cw, 0:bw], a2[0:cw, 0:bw], ALU.add, eng="pool")
                                elif nm in ("cq", "ckv"):
                                    qi = (0 if nm == "cq" else 8) + ch
                                    k.act(sg[0:cw, 0:bw], p[0:cw, 0:bw], AF.Copy, scale=qn[0:cw, qi:qi + 1])
                                    sq = sqf[ch % 2]
                                    k.act(sq[0:cw, 0:bw], p[0:cw, 0:bw], AF.Square)
                                    k.mm(pss[:, 0:bw], ones[0:cw, :], sq[0:cw, 0:bw], start=(ch == 0), stop=(ch == nch - 1))
                                elif nm == "rg":
                                    k.act(sg[0:cw, 0:bw], p[0:cw, 0:bw], AF.Silu)
                                else:
                                    k.copy(sg[0:cw, 0:bw], p[0:cw, 0:bw], eng=("act" if cnt["s"] % 2 else "dve"))
                                k.dma("sp", fmv(nm, ch, bi, cw, bw), sg[0:cw, 0:bw])
                        if pss is not None:
                            k.rstd(rbc[:, 0:bw], pss[:, 0:bw], gw, rtmp[:, 0:bw])
                            k.release(pss)
                            if nm == "cq":
                                k.dma("sp", V(rq_bc.t[bi][:, 0:bw], (rq_bc_s[bi],)), rbc[:, 0:bw])
                            else:
                                k.dma("sp", V(rkv_bc.t[bi][:, 0:bw], (rkv_bc_s[bi],)), rbc[:, 0:bw])
                                pt = k.ps()
                                ns = bw // 128
                                for sub in range(ns):
                                    k.tr(pt[:, sub:sub + 1], rbc[0:1, sub * 128:(sub + 1) * 128], ident[0:1, 0:1])
                                k.copy(rtm[:, 0:ns], pt[:, 0:ns])
                                kt0 = t0 // 128
                                k.dma("sp", V(rkv_tm.t[:, kt0:kt0 + ns], (rkv_tm_s[bi],)), rtm[:, 0:ns])

        GWc = cfg.GW // 128
        FM["mix"] = fm_alloc(D, "fm_mix")

        def bcast_col(dst, src11):
            p = k.ps()
            k.mm(p[:, 0:1], ones[0:1, :], src11)
            k.copy(dst, p[:, 0:1])

        def attn_core(ls_bufs, q_parts, k_parts, vT, ktiles, scale, bw):
            pT = ls_bufs
            po = k.ps(hold=True)
            pd = k.ps(hold=True)
            n = len(ktiles)
            for i, kt in enumerate(ktiles):
                p = k.ps()
                for j, (qv, kf) in enumerate(zip(q_parts, k_parts)):
                    k.mm(p[:, 0:bw], kf(kt), qv, start=(j == 0), stop=(j == len(q_parts) - 1))
                pt = pT[i % len(pT)]
                k.act(pt[:, 0:bw], p[:, 0:bw], AF.Exp, scale=scale)
                k.mm(po[:, 0:bw], vT[:, kt, :], pt[:, 0:bw], start=(i == 0), stop=(i == n - 1))
                k.mm(pd[:, 0:bw], onesb[:, :], pt[:, 0:bw], start=(i == 0), stop=(i == n - 1))
            return po, pd

        def q_blocks(need_ctx):
            return [(bi, t0, bw, isc) for bi, (t0, bw, isc) in enumerate(cfg.blocks) if (need_ctx or not isc)]

        def key_tiles(isc):
            return list(range(N // 128, cfg.KT)) if isc else list(range(cfg.KT))

        def stage_diff(l, need_ctx):
            lam_init = 0.8 - 0.6 * math.exp(-0.3 * l)
            with k.scope() as ls:
                kT = k.sb([128, T], BF16, "kT", ls)
                vT = k.sb([128, cfg.KT, 128], BF16, "vT", ls)
                qT = [k.sb([128, 512], BF16, "qT", ls) for _ in range(2)]
                pT = [k.sb([128, 512], BF16, "pT", ls) for _ in range(3)]
                a0 = k.sb([128, 512], F32, "a0", ls)
                a1 = k.sb([128, 512], F32, "a1", ls)
                rr = k.sb([128, 512], F32, "rr", ls)
                og = k.sb([128, 512], BF16, "og", ls)
                lr = k.sb([1, 256], F32, "lr", ls)
                lt = k.sb([1, 128], F32, "lt", ls)
                sc_ = k.sb([128, 8], F32, "sc", ls)
                trow = k.sb([1, 128], F32, "trow", ls)
                k.dma("sp", lr[:, :], I["diff_lambda"][l:l + 1, :])
                k.tt(lt[0:1, 0:64], lr[0:1, 0:64], lr[0:1, 64:128], ALU.mult)
                k.tt(lt[0:1, 64:128], lr[0:1, 128:192], lr[0:1, 192:256], ALU.mult)
                k.rsum(lr[0:1, 0:1], lt[0:1, 0:64])
                k.rsum(lr[0:1, 1:2], lt[0:1, 64:128])
                k.act(lr[0:1, 2:4], lr[0:1, 0:2], AF.Exp)
                k.tt(lr[0:1, 4:5], lr[0:1, 3:4], lr[0:1, 2:3], ALU.subtract)
                k.ts(lr[0:1, 5:6], lr[0:1, 4:5], -lam_init, ALU.add)
                bcast_col(sc_[:, 0:1], lr[0:1, 5:6])
                row_to_cols(sc_[:, 1:2], I["diff_subln"][l:l + 1, :], 128, trow)
                k.ts(sc_[:, 2:3], sc_[:, 1:2], 1.0 - lam_init, ALU.mult)
                qi = 0
                for h in range(cfg.DIFF_HEADS):
                    for bi, (t0, bw, isc) in enumerate(cfg.blocks):
                        k.dma("sp", kT[:, t0:t0 + bw], fmv("dk", h, bi, 128, bw))
                    dv, dvs = TMV["dv"]
                    k.dma("sp", vT[:, :, :], V(dv.t[h], tuple(dvs[h])))
                    for bi, t0, bw, isc in q_blocks(need_ctx):
                        q = qT[qi % 2]
                        qi += 1
                        k.dma("sp", q[:, 0:bw], fmv("dq", h, bi, 128, bw))
                        kts = key_tiles(isc)
                        for j in range(2):
                            r0, r1 = j * 64, (j + 1) * 64
                            po, pd = attn_core(pT, [q[r0:r1, 0:bw]], [lambda kt, r0=r0, r1=r1: kT[r0:r1, kt * 128:(kt + 1) * 128]],
                                               vT, kts, 64 ** -0.5, bw)
                            dst = a0 if j == 0 else a1
                            k.recip(rr[:, 0:bw], pd[:, 0:bw])
                            k.tt(dst[:, 0:bw], po[:, 0:bw], rr[:, 0:bw], ALU.mult)
                            k.release(po)
                            k.release(pd)
                        k.stt(a0[:, 0:bw], a1[:, 0:bw], sc_[:, 0:1], a0[:, 0:bw], ALU.mult, ALU.add)
                        k.act(a1[:, 0:bw], a0[:, 0:bw], AF.Square)
                        pss = k.ps()
                        k.mm(pss[:, 0:bw], ones[:, :], a1[:, 0:bw])
                        k.rstd(rr[:, 0:bw], pss[:, 0:bw], 128, a1[:, 0:bw])
                        k.stt(og[:, 0:bw], a0[:, 0:bw], sc_[:, 2:3], rr[:, 0:bw], ALU.mult, ALU.mult)
                        k.dma("sp", fmv("mix", h, bi, 128, bw), og[:, 0:bw])

        MH = cfg.MLA_HEADS
        mqn = fm_alloc(MH * 128, "mqn")
        mqr = fm_alloc(MH * 128, "mqr")
        mkn = fm_alloc(MH * 128, "mkn")
        mvd = k.dram([MH, 128, cfg.KT, 128], BF16, "mv")
        mvs = [[mvd.sub() for _ in range(cfg.KT)] for _ in range(MH)]

        def stage_mla(l, need_ctx):
            qch = chunks(cfg.QR)
            kch = chunks(cfg.KVR)
            csv = I["ropecs"].t.rearrange("a p n -> p a n")
            with k.scope() as ls:
                wq = k.sb([128, len(qch), MH * 192], BF16, "wq", ls)
                wkv = k.sb([128, len(kch), MH * 256], BF16, "wkv", ls)
                for ci, (c0, cw) in enumerate(qch):
                    k.dma("pool", wq[0:cw, ci, :], I["mla_w_uq"][l, c0:c0 + cw, :])
                for ci, (c0, cw) in enumerate(kch):
                    k.dma("pool", wkv[0:cw, ci, :], I["mla_w_ukv"][l, c0:c0 + cw, :])
                rtm_sb = k.sb([128, cfg.KT], F32, "rtm_sb", ls)
                k.dma("sp", rtm_sb[:, :], V(rkv_tm.t[:, :], tuple(rkv_tm_s)))
                cqs = k.sb([128, len(qch), 512], BF16, "cqs", ls)
                cks = k.sb([128, len(kch), 512], BF16, "cks", ls)
                rqt = k.sb([128, 512], F32, "rqt", ls)
                rkt = k.sb([128, 512], F32, "rkt", ls)
                cs_ = k.sb([128, 2, 512], F32, "cs", ls)
                stg = [k.sb([128, 512], BF16, "stg", ls) for _ in range(3)]
                xs = k.sb([128, 512], BF16, "xs", ls)
                b1 = k.sb([128, 512], F32, "b1", ls)
                b2 = k.sb([128, 512], F32, "b2", ls)
                si = 0
                for bi, (t0, bw, isc) in enumerate(cfg.blocks):
                    for ci, (c0, cw) in enumerate(qch):
                        k.dma("sp", cqs[0:cw, ci, 0:bw], fmv("cq", ci, bi, cw, bw))
                    for ci, (c0, cw) in enumerate(kch):
                        k.dma("sp", cks[0:cw, ci, 0:bw], fmv("ckv", ci, bi, cw, bw))
                    k.dma("sp", rqt[:, 0:bw], V(rq_bc.t[bi][:, 0:bw], (rq_bc_s[bi],)))
                    k.dma("sp", rkt[:, 0:bw], V(rkv_bc.t[bi][:, 0:bw], (rkv_bc_s[bi],)))
                    if not isc:
                        k.dma("sp", cs_[:, :, :], V(csv[:, :, t0:t0 + 512], (I["ropecs"],)))
                    for h in range(MH):
                        if need_ctx or not isc:
                            p = k.ps()
                            for ci, (c0, cw) in enumerate(qch):
                                k.mm(p[:, 0:bw], wq[0:cw, ci, h * 192:h * 192 + 128], cqs[0:cw, ci, 0:bw], start=(ci == 0), stop=(ci == len(qch) - 1))
                            sg = stg[si % 3]
                            si += 1
                            k.tt(sg[:, 0:bw], p[:, 0:bw], rqt[:, 0:bw], ALU.mult)
                            k.dma("sp", V(mqn[0].t[h, bi][:, 0:bw], (mqn[1][h][bi],)), sg[:, 0:bw])
                            p = k.ps()
                            for ci, (c0, cw) in enumerate(qch):
                                k.mm(p[0:64, 0:bw], wq[0:cw, ci, h * 192 + 128:h * 192 + 192], cqs[0:cw, ci, 0:bw], start=(ci == 0), stop=(ci == len(qch) - 1))
                            sg = stg[si % 3]
                            si += 1
                            if isc:
                                k.tt(sg[0:64, 0:bw], p[0:64, 0:bw], rqt[0:64, 0:bw], ALU.mult)
                            else:
                                k.tt(xs[0:64, 0:bw], p[0:64, 0:bw], rqt[0:64, 0:bw], ALU.mult)
                                p2 = k.ps()
                                k.mm(p2[0:64, 0:bw], rperm[0:64, 0:64], xs[0:64, 0:bw])
                                k.tt(b1[0:64, 0:bw], xs[0:64, 0:bw], cs_[0:64, 0, 0:bw], ALU.mult, eng="pool")
                                k.tt(b2[0:64, 0:bw], p2[0:64, 0:bw], cs_[0:64, 1, 0:bw], ALU.mult)
                                k.tt(sg[0:64, 0:bw], b1[0:64, 0:bw], b2[0:64, 0:bw], ALU.add, eng="pool")
                            k.dma("sp", V(mqr[0].t[h, bi][0:64, 0:bw], (mqr[1][h][bi],)), sg[0:64, 0:bw])
                        p = k.ps()
                        for ci, (c0, cw) in enumerate(kch):
                            k.mm(p[:, 0:bw], wkv[0:cw, ci, h * 256:h * 256 + 128], cks[0:cw, ci, 0:bw], start=(ci == 0), stop=(ci == len(kch) - 1))
                        sg = stg[si % 3]
                        si += 1
                        k.tt(sg[:, 0:bw], p[:, 0:bw], rkt[:, 0:bw], ALU.mult)
                        k.dma("sp", V(mkn[0].t[h, bi][:, 0:bw], (mkn[1][h][bi],)), sg[:, 0:bw])
                        for sub in range(bw // 128):
                            kt = t0 // 128 + sub
                            p = k.ps()
                            for ci, (c0, cw) in enumerate(kch):
                                k.mm(p[:, 0:128], cks[0:cw, ci, sub * 128:(sub + 1) * 128], wkv[0:cw, ci, h * 256 + 128:h * 256 + 256], start=(ci == 0), stop=(ci == len(kch) - 1))
                            sg = stg[si % 3]
                            si += 1
                            k.ts(sg[:, 0:128], p[:, 0:128], rtm_sb[:, kt:kt + 1], ALU.mult)
                            k.dma("sp", V(mvd.t[h][:, kt, :], (mvs[h][kt],)), sg[:, 0:128])
            with k.scope() as ls:
                kTn = k.sb([128, T], BF16, "kTn", ls)
                kTr = k.sb([64, T], BF16, "kTr", ls)
                vT = k.sb([128, cfg.KT, 128], BF16, "vT", ls)
                qn = [k.sb([128, 512], BF16, "qn", ls) for _ in range(2)]
                qr = [k.sb([64, 512], BF16, "qr", ls) for _ in range(2)]
                pT = [k.sb([128, 512], BF16, "pT", ls) for _ in range(3)]
                rr = k.sb([128, 512], F32, "rr", ls)
                og = k.sb([128, 512], BF16, "og", ls)
                for bi, (t0, bw, isc) in enumerate(cfg.blocks):
                    k.dma("sp", kTr[:, t0:t0 + bw], fmv("kr", 0, bi, 64, bw))
                qi = 0
                for h in range(MH):
                    for bi, (t0, bw, isc) in enumerate(cfg.blocks):
                        k.dma("sp", kTn[:, t0:t0 + bw], V(mkn[0].t[h, bi][:, 0:bw], (mkn[1][h][bi],)))
                    k.dma("sp", vT[:, :, :], V(mvd.t[h], tuple(mvs[h])))
                    for bi, t0, bw, isc in q_blocks(need_ctx):
                        qa, qb_ = qn[qi % 2], qr[qi % 2]
                        qi += 1
                        k.dma("sp", qa[:, 0:bw], V(mqn[0].t[h, bi][:, 0:bw], (mqn[1][h][bi],)))
                        k.dma("sp", qb_[:, 0:bw], V(mqr[0].t[h, bi][0:64, 0:bw], (mqr[1][h][bi],)))
                        po, pd = attn_core(pT, [qa[:, 0:bw], qb_[0:64, 0:bw]],
                                           [lambda kt: kTn[:, kt * 128:(kt + 1) * 128], lambda kt: kTr[0:64, kt * 128:(kt + 1) * 128]],
                                           vT, key_tiles(isc), 192 ** -0.5, bw)
                        k.recip(rr[:, 0:bw], pd[:, 0:bw])
                        k.tt(og[:, 0:bw], po[:, 0:bw], rr[:, 0:bw], ALU.mult)
                        k.release(po)
                        k.release(pd)
                        k.dma("sp", fmv("mix", 2 * GWc + h, bi, 128, bw), og[:, 0:bw])

        def stage_ret(l, need_ctx):
            RH = cfg.RET_HEADS
            ksc = 64 ** -0.5
            NI = cfg.KT + 8
            with k.scope() as ls:
                dr_ = k.sb([1, 2 * RH], F32, "dr", ls)
                lgb_ = k.sb([128, 2 * RH], F32, "lg", ls)
                nlg = k.sb([128, 2 * RH], F32, "nlg", ls)
                d0i = k.sb([128, 512], mybir.dt.int32, "d0i", ls)
                D0 = k.sb([128, 512], F32, "D0", ls)
                ioi = k.sb([128, NI], mybir.dt.int32, "ioi", ls)
                iof = k.sb([128, NI], F32, "iof", ls)
                ctf = k.sb([128, NI], F32, "ctf", ls)
                ctb = k.sb([128, NI], F32, "ctb", ls)
                Ef = k.sb([128, 512], F32, "Ef", ls)
                Eb = k.sb([128, 512], F32, "Eb", ls)
                Wd = [k.sb([128, 512], F32, "Wd", ls) for _ in range(4)]
                w1 = k.sb([128, 512], F32, "w1", ls)
                w2 = k.sb([128, 512], F32, "w2", ls)
                w3 = k.sb([128, 512], F32, "w3", ls)
                kT = k.sb([128, T], BF16, "kT", ls)
                vT = k.sb([128, cfg.KT, 128], BF16, "vT", ls)
                qT = [k.sb([128, 512], BF16, "qT", ls) for _ in range(2)]
                gT = [k.sb([128, 512], BF16, "gT", ls) for _ in range(2)]
                pT = [k.sb([128, 512], BF16, "pT", ls) for _ in range(3)]
                ya = k.sb([128, 512], F32, "ya", ls)
                yb = k.sb([128, 512], F32, "yb", ls)
                rr = k.sb([128, 512], F32, "rr", ls)
                og = k.sb([128, 512], BF16, "og", ls)
                gcol = k.sb([128, GWc], F32, "gcol", ls)
                trow = k.sb([1, cfg.GW], F32, "trow", ls)
                row_to_cols(gcol, I["ret_norm"][l:l + 1, :], cfg.GW, trow)
                k.dma("sp", dr_[:, :], I["ret_decay"][l:l + 1, :])
                k.act(dr_[:, :], dr_[:, :], AF.Exp, scale=-1.0)
                k.ts(dr_[:, :], dr_[:, :], 1.0, ALU.add)
                k.act(dr_[:, :], dr_[:, :], AF.Ln)
                p = k.ps()
                k.mm(p[:, 0:2 * RH], ones[0:1, :], dr_[0:1, :])
                k.ts(lgb_[:, :], p[:, 0:2 * RH], -1.0, ALU.mult)
                k.ts(nlg[:, :], lgb_[:, :], -1.0, ALU.mult)
                k.op("pool", lambda e: e.iota(d0i.t[:, :], [[1, 512]], base=0, channel_multiplier=-1), [], [d0i[:, :]])
                k.copy(D0[:, :], d0i[:, :])
                k.op("pool", lambda e: e.iota(ioi.t[:, :], [[128, NI]], base=0, channel_multiplier=0), [], [ioi[:, :]])
                k.copy(iof[:, :], ioi[:, :])
                lnk = math.log(ksc)
                qi = 0
                for h in range(RH):
                    lf, lb = lgb_[:, h:h + 1], lgb_[:, RH + h:RH + h + 1]
                    nlb = nlg[:, RH + h:RH + h + 1]
                    k.act(ctf[:, :], iof[:, :], AF.Exp, scale=lf)
                    k.act(ctb[:, :], iof[:, :], AF.Exp, scale=lb)
                    k.act(Ef[:, :], D0[:, :], AF.Exp, scale=lf, bias=lnk)
                    k.act(Eb[:, :], D0[:, :], AF.Exp, scale=nlb, bias=lnk)
                    for oi in range(4):
                        off = float(oi * 128)
                        k.ts(w1[:, :], D0[:, :], -off, ALU.add, 0.0, ALU.max)
                        k.act(w1[:, :], w1[:, :], AF.Exp, scale=lf, bias=lnk)
                        k.ts(w2[:, :], D0[:, :], -off, ALU.add, 0.0, ALU.min)
                        k.act(w2[:, :], w2[:, :], AF.Exp, scale=nlb, bias=lnk)
                        k.ts(w3[:, :], D0[:, :], -off, ALU.add, 0.0, ALU.is_ge)
                        k.tt(w1[:, :], w1[:, :], w2[:, :], ALU.subtract)
                        k.tt(w1[:, :], w1[:, :], w3[:, :], ALU.mult)
                        k.tt(Wd[oi][:, :], w1[:, :], w2[:, :], ALU.add)
                    rch, rro = h // 2, (h % 2) * 64
                    for bi, (t0, bw, isc) in enumerate(cfg.blocks):
                        k.dma("sp", kT[:, t0:t0 + bw], fmv("rk", rch, bi, 128, bw))
                    rv_, rvs = TMV["rv"]
                    k.dma("sp", vT[:, :, :], V(rv_.t[h], tuple(rvs[h])))
                    for bi, t0, bw, isc in q_blocks(need_ctx):
                        q, g = qT[qi % 2], gT[qi % 2]
                        qi += 1
                        k.dma("sp", q[:, 0:bw], fmv("rq", rch, bi, 128, bw))
                        k.dma("sp", g[:, 0:bw], fmv("rg", h, bi, 128, bw))
                        kts = key_tiles(isc)
                        po = k.ps(hold=True)
                        for i, kt in enumerate(kts):
                            p = k.ps()
                            k.mm(p[:, 0:bw], kT[rro:rro + 64, kt * 128:(kt + 1) * 128], q[rro:rro + 64, 0:bw])
                            pt = pT[i % 3]
                            kctx = kt >= N // 128
                            if kctx == isc:
                                s0 = (kt * 128 - N) if isc else kt * 128
                                tq = 0 if isc else t0
                                if s0 + 128 <= tq:
                                    ci_ = (tq - s0) // 128
                                    k.stt(pt[:, 0:bw], p[:, 0:bw], ctf[:, ci_:ci_ + 1], Ef[:, 0:bw], ALU.mult, ALU.mult)
                                elif s0 >= tq + bw:
                                    ci_ = (s0 - tq) // 128
                                    k.stt(pt[:, 0:bw], p[:, 0:bw], ctb[:, ci_:ci_ + 1], Eb[:, 0:bw], ALU.mult, ALU.mult)
                                else:
                                    k.tt(pt[:, 0:bw], p[:, 0:bw], Wd[(s0 - tq) // 128][:, 0:bw], ALU.mult)
                            else:
                                c0 = kt * 128 - N
                                cf = (t0 - (c0 - LC)) // 128
                                cb = (N + c0 - t0) // 128
                                k.ts(w1[:, 0:bw], Ef[:, 0:bw], ctf[:, cf:cf + 1], ALU.mult)
                                k.stt(w1[:, 0:bw], Eb[:, 0:bw], ctb[:, cb:cb + 1], w1[:, 0:bw], ALU.mult, ALU.add)
                                k.tt(pt[:, 0:bw], p[:, 0:bw], w1[:, 0:bw], ALU.mult)
                            k.mm(po[:, 0:bw], vT[:, kt, :], pt[:, 0:bw], start=(i == 0), stop=(i == len(kts) - 1))
                        k.copy(ya[:, 0:bw], po[:, 0:bw])
                        k.release(po)
                        k.act(yb[:, 0:bw], ya[:, 0:bw], AF.Square)
                        pss = k.ps()
                        k.mm(pss[:, 0:bw], ones[:, :], yb[:, 0:bw])
                        k.rstd(rr[:, 0:bw], pss[:, 0:bw], 128, yb[:, 0:bw])
                        k.stt(ya[:, 0:bw], ya[:, 0:bw], gcol[:, h:h + 1], rr[:, 0:bw], ALU.mult, ALU.mult)
                        k.tt(og[:, 0:bw], ya[:, 0:bw], g[:, 0:bw], ALU.mult)
                        k.dma("sp", fmv("mix", 3 * GWc + h, bi, 128, bw), og[:, 0:bw])

        s5g = fm_alloc(cfg.GW, "s5g")
        PI = math.pi

        def stage_s5(l, need_ctx):
            G = cfg.S5_G
            NP = G // 2
            lat_b = [(bi, t0, bw) for bi, (t0, bw, isc) in enumerate(cfg.blocks) if not isc]
            ctx_b = [(bi, t0, bw) for bi, (t0, bw, isc) in enumerate(cfg.blocks) if isc]
            with k.scope() as ls:
                trow = k.sb([1, max(G * 64, cfg.GW)], F32, "trow", ls)
                bc = k.sb([128, G], F32, "bc", ls)
                are = k.sb([128, 2, NP], F32, "are", ls)
                aim = k.sb([128, 2, NP], F32, "aim", ls)
                dtc = k.sb([128, 2, NP], F32, "dtc", ls)
                rr = k.sb([128, 2, NP], F32, "rr", ls)
                th = k.sb([128, 2, NP], F32, "th", ls)
                cr = k.sb([128, 2, NP], F32, "cr", ls)
                ci = k.sb([128, 2, NP], F32, "ci", ls)
                ncr = k.sb([128, 2, NP], F32, "ncr", ls)
                q1 = k.sb([128, 2, NP], F32, "q1", ls)
                q2 = k.sb([128, 2, NP], F32, "q2", ls)
                q3 = k.sb([128, 2, NP], F32, "q3", ls)
                q4 = k.sb([128, 2, NP], F32, "q4", ls)
                dcol = k.sb([32, NP], F32, "dcol", ls)
                jfi = k.sb([128, 512], mybir.dt.int32, "jfi", ls)
                jf = k.sb([128, 512], F32, "jf", ls)

                def sincos(out_s, out_c, ang, tmp, tmpi):
                    k.ts(tmp, ang, 1.0 / (2 * PI), ALU.mult)
                    k.copy(tmpi, tmp)
                    k.copy(tmp, tmpi)
                    k.stt(tmp, tmp, -2 * PI, ang, ALU.mult, ALU.add)
                    k.ts(out_c, tmp, PI, ALU.is_gt)
                    k.stt(tmp, out_c, -2 * PI, tmp, ALU.mult, ALU.add)
                    k.ts(out_c, tmp, -PI, ALU.is_lt)
                    k.stt(tmp, out_c, 2 * PI, tmp, ALU.mult, ALU.add)
                    k.act(out_s, tmp, AF.Sin)
                    k.ts(tmp, tmp, 0.5 * PI, ALU.add)
                    k.ts(out_c, tmp, PI, ALU.is_gt)
                    k.stt(tmp, out_c, -2 * PI, tmp, ALU.mult, ALU.add)
                    k.act(out_c, tmp, AF.Sin)

                negpi = k.sb([128, 1], F32, "negpi", ls)
                k.memset(negpi[:, :], -PI)
                k.op("pool", lambda e: e.iota(jfi.t[:, :], [[1, 512]], base=1, channel_multiplier=0), [], [jfi[:, :]])
                k.copy(jf[:, :], jfi[:, :])
                for dr in range(2):
                    row_to_cols(are[:, dr, :], I["s5_a_re"][l, dr:dr + 1, :], G * 64, trow)
                    row_to_cols(aim[:, dr, :], I["s5_a_im"][l, dr:dr + 1, :], G * 64, trow)
                    k.dma("sp", trow[0:1, 0:G], I["s5_log_dt"][l, dr:dr + 1, :])
                    k.act(trow[0:1, 0:G], trow[0:1, 0:G], AF.Exp)
                    p = k.ps()
                    k.mm(p[:, 0:G], ones[0:1, :], trow[0:1, 0:G])
                    k.copy(bc[:, :], p[:, 0:G])
                    bcv = bc[:, :].rearrange("p (n two) -> p n two", two=2)
                    k.copy(dtc[0:64, dr, :], bcv[0:64, :, 0])
                    k.copy(dtc[64:128, dr, :], bcv[64:128, :, 1])
                k.dma("sp", trow[0:1, 0:cfg.GW], I["s5_d"][l:l + 1, :])
                p = k.ps()
                for i in range(NP):
                    k.tr(p[0:32, i:i + 1], trow[0:1, i * 32:(i + 1) * 32], ident[0:1, 0:1])
                k.copy(dcol[:, :], p[0:32, 0:NP])
                fl = lambda b: b[:, :, :].rearrange("p a n -> p (a n)")
                k.tt(fl(q1), fl(are), fl(dtc), ALU.mult)
                k.act(fl(rr), fl(q1), AF.Exp)
                k.tt(fl(th), fl(aim), fl(dtc), ALU.mult)
                qi32 = k.sb([128, 2 * NP], mybir.dt.int32, "qi32", ls)
                sincos(fl(q1), fl(q2), fl(th), fl(q3), qi32[:, :])
                k.tt(fl(q1), fl(q1), fl(rr), ALU.mult)
                k.tt(fl(q2), fl(q2), fl(rr), ALU.mult)
                k.ts(fl(q2), fl(q2), -1.0, ALU.add)
                k.tt(fl(q3), fl(are), fl(are), ALU.mult)
                k.tt(fl(q4), fl(aim), fl(aim), ALU.mult)
                k.tt(fl(q3), fl(q3), fl(q4), ALU.add)
                k.recip(fl(q3), fl(q3))
                k.tt(fl(cr), fl(q2), fl(are), ALU.mult)
                k.tt(fl(q4), fl(q1), fl(aim), ALU.mult)
                k.tt(fl(cr), fl(cr), fl(q4), ALU.add)
                k.tt(fl(cr), fl(cr), fl(q3), ALU.mult)
                k.tt(fl(ci), fl(q1), fl(are), ALU.mult)
                k.tt(fl(q4), fl(q2), fl(aim), ALU.mult)
                k.tt(fl(ci), fl(ci), fl(q4), ALU.subtract)
                k.tt(fl(ci), fl(ci), fl(q3), ALU.mult)
                k.ts(fl(ncr), fl(cr), -1.0, ALU.mult)

                ang = k.sb([128, 512], F32, "ang", ls)
                atmp = k.sb([128, 512], F32, "atmp", ls)
                cosJ = k.sb([128, 512], F32, "cosJ", ls)
                sinJ = k.sb([128, 512], F32, "sinJ", ls)
                tre = k.sb([128, 512], F32, "tre", ls)
                tim = k.sb([128, 512], F32, "tim", ls)
                rJ = k.sb([128, 512], F32, "rJ", ls)
                z1 = k.sb([128, 512], F32, "z1", ls)
                z2 = k.sb([128, 512], F32, "z2", ls)
                zr = k.sb([128, 512], F32, "zr", ls)
                zi = k.sb([128, 512], F32, "zi", ls)
                xr = k.sb([128, 512], F32, "xr", ls)
                xi = k.sb([128, 512], F32, "xi", ls)
                zp = k.sb([128, 2], F32, "zp", ls)
                Bw = [k.sb([128, 32], F32, "Bw", ls) for _ in range(2)]
                Bl = [k.sb([32, 128], F32, "Bl", ls) for _ in range(2)]
                Cw = [k.sb([32, 128], F32, "Cw", ls) for _ in range(2)]
                Cl = [k.sb([128, 32], F32, "Cl", ls) for _ in range(2)]
                uf = k.sb([32, T], F32, "uf", ls)
                ya = k.sb([32, T], F32, "ya", ls)
                g1 = k.sb([32, T], F32, "g1", ls)
                gb = k.sb([32, T], BF16, "gb", ls)
                for bw_ in Bw:
                    k.memset(bw_[:, :], 0.0)
                for cw_ in Cw:
                    k.memset(cw_[:, :], 0.0)
                GC = math.sqrt(2.0 / math.pi) * 2.0
                for pk in range(NP):
                    ch, ro = (pk * 32) // 128, (pk * 32) % 128
                    for bi, (t0, bw, isc) in enumerate(cfg.blocks):
                        d_, subs_ = FM["su"]
                        k.dma("pool", uf[:, t0:t0 + bw], V(d_.t[ch, bi][ro:ro + 32, 0:bw], (subs_[ch][bi],)))
                    for dr in range(2):
                        for ri, nm in enumerate(("s5_b_re", "s5_b_im")):
                            src = I[nm]
                            k.dma("sp", Bw[ri][0:64, 0:16], src[l, dr, pk * 128:pk * 128 + 64, :])
                            k.dma("sp", Bw[ri][64:128, 16:32], src[l, dr, pk * 128 + 64:pk * 128 + 128, :])
                            p = k.ps()
                            k.tr(p[0:32, 0:128], Bw[ri][:, :], ident[:, :])
                            k.copy(Bl[ri][:, :], p[0:32, 0:128])
                        for ri, nm in enumerate(("s5_c_re", "s5_c_im")):
                            src = I[nm]
                            k.dma("sp", Cw[ri][0:16, 0:64], src[l, dr, pk * 32:pk * 32 + 16, :])
                            k.dma("sp", Cw[ri][16:32, 64:128], src[l, dr, pk * 32 + 16:pk * 32 + 32, :])
                            p = k.ps()
                            k.tr(p[:, 0:32], Cw[ri][:, :], ident[0:32, 0:32])
                            if ri == 0:
                                k.copy(Cl[ri][:, :], p[:, 0:32])
                            else:
                                k.ts(Cl[ri][:, :], p[:, 0:32], -1.0, ALU.mult)
                        k.ts(ang[:, :], jf[:, :], th[:, dr, pk:pk + 1], ALU.mult)
                        sincos(sinJ[:, :], cosJ[:, :], ang[:, :], atmp[:, :], jfi[:, :])
                        k.ts(tre[:, :], cosJ[:, :], cr[:, dr, pk:pk + 1], ALU.mult)
                        k.stt(tre[:, :], sinJ[:, :], ci[:, dr, pk:pk + 1], tre[:, :], ALU.mult, ALU.add)
                        k.ts(tim[:, :], cosJ[:, :], ci[:, dr, pk:pk + 1], ALU.mult)
                        k.stt(tim[:, :], sinJ[:, :], ncr[:, dr, pk:pk + 1], tim[:, :], ALU.mult, ALU.add)
                        k.memset(rJ[:, :], 1.0)
                        k.ts(rJ[:, :], rJ[:, :], rr[:, dr, pk:pk + 1], ALU.mult)
                        k.memset(zp[:, :], 0.0)
                        seq = (ctx_b + lat_b) if dr == 0 else (ctx_b[::-1] + lat_b[::-1])
                        for bi, t0, bw in seq:
                            isc = cfg.blocks[bi][2]
                            rv = (lambda v: v[:, ::-1]) if dr == 1 else (lambda v: v)
                            pbr = k.ps()
                            k.mm(pbr[:, 0:bw], Bl[0][:, :], uf[:, t0:t0 + bw])
                            pbi = k.ps()
                            k.mm(pbi[:, 0:bw], Bl[1][:, :], uf[:, t0:t0 + bw])
                            br_, bi_ = rv(pbr[:, 0:bw]), rv(pbi[:, 0:bw])
                            k.tt(z1[:, 0:bw], tre[:, 0:bw], br_, ALU.mult)
                            k.tt(z2[:, 0:bw], tim[:, 0:bw], bi_, ALU.mult)
                            k.tt(zr[:, 0:bw], z1[:, 0:bw], z2[:, 0:bw], ALU.subtract, eng="pool")
                            k.tt(z1[:, 0:bw], tre[:, 0:bw], bi_, ALU.mult)
                            k.tt(z2[:, 0:bw], tim[:, 0:bw], br_, ALU.mult)
                            k.tt(zi[:, 0:bw], z1[:, 0:bw], z2[:, 0:bw], ALU.add, eng="pool")
                            k.scan(zr[:, 0:bw], rJ[:, 0:bw], zr[:, 0:bw], zp[:, 0:1])
                            k.scan(zi[:, 0:bw], rJ[:, 0:bw], zi[:, 0:bw], zp[:, 1:2])
                            k.tt(z1[:, 0:bw], cosJ[:, 0:bw], zr[:, 0:bw], ALU.mult)
                            k.tt(z2[:, 0:bw], sinJ[:, 0:bw], zi[:, 0:bw], ALU.mult, eng="pool")
                            k.tt(xr[:, 0:bw], z1[:, 0:bw], z2[:, 0:bw], ALU.subtract)
                            k.tt(z1[:, 0:bw], sinJ[:, 0:bw], zr[:, 0:bw], ALU.mult, eng="pool")
                            k.tt(z2[:, 0:bw], cosJ[:, 0:bw], zi[:, 0:bw], ALU.mult)
                            k.tt(xi[:, 0:bw], z1[:, 0:bw], z2[:, 0:bw], ALU.add, eng="pool")
                            k.copy(zp[:, 0:1], xr[:, bw - 1:bw])
                            k.copy(zp[:, 1:2], xi[:, bw - 1:bw])
                            if isc and not need_ctx:
                                continue
                            py = k.ps()
                            k.mm(py[0:32, 0:bw], Cl[0][:, :], xr[:, 0:bw], start=True, stop=False)
                            k.mm(py[0:32, 0:bw], Cl[1][:, :], xi[:, 0:bw], start=False, stop=True)
                            if dr == 0:
                                k.stt(ya[:, t0:t0 + bw], uf[:, t0:t0 + bw], dcol[:, pk:pk + 1], py[0:32, 0:bw], ALU.mult, ALU.add)
                            else:
                                k.tt(ya[:, t0:t0 + bw], ya[:, t0:t0 + bw], py[0:32, 0:bw][:, ::-1], ALU.add)
                    tr_ = [(bi, t0, bw) for bi, (t0, bw, isc) in enumerate(cfg.blocks) if (need_ctx or not isc)]
                    t_lo, t_hi = tr_[0][1], tr_[-1][1] + tr_[-1][2]
                    yv = ya[:, t_lo:t_hi]
                    gv = g1[:, t_lo:t_hi]
                    k.tt(gv, yv, yv, ALU.mult)
                    k.ts(gv, gv, 0.044715, ALU.mult, 1.0, ALU.add)
                    k.tt(gv, gv, yv, ALU.mult, eng="pool")
                    k.act(gv, gv, AF.Sigmoid, scale=GC)
                    k.tt(gb[:, t_lo:t_hi], gv, yv, ALU.mult)
                    for bi, t0, bw in tr_:
                        k.dma("sp", V(s5g[0].t[ch, bi][ro:ro + 32, 0:bw], (s5g[1][ch][bi],)), gb[:, t0:t0 + bw])
            with k.scope() as ls:
                gw = k.sb([128, GWc, cfg.GW], BF16, "gw", ls)
                k.dma("pool", gw[:, :, :], V(I["s5_glu_w"].t[l].rearrange("(c p) n -> p c n", p=128), (I["s5_glu_w"],)))
                bcol = k.sb([128, GWc], F32, "bcol", ls)
                trow = k.sb([1, cfg.GW], F32, "trow", ls)
                row_to_cols(bcol, I["s5_glu_b"][l:l + 1, :], cfg.GW, trow)
                gin = [k.sb([128, GWc, 512], BF16, "gin", ls) for _ in range(2)]
                gt = [k.sb([128, 512], F32, "gt", ls) for _ in range(2)]
                og = [k.sb([128, 512], BF16, "og", ls) for _ in range(2)]
                n = 0
                for bi, t0, bw, isc in q_blocks(need_ctx):
                    gi = gin[bi % 2]
                    for c in range(GWc):
                        k.dma("sp", gi[:, c, 0:bw], V(s5g[0].t[c, bi][:, 0:bw], (s5g[1][c][bi],)))
                    for oc in range(GWc):
                        p = k.ps()
                        for c in range(GWc):
                            k.mm(p[:, 0:bw], gw[:, c, oc * 128:(oc + 1) * 128], gi[:, c, 0:bw], start=(c == 0), stop=(c == GWc - 1))
                        g_, o_ = gt[n % 2], og[n % 2]
                        n += 1
                        k.act(g_[:, 0:bw], p[:, 0:bw], AF.Sigmoid, bias=bcol[:, oc:oc + 1])
                        k.tt(o_[:, 0:bw], g_[:, 0:bw], gi[:, oc, 0:bw], ALU.mult)
                        k.dma("sp", fmv("mix", GWc + oc, bi, 128, bw), o_[:, 0:bw])

        def xv(ti):
            return V(xres.t[ti * 128:(ti + 1) * 128, :], (xres_t[ti],))

        def stage_wout(l, blocks):
            wv = I["w_out"].t[l].rearrange("(c p) n -> p c n", p=128)
            with k.scope() as ls:
                mb = k.sb([128, KC, 512], BF16, "mb", ls)
                xt = [k.sb([128, D], F32, "xt", ls) for _ in range(4)]
                wt = [k.sb([128, KC, 256], BF16, "wt", ls) for _ in range(3)]
                G1 = k.sb([128, D], F32, "G1", ls)
                tmp = [k.sb([128, 256], F32, "tmp", ls) for _ in range(2)]
                cur_r = None
                wi = 0
                n = 0
                for bi in blocks:
                    t0, bw, isc = cfg.blocks[bi]
                    r = 1 if isc else 0
                    if r != cur_r:
                        k.dma("sp", G1[:, :], modbc[l][r][2][:, :])
                        cur_r = r
                    for c in range(KC):
                        k.dma("sp", mb[:, c, 0:bw], fmv("mix", c, bi, 128, bw))
                    ns = bw // 128
                    for sub in range(ns):
                        k.dma("sp", xt[sub][:, :], xv(t0 // 128 + sub))
                    for ct in range(D // 256):
                        w = wt[wi % 3]
                        wi += 1
                        k.dma("pool", w[:, :, :], V(wv[:, :, ct * 256:(ct + 1) * 256], (I["w_out"],)))
                        for sub in range(ns):
                            p = k.ps()
                            for c in range(KC):
                                k.mm(p[:, 0:256], mb[:, c, sub * 128:(sub + 1) * 128], w[:, c, :], start=(c == 0), stop=(c == KC - 1))
                            tm = tmp[n % 2]
                            n += 1
                            k.tt(tm[:, :], p[:, 0:256], G1[:, ct * 256:(ct + 1) * 256], ALU.mult)
                            k.tt(xt[sub][:, ct * 256:(ct + 1) * 256], xt[sub][:, ct * 256:(ct + 1) * 256], tm[:, :], ALU.add, eng="pool")
                    for sub in range(ns):
                        k.dma("sp", xv(t0 // 128 + sub), xt[sub][:, :])

        def stage_moe(l, blocks):
            FC = cfg.FF // 128
            with k.scope() as ls:
                hb = k.sb([128, KC, 512], BF16, "hb", ls)
                acc = [k.sb([128, D], F32, "acc", ls) for _ in range(4)]
                wt = [k.sb([128, KC, 256], BF16, "wt", ls) for _ in range(2)]
                hid = k.sb([128, FC, 512], BF16, "hid", ls)
                wd = [k.sb([128, FC, 512], BF16, "wd", ls) for _ in range(2)]
                HC = min(2048, D)
                G2 = k.sb([128, HC], F32, "G2", ls)
                xh = k.sb([128, HC], F32, "xh", ls)
                sl = [k.sb([128, 512], F32, "sl", ls) for _ in range(2)]
                dws = k.sb([128, 4, 16], F32, "dws", ls)
                wi = 0
                di = 0
                n = 0
                for bi in blocks:
                    t0, bw, isc = cfg.blocks[bi]
                    r = 1 if isc else 0
                    ns = bw // 128
                    k.dma("sp", hb[:, :, 0:bw], V(hT.t[bi][:, :, 0:bw], (hT_b[bi],)))
                    for sub in range(ns):
                        ti = t0 // 128 + sub
                        k.dma("sp", dws[:, sub, :], V(dwd.t[ti * 128:(ti + 1) * 128, :], (dwd_t[ti],)))
                    for e in range(16):
                        wgv = I["moe_w_gate"].t[l, e].rearrange("(c p) f -> p c f", p=128)
                        wuv = I["moe_w_up"].t[l, e].rearrange("(c p) f -> p c f", p=128)
                        wdv = I["moe_w_down"].t[l, e].rearrange("(c p) n -> p c n", p=128)
                        for f0 in range(0, cfg.FF, 256):
                            fw = min(256, cfg.FF - f0)
                            wg_ = wt[wi % 2]
                            wu_ = wt[(wi + 1) % 2]
                            wi += 2
                            k.dma("pool", wg_[:, :, 0:fw], V(wgv[:, :, f0:f0 + fw], (I["moe_w_gate"],)))
                            k.dma("pool", wu_[:, :, 0:fw], V(wuv[:, :, f0:f0 + fw], (I["moe_w_up"],)))
                            for j0 in range(0, fw, 128):
                                fc = (f0 + j0) // 128
                                pg = k.ps()
                                for c in range(KC):
                                    k.mm(pg[:, 0:bw], wg_[:, c, j0:j0 + 128], hb[:, c, 0:bw], start=(c == 0), stop=(c == KC - 1))
                                pu = k.ps()
                                for c in range(KC):
                                    k.mm(pu[:, 0:bw], wu_[:, c, j0:j0 + 128], hb[:, c, 0:bw], start=(c == 0), stop=(c == KC - 1))
                                s_ = sl[n % 2]
                                n += 1
                                k.act(s_[:, 0:bw], pg[:, 0:bw], AF.Silu)
                                k.tt(hid[:, fc, 0:bw], pu[:, 0:bw], s_[:, 0:bw], ALU.mult)
                        for ct in range(D // 512):
                            w = wd[di % 2]
                            di += 1
                            k.dma("pool", w[:, :, :], V(wdv[:, :, ct * 512:(ct + 1) * 512], (I["moe_w_down"],)))
                            for sub in range(ns):
                                p = k.ps()
                                for fc in range(FC):
                                    k.mm(p[:, :], hid[:, fc, sub * 128:(sub + 1) * 128], w[:, fc, :], start=(fc == 0), stop=(fc == FC - 1))
                                av = acc[sub][:, ct * 512:(ct + 1) * 512]
                                if e == 0:
                                    k.ts(av, p[:, :], dws[:, sub, e:e + 1], ALU.mult)
                                else:
                                    k.stt(av, p[:, :], dws[:, sub, e:e + 1], av, ALU.mult, ALU.add)
                    for hc in range(0, D, HC):
                        k.dma("sp", G2[:, :], modbc[l][r][5][:, hc:hc + HC])
                        for sub in range(ns):
                            ti = t0 // 128 + sub
                            k.dma("sp", xh[:, :], V(xres.t[ti * 128:(ti + 1) * 128, hc:hc + HC], (xres_t[ti],)))
                            k.tt(acc[sub][:, hc:hc + HC], acc[sub][:, hc:hc + HC], G2[:, :], ALU.mult, eng="pool")
                            k.tt(xh[:, :], xh[:, :], acc[sub][:, hc:hc + HC], ALU.add)
                            k.dma("sp", V(xres.t[ti * 128:(ti + 1) * 128, hc:hc + HC], (xres_t[ti],)), xh[:, :])

        def stage_final():
            with k.scope() as ls:
                gb_ = k.sb([128, D], F32, "gfin", ls)
                grow = k.sb([1, D], F32, "grow", ls)
                xt = [k.sb([128, D], F32, "xt", ls) for _ in range(2)]
                junk = k.sb([128, D], BF16, "junk", ls)
                sm = k.sb([128, 4], F32, "sm", ls)
                k.dma("sp", grow[:, :], I["final_norm"][0:1, :])
                for ct in range(D // 512):
                    p = k.ps()
                    k.mm(p[:, :], ones[0:1, :], grow[0:1, ct * 512:(ct + 1) * 512])
                    k.copy(gb_[:, ct * 512:(ct + 1) * 512], p[:, :])
                for ti in range(N // 128):
                    x = xt[ti % 2]
                    k.dma("sp", x[:, :], xv(ti))
                    k.act(junk[:, :], x[:, :], AF.Square, accum=sm[:, 0:1])
                    k.rstd(sm[:, 1:2], sm[:, 0:1], D, sm[:, 2:3])
                    k.stt(x[:, :], x[:, :], sm[:, 1:2], gb_[:, :], ALU.mult, ALU.mult)
                    k.dma("sp", OUT[ti * 128:(ti + 1) * 128, :], x[:, :])

        def run_all():
            stage_ada()
            allb = list(range(cfg.NTB))
            for l in range(L):
                need_ctx = l < L - 1
                ob = [bi for bi in allb if (need_ctx or not cfg.blocks[bi][2])]
                stage_norm(l, 0, allb, False)
                stage_proj(l)
                stage_diff(l, need_ctx)
                stage_s5(l, need_ctx)
                stage_mla(l, need_ctx)
                stage_ret(l, need_ctx)
                stage_wout(l, ob)
                stage_norm(l, 1, ob, True)
                stage_moe(l, ob)
            stage_final()

        def dbg_out(name, d, subs, dt):
            shape = list(d.t.shape)
            o = Buf(nc.dram_tensor("o_" + name, shape, dt, kind="ExternalOutput"), "o_" + name)
            idx = tuple(slice(None) for _ in shape)
            k.dma("sp", V(o.t[idx], (o,)), V(d.t[idx], tuple(subs)))
            return o

        outs = [OUT]
        run_all()
        k.finish(outs)
    return nc


def host_consts(cfg):
    ident = np.eye(128, dtype=np.float32)
    R = np.zeros((64, 64), np.float32)
    for i in range(16):
        R[i + 16, i] = -1.0
        R[i, i + 16] = 1.0
        R[i + 48, i + 32] = -1.0
        R[i + 32, i + 48] = 1.0
    rp = np.zeros((128, 128), np.float32)
    rp[:64, :64] = R
    rp[64:, 64:] = R
    rows = cfg.N // 64
    row = np.repeat(np.arange(rows, dtype=np.float32), 64)
    col = np.tile(np.arange(64, dtype=np.float32), rows)
    inv = (10000.0 ** (-np.arange(16, dtype=np.float32) / 16)).astype(np.float32)
    ar = row[:, None] * inv
    ac = col[:, None] * inv
    ang = np.concatenate([ar, ar, ac, ac], axis=-1)
    cs = np.stack([np.cos(ang).T, np.sin(ang).T]).astype(np.float32)
    cs = np.concatenate([cs, cs], axis=1)
    return {"ident": ident, "rperm": rp, "ropecs": np.ascontiguousarray(cs)}


def make_in_maps(cfg, inp, ncores):
    L = cfg.DEPTH
    f = lambda a: np.ascontiguousarray(np.asarray(a, dtype=np.float32))
    shared = {
        "c_ctx": f(inp["c_ctx"]).reshape(cfg.KC, 128),
        "ada_w": f(inp["ada_w"]), "ada_b": f(inp["ada_b"]),
        "norm_mix": f(inp["norm_mix"]), "norm_ffn": f(inp["norm_ffn"]),
        "w_in": f(inp["w_in"]), "w_out": f(inp["w_out"]),
        "diff_lambda": f(inp["diff_lambda"]).reshape(L, 256), "diff_subln": f(inp["diff_subln"]),
        "s5_a_re": f(inp["s5_a_re"]).reshape(L, 2, -1), "s5_a_im": f(inp["s5_a_im"]).reshape(L, 2, -1),
        "s5_log_dt": f(inp["s5_log_dt"]),
        "s5_b_re": f(inp["s5_b_re"]).reshape(L, 2, -1, 16), "s5_b_im": f(inp["s5_b_im"]).reshape(L, 2, -1, 16),
        "s5_c_re": f(inp["s5_c_re"]).reshape(L, 2, -1, 64), "s5_c_im": f(inp["s5_c_im"]).reshape(L, 2, -1, 64),
        "s5_d": f(inp["s5_d"]).reshape(L, -1), "s5_glu_w": f(inp["s5_glu_w"]), "s5_glu_b": f(inp["s5_glu_b"]),
        "mla_q_norm": f(inp["mla_q_norm"]), "mla_kv_norm": f(inp["mla_kv_norm"]),
        "mla_w_uq": f(inp["mla_w_uq"]), "mla_w_ukv": f(inp["mla_w_ukv"]),
        "ret_decay": f(inp["ret_decay"]).reshape(L, -1), "ret_norm": f(inp["ret_norm"]),
        "moe_wr": np.ascontiguousarray(np.concatenate([f(inp["moe_wg"]), f(inp["moe_we"])], axis=-1)),
        "moe_br": np.ascontiguousarray(np.concatenate([f(inp["moe_bg"]), f(inp["moe_be"])], axis=-1)),
        "moe_w_gate": f(inp["moe_w_gate"]), "moe_w_up": f(inp["moe_w_up"]), "moe_w_down": f(inp["moe_w_down"]),
        "final_norm": f(inp["final_norm"]).reshape(1, -1),
    }
    shared.update(host_consts(cfg))
    maps = []
    for b in range(ncores):
        m = dict(shared)
        m["x"] = f(inp["x"][b])
        m["ctx"] = f(inp["ctx"][b])
        m["c"] = f(inp["c"][b]).reshape(cfg.KC, 128)
        maps.append(m)
    return maps


def kernel(**inputs):
    x = np.asarray(inputs["x"])
    B, N, D = x.shape
    cfg = Cfg(D=D, N=N, LC=np.asarray(inputs["ctx"]).shape[1], DEPTH=np.asarray(inputs["ada_w"]).shape[0], B=B)
    nc = build_program(cfg)
    maps = make_in_maps(cfg, inputs, B)
    res = run_bass_kernel_spmd(nc, maps, core_ids=list(range(B)))
    return np.stack([np.asarray(r["out"]) for r in res.results]).astype(np.float32)
```

```python
import math
import os
from contextlib import ExitStack
import numpy as np
import concourse.bass as bass
import concourse.mybir as mybir
from concourse.bass_utils import run_bass_kernel_spmd

F32 = mybir.dt.float32
BF16 = mybir.dt.bfloat16
AF = mybir.ActivationFunctionType
ALU = mybir.AluOpType
AX = mybir.AxisListType
NORM_EPS = 1e-6


class Cfg:
    def __init__(s, D=4096, N=4096, LC=256, DEPTH=2, B=4):
        s.D, s.N, s.LC, s.DEPTH, s.B = D, N, LC, DEPTH, B
        s.T = N + LC
        s.GW = D // 4
        s.DH = 64
        s.DIFF_HEADS = s.GW // 128
        s.S5_CH, s.S5_P = 16, 64
        s.S5_G = s.GW // 16
        s.MLA_HEADS = s.GW // 128
        s.QR = 3 * D // 16
        s.KVR = D // 16
        s.RET_HEADS = s.GW // 128
        s.RQK = s.RET_HEADS * 64
        s.E, s.FF = 16, D // 4
        s.splits = [s.GW, s.GW, s.GW, s.GW, s.QR, s.KVR, 64, s.RQK, s.RQK, s.GW, s.GW]
        s.names = ["dq", "dk", "dv", "su", "cq", "ckv", "kr", "rq", "rk", "rv", "rg"]
        s.off = {}
        o = 0
        for n, w in zip(s.names, s.splits):
            s.off[n] = (o, w)
            o += w
        s.INW = o
        s.KC = D // 128
        s.TB = 512
        s.blocks = [(i * 512, 512, False) for i in range(N // 512)]
        c0 = N
        while c0 < s.T:
            w = min(512, s.T - c0)
            s.blocks.append((c0, w, True))
            c0 += w
        s.NTB = len(s.blocks)
        s.KT = s.T // 128


class Buf:
    __slots__ = ("t", "w", "r", "name")

    def __init__(s, t, name=""):
        s.t, s.w, s.r, s.name = t, None, {}, name

    def __getitem__(s, idx):
        return V(s.t[idx], (s,))

    def sub(s):
        return Buf(s.t, s.name)


class V:
    __slots__ = ("ap", "bufs")

    def __init__(s, ap, bufs):
        s.ap, s.bufs = ap, bufs

    def __getitem__(s, idx):
        return V(s.ap[idx], s.bufs)

    def rearrange(s, pat, **kw):
        return V(s.ap.rearrange(pat, **kw), s.bufs)


class K:
    ND = 12

    def __init__(s, nc, st):
        s.nc, s.st = nc, st
        s.E = {"pe": nc.tensor, "act": nc.scalar, "dve": nc.vector, "pool": nc.gpsimd, "sp": nc.sync}
        s.esem = {e: st.enter_context(nc.semaphore("es_" + e)) for e in ("pe", "act", "dve", "pool")}
        s.cnt = {e: 0 for e in s.esem}
        s.known = {e: {} for e in s.E}
        s.dsl = {q: [[st.enter_context(nc.semaphore("ds_%s%d" % (q, i))), 0] for i in range(s.ND)] for q in ("sp", "pool", "act")}
        s.dnext = {"sp": 0, "pool": 0, "act": 0}
        s.nbuf = 0
        s.psb = None
        s.psi = 0
        s.held = []

    def sb(s, shape, dt=F32, name=None, stack=None):
        s.nbuf += 1
        nm = "%s_%d" % (name or "b", s.nbuf)
        t = (stack or s.st).enter_context(s.nc.sbuf_tensor(nm, list(shape), dt))
        return Buf(t, nm)

    def dram(s, shape, dt=F32, name=None):
        s.nbuf += 1
        nm = "%s_%d" % (name or "d", s.nbuf)
        return Buf(s.nc.dram_tensor(nm, list(shape), dt), nm)

    def init_psum(s):
        s.psb = [Buf(s.st.enter_context(s.nc.psum_tensor("ps%d" % i, [128, 512], F32)), "ps%d" % i) for i in range(8)]

    def ps(s, hold=False):
        while True:
            b = s.psb[s.psi]
            s.psi = (s.psi + 1) % 8
            if b not in s.held:
                break
        if hold:
            s.held.append(b)
        return b

    def release(s, b):
        s.held.remove(b)

    def _toks(s, reads, writes):
        toks = []
        for v in reads:
            for b in v.bufs:
                if b.w is not None:
                    toks.append(b.w)
        for v in writes:
            for b in v.bufs:
                if b.w is not None:
                    toks.append(b.w)
                toks.extend(b.r.values())
        return toks

    def _wait(s, eng, toks):
        kn = s.known[eng]
        for tok in toks:
            key = tok[0]
            val = tok[1]
            if key == eng and eng == "pe":
                continue
            if kn.get(key, 0) >= val:
                continue
            sem = s.esem[key] if isinstance(key, str) else s.dsl[key[0]][key[1]][0]
            s.E[eng].wait_ge(sem, val)
            kn[key] = val

    def _mark(s, tok, rkey, reads, writes):
        for v in writes:
            for b in v.bufs:
                b.w = tok
                b.r = {}
        for v in reads:
            for b in v.bufs:
                if b.w is tok:
                    continue
                b.r[rkey] = tok

    def op(s, eng, fn, reads, writes):
        reads = [v for v in reads if isinstance(v, V)]
        s._wait(eng, s._toks(reads, writes))
        ins = fn(s.E[eng])
        s.cnt[eng] += 1
        ins.then_inc(s.esem[eng], 1)
        tok = (eng, s.cnt[eng])
        s._mark(tok, eng, reads, writes)

    def dma(s, q, out, in_):
        si = s.dnext[q]
        s.dnext[q] = (si + 1) % s.ND
        slot = s.dsl[q][si]
        key = (q, si)
        toks = s._toks([in_], [out])
        if slot[1] > 0:
            toks.append((key, slot[1]))
        s._wait(q, toks)
        ins = s.E[q].dma_start(out=out.ap, in_=in_.ap)
        slot[1] += 16
        ins.then_inc(slot[0], 16)
        tok = (key, slot[1])
        s._mark(tok, key, [in_], [out])

    def barrier(s):
        toks = [(e, c) for e, c in s.cnt.items() if c > 0]
        for q in s.dsl:
            for i, (sem, val) in enumerate(s.dsl[q]):
                if val > 0:
                    toks.append(((q, i), val))
        for eng in s.E:
            s._wait(eng, toks)

    def scope(s):
        k = s

        class _Scope(ExitStack):
            def __exit__(self, *a):
                if a[0] is None:
                    k.barrier()
                return super().__exit__(*a)

        return _Scope()

    def finish(s, outs):
        toks = []
        for b in outs:
            if b.w is not None:
                toks.append(b.w)
        s._wait("sp", toks)

    @staticmethod
    def _a(x):
        return x.ap if isinstance(x, V) else x

    def mm(s, out, lhsT, rhs, start=True, stop=True):
        s.op("pe", lambda e: e.matmul(out.ap, lhsT.ap, rhs.ap, start=start, stop=stop), [lhsT, rhs] + ([] if start else [out]), [out])

    def tr(s, out, in_, ident):
        s.op("pe", lambda e: e.transpose(out.ap, in_.ap, ident.ap), [in_, ident], [out])

    def act(s, out, in_, func, bias=0.0, scale=1.0, accum=None):
        kw = {}
        if accum is not None:
            kw["accum_out"] = accum.ap
        s.op("act", lambda e: e.activation(out=out.ap, in_=in_.ap, func=func, bias=s._a(bias), scale=s._a(scale), **kw),
             [in_, bias, scale], [out] + ([accum] if accum is not None else []))

    def tt(s, out, a, b, op, eng="dve"):
        s.op(eng, lambda e: e.tensor_tensor(out=out.ap, in0=a.ap, in1=b.ap, op=op), [a, b], [out])

    def ts(s, out, a, s1, op0, s2=None, op1=None, eng="dve", accum=None):
        kw = {}
        if accum is not None:
            kw["accum_out"] = accum.ap
        if op1 is None:
            s.op(eng, lambda e: e.tensor_scalar(out=out.ap, in0=a.ap, scalar1=s._a(s1), scalar2=None, op0=op0, **kw), [a, s1], [out])
        else:
            s.op(eng, lambda e: e.tensor_scalar(out=out.ap, in0=a.ap, scalar1=s._a(s1), scalar2=s._a(s2), op0=op0, op1=op1, **kw),
                 [a, s1, s2], [out] + ([accum] if accum is not None else []))

    def stt(s, out, a, sc, b, op0, op1):
        s.op("dve", lambda e: e.scalar_tensor_tensor(out=out.ap, in0=a.ap, scalar=s._a(sc), in1=b.ap, op0=op0, op1=op1), [a, sc, b], [out])

    def copy(s, out, in_, eng="dve"):
        if eng == "act":
            s.op("act", lambda e: e.copy(out=out.ap, in_=in_.ap), [in_], [out])
        else:
            s.op(eng, lambda e: e.tensor_copy(out=out.ap, in_=in_.ap), [in_], [out])

    def memset(s, out, val, eng="dve"):
        s.op(eng, lambda e: e.memset(out.ap, val), [], [out])

    def recip(s, out, in_):
        s.op("dve", lambda e: e.reciprocal(out=out.ap, in_=in_.ap), [in_], [out])

    def scan(s, out, d0, d1, init, op0=ALU.mult, op1=ALU.add):
        s.op("dve", lambda e: e.tensor_tensor_scan(out.ap, d0.ap, d1.ap, s._a(init), op0, op1), [d0, d1, init], [out])

    def rmax(s, out, in_):
        s.op("dve", lambda e: e.reduce_max(out=out.ap, in_=in_.ap, axis=AX.X), [in_], [out])

    def rsum(s, out, in_):
        s.op("dve", lambda e: e.reduce_sum(out=out.ap, in_=in_.ap, axis=AX.X), [in_], [out])

    def rstd(s, out, ss, n, tmp):
        s.ts(tmp, ss, 1.0 / n, ALU.mult, NORM_EPS, ALU.add)
        s.act(tmp, tmp, AF.Sqrt)
        s.recip(out, tmp)


def chunks(n, c=128):
    return [(i, min(c, n - i)) for i in range(0, n, c)]


def build_program(cfg, debug=()):
    nc = bass.Bass("TRN2", target_bir_lowering=False)
    D, N, LC, T, KC = cfg.D, cfg.N, cfg.LC, cfg.T, cfg.KC
    L = cfg.DEPTH
    dbg = {}

    def ext(name, shape, dt=F32):
        return Buf(nc.dram_tensor(name, list(shape), dt, kind="ExternalInput"), name)

    I = {}
    I["x"] = ext("x", [N, D])
    I["ctx"] = ext("ctx", [LC, D])
    I["c"] = ext("c", [KC, 128])
    I["c_ctx"] = ext("c_ctx", [KC, 128])
    I["ada_w"] = ext("ada_w", [L, D, 6 * D])
    I["ada_b"] = ext("ada_b", [L, 6 * D])
    I["norm_mix"] = ext("norm_mix", [L, D])
    I["norm_ffn"] = ext("norm_ffn", [L, D])
    I["w_in"] = ext("w_in", [L, D, cfg.INW])
    I["w_out"] = ext("w_out", [L, D, D])
    I["diff_lambda"] = ext("diff_lambda", [L, 256])
    I["diff_subln"] = ext("diff_subln", [L, 128])
    I["s5_a_re"] = ext("s5_a_re", [L, 2, cfg.S5_G * 64])
    I["s5_a_im"] = ext("s5_a_im", [L, 2, cfg.S5_G * 64])
    I["s5_log_dt"] = ext("s5_log_dt", [L, 2, cfg.S5_G])
    I["s5_b_re"] = ext("s5_b_re", [L, 2, cfg.S5_G * 64, 16])
    I["s5_b_im"] = ext("s5_b_im", [L, 2, cfg.S5_G * 64, 16])
    I["s5_c_re"] = ext("s5_c_re", [L, 2, cfg.S5_G * 16, 64])
    I["s5_c_im"] = ext("s5_c_im", [L, 2, cfg.S5_G * 16, 64])
    I["s5_d"] = ext("s5_d", [L, cfg.GW])
    I["s5_glu_w"] = ext("s5_glu_w", [L, cfg.GW, cfg.GW])
    I["s5_glu_b"] = ext("s5_glu_b", [L, cfg.GW])
    I["mla_q_norm"] = ext("mla_q_norm", [L, cfg.QR])
    I["mla_kv_norm"] = ext("mla_kv_norm", [L, cfg.KVR])
    I["mla_w_uq"] = ext("mla_w_uq", [L, cfg.QR, cfg.MLA_HEADS * 192])
    I["mla_w_ukv"] = ext("mla_w_ukv", [L, cfg.KVR, cfg.MLA_HEADS * 256])
    I["ret_decay"] = ext("ret_decay", [L, 2 * cfg.RET_HEADS])
    I["ret_norm"] = ext("ret_norm", [L, cfg.GW])
    I["moe_wr"] = ext("moe_wr", [L, D, 20])
    I["moe_br"] = ext("moe_br", [L, 20])
    I["moe_w_gate"] = ext("moe_w_gate", [L, 16, D, cfg.FF])
    I["moe_w_up"] = ext("moe_w_up", [L, 16, D, cfg.FF])
    I["moe_w_down"] = ext("moe_w_down", [L, 16, cfg.FF, D])
    I["final_norm"] = ext("final_norm", [1, D])
    I["ident"] = ext("ident", [128, 128])
    I["rperm"] = ext("rperm", [128, 128])
    I["ropecs"] = ext("ropecs", [2, 128, N])
    OUT = Buf(nc.dram_tensor("out", [N, D], F32, kind="ExternalOutput"), "out")

    with ExitStack() as st:
        k = K(nc, st)
        k.init_psum()
        ident = k.sb([128, 128], F32, "ident")
        identb = k.sb([128, 128], BF16, "identb")
        ones = k.sb([128, 128], F32, "ones")
        onesb = k.sb([128, 128], BF16, "onesb")
        rperm = k.sb([128, 128], BF16, "rperm")
        k.dma("sp", ident[:, :], I["ident"][:, :])
        k.dma("pool", identb[:, :], I["ident"][:, :])
        k.dma("pool", rperm[:, :], I["rperm"][:, :])
        k.memset(ones[:, :], 1.0)
        k.memset(onesb[:, :], 1.0)

        xres = k.dram([T, D], F32, "xres")
        xres_t = [xres.sub() for _ in range(T // 128)]
        modbc = [[[k.dram([128, D], F32, "modbc") for j in range(6)] for r in range(2)] for l in range(L)]
        hT = k.dram([cfg.NTB, 128, KC, 512], BF16, "hT")
        hT_b = [hT.sub() for _ in range(cfg.NTB)]
        dwd = k.dram([T, 16], F32, "dw")
        dwd_t = [dwd.sub() for _ in range(T // 128)]

        for i in range(N // 128):
            k.dma("sp", V(xres.t[i * 128:(i + 1) * 128, :], (xres_t[i],)), I["x"][i * 128:(i + 1) * 128, :])
        for i in range(LC // 128):
            j = N // 128 + i
            k.dma("sp", V(xres.t[j * 128:(j + 1) * 128, :], (xres_t[j],)), I["ctx"][i * 128:(i + 1) * 128, :])

        def stage_ada():
            with k.scope() as ls:
                rep = [k.sb([128, KC, 128], F32, "rep", ls) for r in range(2)]
                crow = k.sb([KC, 128], F32, "crow", ls)
                colv = k.sb([128, KC], F32, "colv", ls)
                for r, src in enumerate((I["c"], I["c_ctx"])):
                    k.dma("sp", crow[:, :], src[:, :])
                    k.act(crow[:, :], crow[:, :], AF.Silu)
                    p = k.ps()
                    k.tr(p[:, 0:KC], crow[:, :], ident[0:KC, 0:KC])
                    k.copy(colv[:, :], p[:, 0:KC])
                    for c in range(KC):
                        k.ts(rep[r][:, c, :], ones[:, :], colv[:, c:c + 1], ALU.mult, eng=("dve" if c % 2 else "pool"))
                KB = min(8, KC)
                wt = [k.sb([128, KB, 512], F32, "adaw", ls) for _ in range(3)]
                brow = [k.sb([1, 512], F32, "brow", ls) for _ in range(2)]
                grow = [k.sb([1, 512], F32, "grow", ls) for _ in range(2)]
                stg = [k.sb([128, 512], F32, "stg", ls) for _ in range(2)]
                gbc = k.sb([128, 512], F32, "gbc", ls)
                wi = 0
                si = 0
                for l in range(L):
                    wv = I["ada_w"].t[l].rearrange("(c p) n -> p c n", p=128)
                    for ct in range(6 * D // 512):
                        j = (ct * 512) // D
                        col = ct * 512 - j * D
                        pp = [k.ps(), k.ps()]
                        br_ = brow[ct % 2]
                        k.dma("sp", br_[:, :], I["ada_b"][l:l + 1, ct * 512:(ct + 1) * 512])
                        for kb in range(0, KC, KB):
                            w = wt[wi % 3]
                            wi += 1
                            k.dma("sp", w[:, :, :], V(wv[:, kb:kb + KB, ct * 512:(ct + 1) * 512], (I["ada_w"],)))
                            for c in range(KB):
                                for r in range(2):
                                    k.mm(pp[r][:, :], rep[r][:, kb + c, :], w[:, c, :], start=(kb + c == 0), stop=False)
                        for r in range(2):
                            k.mm(pp[r][:, :], ones[0:1, :], br_[0:1, :], start=False, stop=True)
                        if j in (1, 4):
                            pg = k.ps()
                            gr_ = grow[ct % 2]
                            k.dma("sp", gr_[:, :], (I["norm_mix"] if j == 1 else I["norm_ffn"])[l:l + 1, col:col + 512])
                            k.mm(pg[:, :], ones[0:1, :], gr_[0:1, :])
                            k.copy(gbc[:, :], pg[:, :], eng="act")
                        for r in range(2):
                            sg = stg[si % 2]
                            si += 1
                            if j in (1, 4):
                                k.stt(sg[:, :], pp[r][:, :], 1.0, gbc[:, :], ALU.add, ALU.mult)
                            else:
                                k.copy(sg[:, :], pp[r][:, :], eng="act")
                            k.dma("sp", modbc[l][r][j][:, col:col + 512], sg[:, :])

        def stage_norm(l, which, blocks, router):
            jS, jA = (0, 1) if which == 0 else (3, 4)
            with k.scope() as ls:
                A = k.sb([128, D], F32, "A", ls)
                S = k.sb([128, D], F32, "S", ls)
                xt = [k.sb([128, D], F32, "xt", ls) for _ in range(2)]
                junk = k.sb([128, D], BF16, "junk", ls)
                hb = [k.sb([128, KC, 512], BF16, "hb", ls) for _ in range(2)]
                sm = k.sb([128, 8], F32, "sm", ls)
                if router:
                    hf = k.sb([128, KC * 128], F32, "hf", ls)
                    wr = k.sb([128, KC, 20], F32, "wr", ls)
                    brr = k.sb([1, 20], F32, "brr", ls)
                    k.dma("sp", wr[:, :, :], V(I["moe_wr"].t[l].rearrange("(c p) n -> p c n", p=128), (I["moe_wr"],)))
                    k.dma("sp", brr[:, :], I["moe_br"][l:l + 1, :])
                    rt = k.sb([128, 64], F32, "rt", ls)
                    dwt = k.sb([128, 16], F32, "dwt", ls)
                cur_r = None
                xi = 0
                for bi in blocks:
                    t0, bw, isc = cfg.blocks[bi]
                    r = 1 if isc else 0
                    if r != cur_r:
                        k.dma("sp", A[:, :], modbc[l][r][jA][:, :])
                        k.dma("sp", S[:, :], modbc[l][r][jS][:, :])
                        cur_r = r
                    hbb = hb[bi % 2]
                    for sub in range(bw // 128):
                        ti = (t0 + sub * 128) // 128
                        x = xt[xi % 2]
                        xi += 1
                        k.dma("sp", x[:, :], V(xres.t[ti * 128:(ti + 1) * 128, :], (xres_t[ti],)))
                        k.act(junk[:, :], x[:, :], AF.Square, accum=sm[:, 0:1])
                        k.rstd(sm[:, 1:2], sm[:, 0:1], D, sm[:, 2:3])
                        k.stt(x[:, :], x[:, :], sm[:, 1:2], A[:, :], ALU.mult, ALU.mult)
                        k.tt(x[:, :], x[:, :], S[:, :], ALU.add, eng="pool")
                        for c4 in range(0, KC, 4):
                            p = k.ps()
                            for c in range(c4, min(c4 + 4, KC)):
                                k.tr(p[:, (c - c4) * 128:(c - c4 + 1) * 128], x[:, c * 128:(c + 1) * 128], ident[:, :])
                            nn = min(4, KC - c4)
                            pv = p[:, 0:nn * 128].rearrange("p (c t) -> p c t", c=nn)
                            k.copy(hbb[:, c4:c4 + nn, sub * 128:(sub + 1) * 128], pv, eng=("dve" if router or (c4 // 4) % 2 else "act"))
                            if router:
                                k.ts(hf[:, c4 * 128:(c4 + nn) * 128], p[:, 0:nn * 128], 1.0, ALU.mult)
                        if router:
                            pr = k.ps()
                            for c in range(KC):
                                k.mm(pr[:, 0:20], hf[:, c * 128:(c + 1) * 128], wr[:, c, :], start=(c == 0), stop=False)
                            k.mm(pr[:, 0:20], ones[0:1, :], brr[0:1, :], start=False, stop=True)
                            lg = rt[:, 0:20]
                            k.copy(lg, pr[:, 0:20])
                            gmx = rt[:, 20:21]
                            k.rmax(gmx, rt[:, 0:4])
                            k.ts(rt[:, 24:28], rt[:, 0:4], gmx, ALU.subtract)
                            k.act(rt[:, 28:32], rt[:, 24:28], AF.Exp, accum=rt[:, 21:22])
                            k.recip(rt[:, 22:23], rt[:, 21:22])
                            k.ts(rt[:, 24:28], rt[:, 24:28], 0.0, ALU.is_ge)
                            k.ts(rt[:, 28:32], rt[:, 24:28], 1.0, ALU.subtract, 1e30, ALU.mult)
                            for g in range(4):
                                k.ts(rt[:, 32 + 4 * g:36 + 4 * g], rt[:, 4 + 4 * g:8 + 4 * g], rt[:, 28 + g:29 + g], ALU.add)
                            em = rt[:, 32:48]
                            k.rmax(rt[:, 48:49], em)
                            k.ts(dwt[:, :], em, rt[:, 48:49], ALU.is_ge)
                            k.stt(rt[:, 4:20], dwt[:, :], -1e30, em, ALU.mult, ALU.add)
                            k.rmax(rt[:, 49:50], rt[:, 4:20])
                            k.ts(rt[:, 32:48], rt[:, 4:20], rt[:, 49:50], ALU.is_ge)
                            k.tt(rt[:, 50:51], rt[:, 49:50], rt[:, 48:49], ALU.subtract)
                            k.act(rt[:, 51:52], rt[:, 50:51], AF.Exp)
                            k.ts(rt[:, 52:53], rt[:, 51:52], 1.0, ALU.add)
                            k.recip(rt[:, 52:53], rt[:, 52:53])
                            k.tt(rt[:, 53:54], rt[:, 52:53], rt[:, 22:23], ALU.mult)
                            k.tt(rt[:, 54:55], rt[:, 22:23], rt[:, 53:54], ALU.subtract)
                            k.ts(dwt[:, :], dwt[:, :], rt[:, 53:54], ALU.mult)
                            k.stt(dwt[:, :], rt[:, 32:48], rt[:, 54:55], dwt[:, :], ALU.mult, ALU.add)
                            k.dma("sp", V(dwd.t[ti * 128:(ti + 1) * 128, :], (dwd_t[ti],)), dwt[:, :])
                    k.dma("sp", V(hT.t[bi][:, :, 0:bw], (hT_b[bi],)), hbb[:, :, 0:bw])

        def wtiles_in():
            tl = []
            for nm in cfg.names:
                g0, gw = cfg.off[nm]
                for c0 in range(0, gw, 256):
                    tl.append((nm, c0, g0 + c0, min(256, gw - c0)))
            return tl

        WIN_T = wtiles_in()
        WIN_IDX = {(nm, c0): i for i, (nm, c0, a, w) in enumerate(WIN_T)}
        FC_ = cfg.FF // 128
        NFT = (cfg.FF + 255) // 256
        winb, woutb, wgb, wub, wdb = [], [], [], [], []
        for l in range(L):
            d = k.dram([len(WIN_T), 128, KC, 256], BF16, "winb")
            winb.append((d, [d.sub() for _ in WIN_T]))
            d = k.dram([D // 256, 128, KC, 256], BF16, "woutb")
            woutb.append((d, [d.sub() for _ in range(D // 256)]))
            d = k.dram([16, NFT, 128, KC, 256], BF16, "wgb")
            wgb.append((d, [[d.sub() for _ in range(NFT)] for _ in range(16)]))
            d = k.dram([16, NFT, 128, KC, 256], BF16, "wub")
            wub.append((d, [[d.sub() for _ in range(NFT)] for _ in range(16)]))
            d = k.dram([16, D // 512, 128, FC_, 512], BF16, "wdb")
            wdb.append((d, [[d.sub() for _ in range(D // 512)] for _ in range(16)]))

        def convert_weights(l):
            wv = I["w_in"].t[l].rearrange("(c p) n -> p c n", p=128)
            for i, (nm, c0, a0, w) in enumerate(WIN_T):
                k.dma("pool", V(winb[l][0].t[i][:, :, 0:w], (winb[l][1][i],)), V(wv[:, :, a0:a0 + w], (I["w_in"],)))
            wv = I["w_out"].t[l].rearrange("(c p) n -> p c n", p=128)
            for ct in range(D // 256):
                k.dma("pool", V(woutb[l][0].t[ct], (woutb[l][1][ct],)), V(wv[:, :, ct * 256:(ct + 1) * 256], (I["w_out"],)))
            for e in range(16):
                wgv = I["moe_w_gate"].t[l, e].rearrange("(c p) f -> p c f", p=128)
                wuv = I["moe_w_up"].t[l, e].rearrange("(c p) f -> p c f", p=128)
                wdv = I["moe_w_down"].t[l, e].rearrange("(c p) n -> p c n", p=128)
                for j in range(NFT):
                    f0 = j * 256
                    fw = min(256, cfg.FF - f0)
                    k.dma("pool", V(wgb[l][0].t[e, j][:, :, 0:fw], (wgb[l][1][e][j],)), V(wgv[:, :, f0:f0 + fw], (I["moe_w_gate"],)))
                    k.dma("pool", V(wub[l][0].t[e, j][:, :, 0:fw], (wub[l][1][e][j],)), V(wuv[:, :, f0:f0 + fw], (I["moe_w_up"],)))
                for ct in range(D // 512):
                    k.dma("pool", V(wdb[l][0].t[e, ct], (wdb[l][1][e][ct],)), V(wdv[:, :, ct * 512:(ct + 1) * 512], (I["moe_w_down"],)))

        def fm_alloc(nrows, name, dt=BF16):
            nch = (nrows + 127) // 128
            d = k.dram([nch, cfg.NTB, 128, 512], dt, name)
            return d, [[d.sub() for _ in range(cfg.NTB)] for _ in range(nch)]

        FM = {}
        for nm in ("dq", "dk", "su", "cq", "ckv", "kr", "rq", "rk", "rg"):
            FM[nm] = fm_alloc(cfg.off[nm][1], "fm_" + nm)

        def fmv(nm, ch, tb, rows=128, w=512):
            d, subs = FM[nm]
            return V(d.t[ch, tb][0:rows, 0:w], (subs[ch][tb],))

        TMV = {}
        for nm, nh in (("dv", cfg.DIFF_HEADS), ("rv", cfg.RET_HEADS)):
            d = k.dram([nh, 128, cfg.KT, 128], BF16, "tm_" + nm)
            TMV[nm] = (d, [[d.sub() for _ in range(cfg.KT)] for _ in range(nh)])
        rq_bc = k.dram([cfg.NTB, 128, 512], F32, "rq_bc")
        rq_bc_s = [rq_bc.sub() for _ in range(cfg.NTB)]
        rkv_bc = k.dram([cfg.NTB, 128, 512], F32, "rkv_bc")
        rkv_bc_s = [rkv_bc.sub() for _ in range(cfg.NTB)]
        rkv_tm = k.dram([128, cfg.KT], F32, "rkv_tm")
        rkv_tm_s = [rkv_tm.sub() for _ in range(cfg.NTB)]

        def row_to_cols(dst, src_row, n, tmp_row):
            k.dma("sp", tmp_row[0:1, 0:n], src_row)
            p = k.ps()
            for i, (c0, cw) in enumerate(chunks(n)):
                k.tr(p[0:cw, i:i + 1], tmp_row[0:1, c0:c0 + cw], ident[0:1, 0:1])
                k.copy(dst[0:cw, i:i + 1], p[0:cw, i:i + 1])

        def stage_proj(l):
            wv = I["w_in"].t[l].rearrange("(c p) n -> p c n", p=128)
            csv = I["ropecs"].t.rearrange("a p n -> p a n")
            with k.scope() as ls:
                hb = [k.sb([128, KC, 512], BF16, "hb", ls) for _ in range(2)]
                wt = [k.sb([128, KC, 256], BF16, "wt", ls) for _ in range(3)]
                cs = [k.sb([128, 2, 512], F32, "cs", ls) for _ in range(2)]
                stg = [k.sb([128, 512], BF16, "stg", ls) for _ in range(3)]
                xs = [k.sb([128, 512], BF16, "xs", ls) for _ in range(2)]
                t1 = [k.sb([128, 512], F32, "t1", ls) for _ in range(2)]
                t2 = [k.sb([128, 512], F32, "t2", ls) for _ in range(2)]
                sqf = [k.sb([128, 512], F32, "sqf", ls) for _ in range(2)]
                rbc = k.sb([128, 512], F32, "rbc", ls)
                rtmp = k.sb([128, 512], F32, "rtmp", ls)
                qn = k.sb([128, 16], F32, "qn", ls)
                rtm = k.sb([128, 4], F32, "rtm", ls)
                trow = k.sb([1, max(cfg.QR, 128)], F32, "trow", ls)
                row_to_cols(qn[:, 0:8], I["mla_q_norm"][l:l + 1, :], cfg.QR, trow)
                row_to_cols(qn[:, 8:16], I["mla_kv_norm"][l:l + 1, :], cfg.KVR, trow)
                cnt = {"w": 0, "s": 0, "x": 0}

                def load_w(c0, w, nm_=None, c0rel=None):
                    t = wt[cnt["w"] % 3]
                    cnt["w"] += 1
                    i = WIN_IDX[(nm_, c0rel)]
                    k.dma("sp" if cnt["w"] % 2 else "pool", t[:, :, 0:w], V(winb[l][0].t[i][:, :, 0:w], (winb[l][1][i],)))
                    return t

                for bi, (t0, bw, isc) in enumerate(cfg.blocks):
                    h = hb[bi % 2]
                    k.dma("sp", h[:, :, 0:bw], V(hT.t[bi][:, :, 0:bw], (hT_b[bi],)))
                    c_ = cs[bi % 2]
                    if not isc:
                        k.dma("sp", c_[:, :, :], V(csv[:, :, t0:t0 + 512], (I["ropecs"],)))
                    for nm in cfg.names:
                        g0, gw = cfg.off[nm]
                        if nm in ("dv", "rv"):
                            d, subs = TMV[nm]
                            for c0 in range(0, gw, 256):
                                w = min(256, gw - c0)
                                t = load_w(g0 + c0, w, nm, c0)
                                for sub in range(bw // 128):
                                    p = k.ps()
                                    for c in range(KC):
                                        k.mm(p[:, 0:w], h[:, c, sub * 128:(sub + 1) * 128], t[:, c, 0:w], start=(c == 0), stop=(c == KC - 1))
                                    sg = stg[cnt["s"] % 3]
                                    cnt["s"] += 1
                                    k.copy(sg[:, 0:w], p[:, 0:w], eng=("act" if cnt["s"] % 2 else "dve"))
                                    kt = (t0 + sub * 128) // 128
                                    for hh in range(w // 128):
                                        head = (c0 + hh * 128) // 128
                                        k.dma("sp", V(d.t[head][:, kt, :], (subs[head][kt],)), sg[:, hh * 128:(hh + 1) * 128])
                            continue
                        nch = (gw + 127) // 128
                        pss = k.ps(hold=True) if nm in ("cq", "ckv") else None
                        for c0 in range(0, gw, 256):
                            w = min(256, gw - c0)
                            t = load_w(g0 + c0, w, nm, c0)
                            for j0 in range(0, w, 128):
                                cw = min(128, w - j0)
                                ch = (c0 + j0) // 128
                                p = k.ps()
                                for c in range(KC):
                                    k.mm(p[0:cw, 0:bw], t[:, c, j0:j0 + cw], h[:, c, 0:bw], start=(c == 0), stop=(c == KC - 1))
                                sg = stg[cnt["s"] % 3]
                                cnt["s"] += 1
                                if nm in ("dq", "dk", "kr", "rq", "rk") and not isc:
                                    x_ = xs[cnt["x"] % 2]
                                    a1 = t1[cnt["x"] % 2]
                                    a2 = t2[cnt["x"] % 2]
                                    cnt["x"] += 1
                                    k.copy(x_[0:cw, 0:bw], p[0:cw, 0:bw], eng="act")
                                    p2 = k.ps()
                                    k.mm(p2[0:cw, 0:bw], rperm[0:cw, 0:cw], x_[0:cw, 0:bw])
                                    k.tt(a1[0:cw, 0:bw], x_[0:cw, 0:bw], c_[0:cw, 0, 0:bw], ALU.mult, eng="pool")
                                    k.tt(a2[0:cw, 0:bw], p2[0:cw, 0:bw], c_[0:cw, 1, 0:bw], ALU.mult)
                                    k.tt(sg[0:cw, 0:bw], a1[0:cw, 0:bw], a2[0:cw, 0:bw], ALU.add, eng="pool")
                                elif nm in ("cq", "ckv"):
                                    qi = (0 if nm == "cq" else 8) + ch
                                    k.act(sg[0:cw, 0:bw], p[0:cw, 0:bw], AF.Copy, scale=qn[0:cw, qi:qi + 1])
                                    sq = sqf[ch % 2]
                                    k.act(sq[0:cw, 0:bw], p[0:cw, 0:bw], AF.Square)
                                    k.mm(pss[:, 0:bw], ones[0:cw, :], sq[0:cw, 0:bw], start=(ch == 0), stop=(ch == nch - 1))
                                elif nm == "rg":
                                    k.act(sg[0:cw, 0:bw], p[0:cw, 0:bw], AF.Silu)
                                else:
                                    k.copy(sg[0:cw, 0:bw], p[0:cw, 0:bw], eng=("act" if cnt["s"] % 2 else "dve"))
                                k.dma("sp", fmv(nm, ch, bi, cw, bw), sg[0:cw, 0:bw])
                        if pss is not None:
                            k.rstd(rbc[:, 0:bw], pss[:, 0:bw], gw, rtmp[:, 0:bw])
                            k.release(pss)
                            if nm == "cq":
                                k.dma("sp", V(rq_bc.t[bi][:, 0:bw], (rq_bc_s[bi],)), rbc[:, 0:bw])
                            else:
                                k.dma("sp", V(rkv_bc.t[bi][:, 0:bw], (rkv_bc_s[bi],)), rbc[:, 0:bw])
                                pt = k.ps()
                                ns = bw // 128
                                for sub in range(ns):
                                    k.tr(pt[:, sub:sub + 1], rbc[0:1, sub * 128:(sub + 1) * 128], ident[0:1, 0:1])
                                k.copy(rtm[:, 0:ns], pt[:, 0:ns])
                                kt0 = t0 // 128
                                k.dma("sp", V(rkv_tm.t[:, kt0:kt0 + ns], (rkv_tm_s[bi],)), rtm[:, 0:ns])

        GWc = cfg.GW // 128
        FM["mix"] = fm_alloc(D, "fm_mix")

        def bcast_col(dst, src11):
            p = k.ps()
            k.mm(p[:, 0:1], ones[0:1, :], src11)
            k.copy(dst, p[:, 0:1])

        def attn_core(ls_bufs, q_parts, k_parts, vT, ktiles, scale, bw):
            pT = ls_bufs
            po = k.ps(hold=True)
            pd = k.ps(hold=True)
            n = len(ktiles)
            for i, kt in enumerate(ktiles):
                p = k.ps()
                for j, (qv, kf) in enumerate(zip(q_parts, k_parts)):
                    k.mm(p[:, 0:bw], kf(kt), qv, start=(j == 0), stop=(j == len(q_parts) - 1))
                pt = pT[i % len(pT)]
                k.act(pt[:, 0:bw], p[:, 0:bw], AF.Exp, scale=scale)
                k.mm(po[:, 0:bw], vT[:, kt, :], pt[:, 0:bw], start=(i == 0), stop=(i == n - 1))
                k.mm(pd[:, 0:bw], onesb[:, :], pt[:, 0:bw], start=(i == 0), stop=(i == n - 1))
            return po, pd

        def q_blocks(need_ctx):
            return [(bi, t0, bw, isc) for bi, (t0, bw, isc) in enumerate(cfg.blocks) if (need_ctx or not isc)]

        def key_tiles(isc):
            return list(range(N // 128, cfg.KT)) if isc else list(range(cfg.KT))

        def stage_diff(l, need_ctx):
            lam_init = 0.8 - 0.6 * math.exp(-0.3 * l)
            with k.scope() as ls:
                kT = k.sb([128, T], BF16, "kT", ls)
                vT = k.sb([128, cfg.KT, 128], BF16, "vT", ls)
                qT = [k.sb([128, 512], BF16, "qT", ls) for _ in range(2)]
                pT = [k.sb([128, 512], BF16, "pT", ls) for _ in range(3)]
                a0 = k.sb([128, 512], F32, "a0", ls)
                a1 = k.sb([128, 512], F32, "a1", ls)
                rr = k.sb([128, 512], F32, "rr", ls)
                og = k.sb([128, 512], BF16, "og", ls)
                lr = k.sb([1, 256], F32, "lr", ls)
                lt = k.sb([1, 128], F32, "lt", ls)
                sc_ = k.sb([128, 8], F32, "sc", ls)
                trow = k.sb([1, 128], F32, "trow", ls)
                k.dma("sp", lr[:, :], I["diff_lambda"][l:l + 1, :])
                k.tt(lt[0:1, 0:64], lr[0:1, 0:64], lr[0:1, 64:128], ALU.mult)
                k.tt(lt[0:1, 64:128], lr[0:1, 128:192], lr[0:1, 192:256], ALU.mult)
                k.rsum(lr[0:1, 0:1], lt[0:1, 0:64])
                k.rsum(lr[0:1, 1:2], lt[0:1, 64:128])
                k.act(lr[0:1, 2:4], lr[0:1, 0:2], AF.Exp)
                k.tt(lr[0:1, 4:5], lr[0:1, 3:4], lr[0:1, 2:3], ALU.subtract)
                k.ts(lr[0:1, 5:6], lr[0:1, 4:5], -lam_init, ALU.add)
                bcast_col(sc_[:, 0:1], lr[0:1, 5:6])
                row_to_cols(sc_[:, 1:2], I["diff_subln"][l:l + 1, :], 128, trow)
                k.ts(sc_[:, 2:3], sc_[:, 1:2], 1.0 - lam_init, ALU.mult)
                qi = 0
                for h in range(cfg.DIFF_HEADS):
                    for bi, (t0, bw, isc) in enumerate(cfg.blocks):
                        k.dma("sp", kT[:, t0:t0 + bw], fmv("dk", h, bi, 128, bw))
                    dv, dvs = TMV["dv"]
                    k.dma("sp", vT[:, :, :], V(dv.t[h], tuple(dvs[h])))
                    for bi, t0, bw, isc in q_blocks(need_ctx):
                        q = qT[qi % 2]
                        qi += 1
                        k.dma("sp", q[:, 0:bw], fmv("dq", h, bi, 128, bw))
                        kts = key_tiles(isc)
                        for j in range(2):
                            r0, r1 = j * 64, (j + 1) * 64
                            po, pd = attn_core(pT, [q[r0:r1, 0:bw]], [lambda kt, r0=r0, r1=r1: kT[r0:r1, kt * 128:(kt + 1) * 128]],
                                               vT, kts, 64 ** -0.5, bw)
                            dst = a0 if j == 0 else a1
                            k.recip(rr[:, 0:bw], pd[:, 0:bw])
                            k.tt(dst[:, 0:bw], po[:, 0:bw], rr[:, 0:bw], ALU.mult)
                            k.release(po)
                            k.release(pd)
                        k.stt(a0[:, 0:bw], a1[:, 0:bw], sc_[:, 0:1], a0[:, 0:bw], ALU.mult, ALU.add)
                        k.act(a1[:, 0:bw], a0[:, 0:bw], AF.Square)
                        pss = k.ps()
                        k.mm(pss[:, 0:bw], ones[:, :], a1[:, 0:bw])
                        k.rstd(rr[:, 0:bw], pss[:, 0:bw], 128, a1[:, 0:bw])
                        k.stt(og[:, 0:bw], a0[:, 0:bw], sc_[:, 2:3], rr[:, 0:bw], ALU.mult, ALU.mult)
                        k.dma("sp", fmv("mix", h, bi, 128, bw), og[:, 0:bw])

        MH = cfg.MLA_HEADS
        mqn = fm_alloc(MH * 128, "mqn")
        mqr = fm_alloc(MH * 128, "mqr")
        mkn = fm_alloc(MH * 128, "mkn")
        mvd = k.dram([MH, 128, cfg.KT, 128], BF16, "mv")
        mvs = [[mvd.sub() for _ in range(cfg.KT)] for _ in range(MH)]

        def stage_mla(l, need_ctx):
            qch = chunks(cfg.QR)
            kch = chunks(cfg.KVR)
            csv = I["ropecs"].t.rearrange("a p n -> p a n")
            with k.scope() as ls:
                wq = k.sb([128, len(qch), MH * 192], BF16, "wq", ls)
                wkv = k.sb([128, len(kch), MH * 256], BF16, "wkv", ls)
                for ci, (c0, cw) in enumerate(qch):
                    k.dma("pool", wq[0:cw, ci, :], I["mla_w_uq"][l, c0:c0 + cw, :])
                for ci, (c0, cw) in enumerate(kch):
                    k.dma("pool", wkv[0:cw, ci, :], I["mla_w_ukv"][l, c0:c0 + cw, :])
                rtm_sb = k.sb([128, cfg.KT], F32, "rtm_sb", ls)
                k.dma("sp", rtm_sb[:, :], V(rkv_tm.t[:, :], tuple(rkv_tm_s)))
                cqs = k.sb([128, len(qch), 512], BF16, "cqs", ls)
                cks = k.sb([128, len(kch), 512], BF16, "cks", ls)
                rqt = k.sb([128, 512], F32, "rqt", ls)
                rkt = k.sb([128, 512], F32, "rkt", ls)
                cs_ = k.sb([128, 2, 512], F32, "cs", ls)
                stg = [k.sb([128, 512], BF16, "stg", ls) for _ in range(3)]
                xs = k.sb([128, 512], BF16, "xs", ls)
                b1 = k.sb([128, 512], F32, "b1", ls)
                b2 = k.sb([128, 512], F32, "b2", ls)
                si = 0
                for bi, (t0, bw, isc) in enumerate(cfg.blocks):
                    for ci, (c0, cw) in enumerate(qch):
                        k.dma("sp", cqs[0:cw, ci, 0:bw], fmv("cq", ci, bi, cw, bw))
                    for ci, (c0, cw) in enumerate(kch):
                        k.dma("sp", cks[0:cw, ci, 0:bw], fmv("ckv", ci, bi, cw, bw))
                    k.dma("sp", rqt[:, 0:bw], V(rq_bc.t[bi][:, 0:bw], (rq_bc_s[bi],)))
                    k.dma("sp", rkt[:, 0:bw], V(rkv_bc.t[bi][:, 0:bw], (rkv_bc_s[bi],)))
                    if not isc:
                        k.dma("sp", cs_[:, :, :], V(csv[:, :, t0:t0 + 512], (I["ropecs"],)))
                    for h in range(MH):
                        if need_ctx or not isc:
                            p = k.ps()
                            for ci, (c0, cw) in enumerate(qch):
                                k.mm(p[:, 0:bw], wq[0:cw, ci, h * 192:h * 192 + 128], cqs[0:cw, ci, 0:bw], start=(ci == 0), stop=(ci == len(qch) - 1))
                            sg = stg[si % 3]
                            si += 1
                            k.tt(sg[:, 0:bw], p[:, 0:bw], rqt[:, 0:bw], ALU.mult)
                            k.dma("sp", V(mqn[0].t[h, bi][:, 0:bw], (mqn[1][h][bi],)), sg[:, 0:bw])
                            p = k.ps()
                            for ci, (c0, cw) in enumerate(qch):
                                k.mm(p[0:64, 0:bw], wq[0:cw, ci, h * 192 + 128:h * 192 + 192], cqs[0:cw, ci, 0:bw], start=(ci == 0), stop=(ci == len(qch) - 1))
                            sg = stg[si % 3]
                            si += 1
                            if isc:
                                k.tt(sg[0:64, 0:bw], p[0:64, 0:bw], rqt[0:64, 0:bw], ALU.mult)
                            else:
                                k.tt(xs[0:64, 0:bw], p[0:64, 0:bw], rqt[0:64, 0:bw], ALU.mult)
                                p2 = k.ps()
                                k.mm(p2[0:64, 0:bw], rperm[0:64, 0:64], xs[0:64, 0:bw])
                                k.tt(b1[0:64, 0:bw], xs[0:64, 0:bw], cs_[0:64, 0, 0:bw], ALU.mult, eng="pool")
                                k.tt(b2[0:64, 0:bw], p2[0:64, 0:bw], cs_[0:64, 1, 0:bw], ALU.mult)
                                k.tt(sg[0:64, 0:bw], b1[0:64, 0:bw], b2[0:64, 0:bw], ALU.add, eng="pool")
                            k.dma("sp", V(mqr[0].t[h, bi][0:64, 0:bw], (mqr[1][h][bi],)), sg[0:64, 0:bw])
                        p = k.ps()
                        for ci, (c0, cw) in enumerate(kch):
                            k.mm(p[:, 0:bw], wkv[0:cw, ci, h * 256:h * 256 + 128], cks[0:cw, ci, 0:bw], start=(ci == 0), stop=(ci == len(kch) - 1))
                        sg = stg[si % 3]
                        si += 1
                        k.tt(sg[:, 0:bw], p[:, 0:bw], rkt[:, 0:bw], ALU.mult)
                        k.dma("sp", V(mkn[0].t[h, bi][:, 0:bw], (mkn[1][h][bi],)), sg[:, 0:bw])
                        for sub in range(bw // 128):
                            kt = t0 // 128 + sub
                            p = k.ps()
                            for ci, (c0, cw) in enumerate(kch):
                                k.mm(p[:, 0:128], cks[0:cw, ci, sub * 128:(sub + 1) * 128], wkv[0:cw, ci, h * 256 + 128:h * 256 + 256], start=(ci == 0), stop=(ci == len(kch) - 1))
                            sg = stg[si % 3]
                            si += 1
                            k.ts(sg[:, 0:128], p[:, 0:128], rtm_sb[:, kt:kt + 1], ALU.mult)
                            k.dma("sp", V(mvd.t[h][:, kt, :], (mvs[h][kt],)), sg[:, 0:128])
            with k.scope() as ls:
                kTn = k.sb([128, T], BF16, "kTn", ls)
                kTr = k.sb([64, T], BF16, "kTr", ls)
                vT = k.sb([128, cfg.KT, 128], BF16, "vT", ls)
                qn = [k.sb([128, 512], BF16, "qn", ls) for _ in range(2)]
                qr = [k.sb([64, 512], BF16, "qr", ls) for _ in range(2)]
                pT = [k.sb([128, 512], BF16, "pT", ls) for _ in range(3)]
                rr = k.sb([128, 512], F32, "rr", ls)
                og = k.sb([128, 512], BF16, "og", ls)
                for bi, (t0, bw, isc) in enumerate(cfg.blocks):
                    k.dma("sp", kTr[:, t0:t0 + bw], fmv("kr", 0, bi, 64, bw))
                qi = 0
                for h in range(MH):
                    for bi, (t0, bw, isc) in enumerate(cfg.blocks):
                        k.dma("sp", kTn[:, t0:t0 + bw], V(mkn[0].t[h, bi][:, 0:bw], (mkn[1][h][bi],)))
                    k.dma("sp", vT[:, :, :], V(mvd.t[h], tuple(mvs[h])))
                    for bi, t0, bw, isc in q_blocks(need_ctx):
                        qa, qb_ = qn[qi % 2], qr[qi % 2]
                        qi += 1
                        k.dma("sp", qa[:, 0:bw], V(mqn[0].t[h, bi][:, 0:bw], (mqn[1][h][bi],)))
                        k.dma("sp", qb_[:, 0:bw], V(mqr[0].t[h, bi][0:64, 0:bw], (mqr[1][h][bi],)))
                        po, pd = attn_core(pT, [qa[:, 0:bw], qb_[0:64, 0:bw]],
                                           [lambda kt: kTn[:, kt * 128:(kt + 1) * 128], lambda kt: kTr[0:64, kt * 128:(kt + 1) * 128]],
                                           vT, key_tiles(isc), 192 ** -0.5, bw)
                        k.recip(rr[:, 0:bw], pd[:, 0:bw])
                        k.tt(og[:, 0:bw], po[:, 0:bw], rr[:, 0:bw], ALU.mult)
                        k.release(po)
                        k.release(pd)
                        k.dma("sp", fmv("mix", 2 * GWc + h, bi, 128, bw), og[:, 0:bw])

        def stage_ret(l, need_ctx):
            RH = cfg.RET_HEADS
            ksc = 64 ** -0.5
            NI = cfg.KT + 8
            with k.scope() as ls:
                dr_ = k.sb([1, 2 * RH], F32, "dr", ls)
                lgb_ = k.sb([128, 2 * RH], F32, "lg", ls)
                nlg = k.sb([128, 2 * RH], F32, "nlg", ls)
                d0i = k.sb([128, 512], mybir.dt.int32, "d0i", ls)
                D0 = k.sb([128, 512], F32, "D0", ls)
                ioi = k.sb([128, NI], mybir.dt.int32, "ioi", ls)
                iof = k.sb([128, NI], F32, "iof", ls)
                ctf = k.sb([128, NI], F32, "ctf", ls)
                ctb = k.sb([128, NI], F32, "ctb", ls)
                Ef = k.sb([128, 512], F32, "Ef", ls)
                Eb = k.sb([128, 512], F32, "Eb", ls)
                Wd = [k.sb([128, 512], F32, "Wd", ls) for _ in range(4)]
                w1 = k.sb([128, 512], F32, "w1", ls)
                w2 = k.sb([128, 512], F32, "w2", ls)
                w3 = k.sb([128, 512], F32, "w3", ls)
                kT = k.sb([128, T], BF16, "kT", ls)
                vT = k.sb([128, cfg.KT, 128], BF16, "vT", ls)
                qT = [k.sb([128, 512], BF16, "qT", ls) for _ in range(2)]
                gT = [k.sb([128, 512], BF16, "gT", ls) for _ in range(2)]
                pT = [k.sb([128, 512], BF16, "pT", ls) for _ in range(3)]
                ya = k.sb([128, 512], F32, "ya", ls)
                yb = k.sb([128, 512], F32, "yb", ls)
                rr = k.sb([128, 512], F32, "rr", ls)
                og = k.sb([128, 512], BF16, "og", ls)
                gcol = k.sb([128, GWc], F32, "gcol", ls)
                trow = k.sb([1, cfg.GW], F32, "trow", ls)
                row_to_cols(gcol, I["ret_norm"][l:l + 1, :], cfg.GW, trow)
                k.dma("sp", dr_[:, :], I["ret_decay"][l:l + 1, :])
                k.act(dr_[:, :], dr_[:, :], AF.Exp, scale=-1.0)
                k.ts(dr_[:, :], dr_[:, :], 1.0, ALU.add)
                k.act(dr_[:, :], dr_[:, :], AF.Ln)
                p = k.ps()
                k.mm(p[:, 0:2 * RH], ones[0:1, :], dr_[0:1, :])
                k.ts(lgb_[:, :], p[:, 0:2 * RH], -1.0, ALU.mult)
                k.ts(nlg[:, :], lgb_[:, :], -1.0, ALU.mult)
                k.op("pool", lambda e: e.iota(d0i.t[:, :], [[1, 512]], base=0, channel_multiplier=-1), [], [d0i[:, :]])
                k.copy(D0[:, :], d0i[:, :])
                k.op("pool", lambda e: e.iota(ioi.t[:, :], [[128, NI]], base=0, channel_multiplier=0), [], [ioi[:, :]])
                k.copy(iof[:, :], ioi[:, :])
                lnk = math.log(ksc)
                qi = 0
                for h in range(RH):
                    lf, lb = lgb_[:, h:h + 1], lgb_[:, RH + h:RH + h + 1]
                    nlb = nlg[:, RH + h:RH + h + 1]
                    k.act(ctf[:, :], iof[:, :], AF.Exp, scale=lf)
                    k.act(ctb[:, :], iof[:, :], AF.Exp, scale=lb)
                    k.act(Ef[:, :], D0[:, :], AF.Exp, scale=lf, bias=lnk)
                    k.act(Eb[:, :], D0[:, :], AF.Exp, scale=nlb, bias=lnk)
                    for oi in range(4):
                        off = float(oi * 128)
                        k.ts(w1[:, :], D0[:, :], -off, ALU.add, 0.0, ALU.max)
                        k.act(w1[:, :], w1[:, :], AF.Exp, scale=lf, bias=lnk)
                        k.ts(w2[:, :], D0[:, :], -off, ALU.add, 0.0, ALU.min)
                        k.act(w2[:, :], w2[:, :], AF.Exp, scale=nlb, bias=lnk)
                        k.ts(w3[:, :], D0[:, :], -off, ALU.add, 0.0, ALU.is_ge)
                        k.tt(w1[:, :], w1[:, :], w2[:, :], ALU.subtract)
                        k.tt(w1[:, :], w1[:, :], w3[:, :], ALU.mult)
                        k.tt(Wd[oi][:, :], w1[:, :], w2[:, :], ALU.add)
                    rch, rro = h // 2, (h % 2) * 64
                    for bi, (t0, bw, isc) in enumerate(cfg.blocks):
                        k.dma("sp", kT[:, t0:t0 + bw], fmv("rk", rch, bi, 128, bw))
                    rv_, rvs = TMV["rv"]
                    k.dma("sp", vT[:, :, :], V(rv_.t[h], tuple(rvs[h])))
                    for bi, t0, bw, isc in q_blocks(need_ctx):
                        q, g = qT[qi % 2], gT[qi % 2]
                        qi += 1
                        k.dma("sp", q[:, 0:bw], fmv("rq", rch, bi, 128, bw))
                        k.dma("sp", g[:, 0:bw], fmv("rg", h, bi, 128, bw))
                        kts = key_tiles(isc)
                        po = k.ps(hold=True)
                        for i, kt in enumerate(kts):
                            p = k.ps()
                            k.mm(p[:, 0:bw], kT[rro:rro + 64, kt * 128:(kt + 1) * 128], q[rro:rro + 64, 0:bw])
                            pt = pT[i % 3]
                            kctx = kt >= N // 128
                            if kctx == isc:
                                s0 = (kt * 128 - N) if isc else kt * 128
                                tq = 0 if isc else t0
                                if s0 + 128 <= tq:
                                    ci_ = (tq - s0) // 128
                                    k.stt(pt[:, 0:bw], p[:, 0:bw], ctf[:, ci_:ci_ + 1], Ef[:, 0:bw], ALU.mult, ALU.mult)
                                elif s0 >= tq + bw:
                                    ci_ = (s0 - tq) // 128
                                    k.stt(pt[:, 0:bw], p[:, 0:bw], ctb[:, ci_:ci_ + 1], Eb[:, 0:bw], ALU.mult, ALU.mult)
                                else:
                                    k.tt(pt[:, 0:bw], p[:, 0:bw], Wd[(s0 - tq) // 128][:, 0:bw], ALU.mult)
                            else:
                                c0 = kt * 128 - N
                                cf = (t0 - (c0 - LC)) // 128
                                cb = (N + c0 - t0) // 128
                                k.ts(w1[:, 0:bw], Ef[:, 0:bw], ctf[:, cf:cf + 1], ALU.mult)
                                k.stt(w1[:, 0:bw], Eb[:, 0:bw], ctb[:, cb:cb + 1], w1[:, 0:bw], ALU.mult, ALU.add)
                                k.tt(pt[:, 0:bw], p[:, 0:bw], w1[:, 0:bw], ALU.mult)
                            k.mm(po[:, 0:bw], vT[:, kt, :], pt[:, 0:bw], start=(i == 0), stop=(i == len(kts) - 1))
                        k.copy(ya[:, 0:bw], po[:, 0:bw])
                        k.release(po)
                        k.act(yb[:, 0:bw], ya[:, 0:bw], AF.Square)
                        pss = k.ps()
                        k.mm(pss[:, 0:bw], ones[:, :], yb[:, 0:bw])
                        k.rstd(rr[:, 0:bw], pss[:, 0:bw], 128, yb[:, 0:bw])
                        k.stt(ya[:, 0:bw], ya[:, 0:bw], gcol[:, h:h + 1], rr[:, 0:bw], ALU.mult, ALU.mult)
                        k.tt(og[:, 0:bw], ya[:, 0:bw], g[:, 0:bw], ALU.mult)
                        k.dma("sp", fmv("mix", 3 * GWc + h, bi, 128, bw), og[:, 0:bw])

        s5g = fm_alloc(cfg.GW, "s5g")
        PI = math.pi

        def stage_s5(l, need_ctx):
            G = cfg.S5_G
            NP = G // 2
            lat_b = [(bi, t0, bw) for bi, (t0, bw, isc) in enumerate(cfg.blocks) if not isc]
            ctx_b = [(bi, t0, bw) for bi, (t0, bw, isc) in enumerate(cfg.blocks) if isc]
            with k.scope() as ls:
                trow = k.sb([1, max(G * 64, cfg.GW)], F32, "trow", ls)
                bc = k.sb([128, G], F32, "bc", ls)
                are = k.sb([128, 2, NP], F32, "are", ls)
                aim = k.sb([128, 2, NP], F32, "aim", ls)
                dtc = k.sb([128, 2, NP], F32, "dtc", ls)
                rr = k.sb([128, 2, NP], F32, "rr", ls)
                th = k.sb([128, 2, NP], F32, "th", ls)
                cr = k.sb([128, 2, NP], F32, "cr", ls)
                ci = k.sb([128, 2, NP], F32, "ci", ls)
                ncr = k.sb([128, 2, NP], F32, "ncr", ls)
                q1 = k.sb([128, 2, NP], F32, "q1", ls)
                q2 = k.sb([128, 2, NP], F32, "q2", ls)
                q3 = k.sb([128, 2, NP], F32, "q3", ls)
                q4 = k.sb([128, 2, NP], F32, "q4", ls)
                dcol = k.sb([32, NP], F32, "dcol", ls)
                jfi = k.sb([128, 512], mybir.dt.int32, "jfi", ls)
                jf = k.sb([128, 512], F32, "jf", ls)

                def sincos(out_s, out_c, ang, tmp, tmpi):
                    k.ts(tmp, ang, 1.0 / (2 * PI), ALU.mult)
                    k.copy(tmpi, tmp)
                    k.copy(tmp, tmpi)
                    k.stt(tmp, tmp, -2 * PI, ang, ALU.mult, ALU.add)
                    k.ts(out_c, tmp, PI, ALU.is_gt)
                    k.stt(tmp, out_c, -2 * PI, tmp, ALU.mult, ALU.add)
                    k.ts(out_c, tmp, -PI, ALU.is_lt)
                    k.stt(tmp, out_c, 2 * PI, tmp, ALU.mult, ALU.add)
                    k.act(out_s, tmp, AF.Sin)
                    k.ts(tmp, tmp, 0.5 * PI, ALU.add)
                    k.ts(out_c, tmp, PI, ALU.is_gt)
                    k.stt(tmp, out_c, -2 * PI, tmp, ALU.mult, ALU.add)
                    k.act(out_c, tmp, AF.Sin)

                negpi = k.sb([128, 1], F32, "negpi", ls)
                k.memset(negpi[:, :], -PI)
                k.op("pool", lambda e: e.iota(jfi.t[:, :], [[1, 512]], base=1, channel_multiplier=0), [], [jfi[:, :]])
                k.copy(jf[:, :], jfi[:, :])
                for dr in range(2):
                    row_to_cols(are[:, dr, :], I["s5_a_re"][l, dr:dr + 1, :], G * 64, trow)
                    row_to_cols(aim[:, dr, :], I["s5_a_im"][l, dr:dr + 1, :], G * 64, trow)
                    k.dma("sp", trow[0:1, 0:G], I["s5_log_dt"][l, dr:dr + 1, :])
                    k.act(trow[0:1, 0:G], trow[0:1, 0:G], AF.Exp)
                    p = k.ps()
                    k.mm(p[:, 0:G], ones[0:1, :], trow[0:1, 0:G])
                    k.copy(bc[:, :], p[:, 0:G])
                    bcv = bc[:, :].rearrange("p (n two) -> p n two", two=2)
                    k.copy(dtc[0:64, dr, :], bcv[0:64, :, 0])
                    k.copy(dtc[64:128, dr, :], bcv[64:128, :, 1])
                k.dma("sp", trow[0:1, 0:cfg.GW], I["s5_d"][l:l + 1, :])
                p = k.ps()
                for i in range(NP):
                    k.tr(p[0:32, i:i + 1], trow[0:1, i * 32:(i + 1) * 32], ident[0:1, 0:1])
                k.copy(dcol[:, :], p[0:32, 0:NP])
                fl = lambda b: b[:, :, :].rearrange("p a n -> p (a n)")
                k.tt(fl(q1), fl(are), fl(dtc), ALU.mult)
                k.act(fl(rr), fl(q1), AF.Exp)
                k.tt(fl(th), fl(aim), fl(dtc), ALU.mult)
                qi32 = k.sb([128, 2 * NP], mybir.dt.int32, "qi32", ls)
                sincos(fl(q1), fl(q2), fl(th), fl(q3), qi32[:, :])
                k.tt(fl(q1), fl(q1), fl(rr), ALU.mult)
                k.tt(fl(q2), fl(q2), fl(rr), ALU.mult)
                k.ts(fl(q2), fl(q2), -1.0, ALU.add)
                k.tt(fl(q3), fl(are), fl(are), ALU.mult)
                k.tt(fl(q4), fl(aim), fl(aim), ALU.mult)
                k.tt(fl(q3), fl(q3), fl(q4), ALU.add)
                k.recip(fl(q3), fl(q3))
                k.tt(fl(cr), fl(q2), fl(are), ALU.mult)
                k.tt(fl(q4), fl(q1), fl(aim), ALU.mult)
                k.tt(fl(cr), fl(cr), fl(q4), ALU.add)
                k.tt(fl(cr), fl(cr), fl(q3), ALU.mult)
                k.tt(fl(ci), fl(q1), fl(are), ALU.mult)
                k.tt(fl(q4), fl(q2), fl(aim), ALU.mult)
                k.tt(fl(ci), fl(ci), fl(q4), ALU.subtract)
                k.tt(fl(ci), fl(ci), fl(q3), ALU.mult)
                k.ts(fl(ncr), fl(cr), -1.0, ALU.mult)

                ang = k.sb([128, 512], F32, "ang", ls)
                atmp = k.sb([128, 512], F32, "atmp", ls)
                cosJ = k.sb([128, 512], F32, "cosJ", ls)
                sinJ = k.sb([128, 512], F32, "sinJ", ls)
                tre = k.sb([128, 512], F32, "tre", ls)
                tim = k.sb([128, 512], F32, "tim", ls)
                rJ = k.sb([128, 512], F32, "rJ", ls)
                z1 = k.sb([128, 512], F32, "z1", ls)
                z2 = k.sb([128, 512], F32, "z2", ls)
                zr = k.sb([128, 512], F32, "zr", ls)
                zi = k.sb([128, 512], F32, "zi", ls)
                xr = k.sb([128, 512], F32, "xr", ls)
                xi = k.sb([128, 512], F32, "xi", ls)
                zp = k.sb([128, 2], F32, "zp", ls)
                Bw = [k.sb([128, 32], F32, "Bw", ls) for _ in range(2)]
                Bl = [k.sb([32, 128], F32, "Bl", ls) for _ in range(2)]
                Cw = [k.sb([32, 128], F32, "Cw", ls) for _ in range(2)]
                Cl = [k.sb([128, 32], F32, "Cl", ls) for _ in range(2)]
                uf = k.sb([32, T], F32, "uf", ls)
                ya = k.sb([32, T], F32, "ya", ls)
                g1 = k.sb([32, T], F32, "g1", ls)
                gb = k.sb([32, T], BF16, "gb", ls)
                for bw_ in Bw:
                    k.memset(bw_[:, :], 0.0)
                for cw_ in Cw:
                    k.memset(cw_[:, :], 0.0)
                GC = math.sqrt(2.0 / math.pi) * 2.0
                for pk in range(NP):
                    ch, ro = (pk * 32) // 128, (pk * 32) % 128
                    for bi, (t0, bw, isc) in enumerate(cfg.blocks):
                        d_, subs_ = FM["su"]
                        k.dma("pool", uf[:, t0:t0 + bw], V(d_.t[ch, bi][ro:ro + 32, 0:bw], (subs_[ch][bi],)))
                    for dr in range(2):
                        for ri, nm in enumerate(("s5_b_re", "s5_b_im")):
                            src = I[nm]
                            k.dma("sp", Bw[ri][0:64, 0:16], src[l, dr, pk * 128:pk * 128 + 64, :])
                            k.dma("sp", Bw[ri][64:128, 16:32], src[l, dr, pk * 128 + 64:pk * 128 + 128, :])
                            p = k.ps()
                            k.tr(p[0:32, 0:128], Bw[ri][:, :], ident[:, :])
                            k.copy(Bl[ri][:, :], p[0:32, 0:128])
                        for ri, nm in enumerate(("s5_c_re", "s5_c_im")):
                            src = I[nm]
                            k.dma("sp", Cw[ri][0:16, 0:64], src[l, dr, pk * 32:pk * 32 + 16, :])
                            k.dma("sp", Cw[ri][16:32, 64:128], src[l, dr, pk * 32 + 16:pk * 32 + 32, :])
                            p = k.ps()
                            k.tr(p[:, 0:32], Cw[ri][:, :], ident[0:32, 0:32])
                            if ri == 0:
                                k.copy(Cl[ri][:, :], p[:, 0:32])
                            else:
                                k.ts(Cl[ri][:, :], p[:, 0:32], -1.0, ALU.mult)
                        k.ts(ang[:, :], jf[:, :], th[:, dr, pk:pk + 1], ALU.mult)
                        sincos(sinJ[:, :], cosJ[:, :], ang[:, :], atmp[:, :], jfi[:, :])
                        k.ts(tre[:, :], cosJ[:, :], cr[:, dr, pk:pk + 1], ALU.mult)
                        k.stt(tre[:, :], sinJ[:, :], ci[:, dr, pk:pk + 1], tre[:, :], ALU.mult, ALU.add)
                        k.ts(tim[:, :], cosJ[:, :], ci[:, dr, pk:pk + 1], ALU.mult)
                        k.stt(tim[:, :], sinJ[:, :], ncr[:, dr, pk:pk + 1], tim[:, :], ALU.mult, ALU.add)
                        k.memset(rJ[:, :], 1.0)
                        k.ts(rJ[:, :], rJ[:, :], rr[:, dr, pk:pk + 1], ALU.mult)
                        k.memset(zp[:, :], 0.0)
                        seq = (ctx_b + lat_b) if dr == 0 else (ctx_b[::-1] + lat_b[::-1])
                        for bi, t0, bw in seq:
                            isc = cfg.blocks[bi][2]
                            rv = (lambda v: v[:, ::-1]) if dr == 1 else (lambda v: v)
                            pbr = k.ps()
                            k.mm(pbr[:, 0:bw], Bl[0][:, :], uf[:, t0:t0 + bw])
                            pbi = k.ps()
                            k.mm(pbi[:, 0:bw], Bl[1][:, :], uf[:, t0:t0 + bw])
                            br_, bi_ = rv(pbr[:, 0:bw]), rv(pbi[:, 0:bw])
                            k.tt(z1[:, 0:bw], tre[:, 0:bw], br_, ALU.mult)
                            k.tt(z2[:, 0:bw], tim[:, 0:bw], bi_, ALU.mult)
                            k.tt(zr[:, 0:bw], z1[:, 0:bw], z2[:, 0:bw], ALU.subtract, eng="pool")
                            k.tt(z1[:, 0:bw], tre[:, 0:bw], bi_, ALU.mult)
                            k.tt(z2[:, 0:bw], tim[:, 0:bw], br_, ALU.mult)
                            k.tt(zi[:, 0:bw], z1[:, 0:bw], z2[:, 0:bw], ALU.add, eng="pool")
                            k.scan(zr[:, 0:bw], rJ[:, 0:bw], zr[:, 0:bw], zp[:, 0:1])
                            k.scan(zi[:, 0:bw], rJ[:, 0:bw], zi[:, 0:bw], zp[:, 1:2])
                            k.tt(z1[:, 0:bw], cosJ[:, 0:bw], zr[:, 0:bw], ALU.mult)
                            k.tt(z2[:, 0:bw], sinJ[:, 0:bw], zi[:, 0:bw], ALU.mult, eng="pool")
                            k.tt(xr[:, 0:bw], z1[:, 0:bw], z2[:, 0:bw], ALU.subtract)
                            k.tt(z1[:, 0:bw], sinJ[:, 0:bw], zr[:, 0:bw], ALU.mult, eng="pool")
                            k.tt(z2[:, 0:bw], cosJ[:, 0:bw], zi[:, 0:bw], ALU.mult)
                            k.tt(xi[:, 0:bw], z1[:, 0:bw], z2[:, 0:bw], ALU.add, eng="pool")
                            k.copy(zp[:, 0:1], xr[:, bw - 1:bw])
                            k.copy(zp[:, 1:2], xi[:, bw - 1:bw])
                            if isc and not need_ctx:
                                continue
                            py = k.ps()
                            k.mm(py[0:32, 0:bw], Cl[0][:, :], xr[:, 0:bw], start=True, stop=False)
                            k.mm(py[0:32, 0:bw], Cl[1][:, :], xi[:, 0:bw], start=False, stop=True)
                            if dr == 0:
                                k.stt(ya[:, t0:t0 + bw], uf[:, t0:t0 + bw], dcol[:, pk:pk + 1], py[0:32, 0:bw], ALU.mult, ALU.add)
                            else:
                                k.tt(ya[:, t0:t0 + bw], ya[:, t0:t0 + bw], py[0:32, 0:bw][:, ::-1], ALU.add)
                    tr_ = [(bi, t0, bw) for bi, (t0, bw, isc) in enumerate(cfg.blocks) if (need_ctx or not isc)]
                    t_lo, t_hi = tr_[0][1], tr_[-1][1] + tr_[-1][2]
                    yv = ya[:, t_lo:t_hi]
                    gv = g1[:, t_lo:t_hi]
                    k.tt(gv, yv, yv, ALU.mult)
                    k.ts(gv, gv, 0.044715, ALU.mult, 1.0, ALU.add)
                    k.tt(gv, gv, yv, ALU.mult, eng="pool")
                    k.act(gv, gv, AF.Sigmoid, scale=GC)
                    k.tt(gb[:, t_lo:t_hi], gv, yv, ALU.mult)
                    for bi, t0, bw in tr_:
                        k.dma("sp", V(s5g[0].t[ch, bi][ro:ro + 32, 0:bw], (s5g[1][ch][bi],)), gb[:, t0:t0 + bw])
            with k.scope() as ls:
                gw = k.sb([128, GWc, cfg.GW], BF16, "gw", ls)
                k.dma("pool", gw[:, :, :], V(I["s5_glu_w"].t[l].rearrange("(c p) n -> p c n", p=128), (I["s5_glu_w"],)))
                bcol = k.sb([128, GWc], F32, "bcol", ls)
                trow = k.sb([1, cfg.GW], F32, "trow", ls)
                row_to_cols(bcol, I["s5_glu_b"][l:l + 1, :], cfg.GW, trow)
                gin = [k.sb([128, GWc, 512], BF16, "gin", ls) for _ in range(2)]
                gt = [k.sb([128, 512], F32, "gt", ls) for _ in range(2)]
                og = [k.sb([128, 512], BF16, "og", ls) for _ in range(2)]
                n = 0
                for bi, t0, bw, isc in q_blocks(need_ctx):
                    gi = gin[bi % 2]
                    for c in range(GWc):
                        k.dma("sp", gi[:, c, 0:bw], V(s5g[0].t[c, bi][:, 0:bw], (s5g[1][c][bi],)))
                    for oc in range(GWc):
                        p = k.ps()
                        for c in range(GWc):
                            k.mm(p[:, 0:bw], gw[:, c, oc * 128:(oc + 1) * 128], gi[:, c, 0:bw], start=(c == 0), stop=(c == GWc - 1))
                        g_, o_ = gt[n % 2], og[n % 2]
                        n += 1
                        k.act(g_[:, 0:bw], p[:, 0:bw], AF.Sigmoid, bias=bcol[:, oc:oc + 1])
                        k.tt(o_[:, 0:bw], g_[:, 0:bw], gi[:, oc, 0:bw], ALU.mult)
                        k.dma("sp", fmv("mix", GWc + oc, bi, 128, bw), o_[:, 0:bw])

        def xv(ti):
            return V(xres.t[ti * 128:(ti + 1) * 128, :], (xres_t[ti],))

        def stage_wout(l, blocks):
            wv = I["w_out"].t[l].rearrange("(c p) n -> p c n", p=128)
            with k.scope() as ls:
                mb = k.sb([128, KC, 512], BF16, "mb", ls)
                xt = [k.sb([128, D], F32, "xt", ls) for _ in range(4)]
                wt = [k.sb([128, KC, 256], BF16, "wt", ls) for _ in range(3)]
                G1 = k.sb([128, D], F32, "G1", ls)
                tmp = [k.sb([128, 256], F32, "tmp", ls) for _ in range(2)]
                cur_r = None
                wi = 0
                n = 0
                for bi in blocks:
                    t0, bw, isc = cfg.blocks[bi]
                    r = 1 if isc else 0
                    if r != cur_r:
                        k.dma("sp", G1[:, :], modbc[l][r][2][:, :])
                        cur_r = r
                    for c in range(KC):
                        k.dma("sp", mb[:, c, 0:bw], fmv("mix", c, bi, 128, bw))
                    ns = bw // 128
                    for sub in range(ns):
                        k.dma("sp", xt[sub][:, :], xv(t0 // 128 + sub))
                    for ct in range(D // 256):
                        w = wt[wi % 3]
                        wi += 1
                        k.dma("sp" if wi % 2 else "pool", w[:, :, :], V(woutb[l][0].t[ct], (woutb[l][1][ct],)))
                        for sub in range(ns):
                            p = k.ps()
                            for c in range(KC):
                                k.mm(p[:, 0:256], mb[:, c, sub * 128:(sub + 1) * 128], w[:, c, :], start=(c == 0), stop=(c == KC - 1))
                            tm = tmp[n % 2]
                            n += 1
                            k.tt(tm[:, :], p[:, 0:256], G1[:, ct * 256:(ct + 1) * 256], ALU.mult)
                            k.tt(xt[sub][:, ct * 256:(ct + 1) * 256], xt[sub][:, ct * 256:(ct + 1) * 256], tm[:, :], ALU.add, eng="pool")
                    for sub in range(ns):
                        k.dma("sp", xv(t0 // 128 + sub), xt[sub][:, :])

        def stage_moe(l, blocks):
            FC = cfg.FF // 128
            with k.scope() as ls:
                hb = k.sb([128, KC, 512], BF16, "hb", ls)
                acc = [k.sb([128, D], F32, "acc", ls) for _ in range(4)]
                wt = [k.sb([128, KC, 256], BF16, "wt", ls) for _ in range(2)]
                hid = k.sb([128, FC, 512], BF16, "hid", ls)
                wd = [k.sb([128, FC, 512], BF16, "wd", ls) for _ in range(2)]
                HC = min(2048, D)
                G2 = k.sb([128, HC], F32, "G2", ls)
                xh = k.sb([128, HC], F32, "xh", ls)
                sl = [k.sb([128, 512], F32, "sl", ls) for _ in range(2)]
                dws = k.sb([128, 4, 16], F32, "dws", ls)
                wi = 0
                di = 0
                n = 0
                for bi in blocks:
                    t0, bw, isc = cfg.blocks[bi]
                    r = 1 if isc else 0
                    ns = bw // 128
                    k.dma("sp", hb[:, :, 0:bw], V(hT.t[bi][:, :, 0:bw], (hT_b[bi],)))
                    for sub in range(ns):
                        ti = t0 // 128 + sub
                        k.dma("sp", dws[:, sub, :], V(dwd.t[ti * 128:(ti + 1) * 128, :], (dwd_t[ti],)))
                    for e in range(16):
                        wgv = I["moe_w_gate"].t[l, e].rearrange("(c p) f -> p c f", p=128)
                        wuv = I["moe_w_up"].t[l, e].rearrange("(c p) f -> p c f", p=128)
                        wdv = I["moe_w_down"].t[l, e].rearrange("(c p) n -> p c n", p=128)
                        for f0 in range(0, cfg.FF, 256):
                            fw = min(256, cfg.FF - f0)
                            wg_ = wt[wi % 2]
                            wu_ = wt[(wi + 1) % 2]
                            wi += 2
                            jt = f0 // 256
                            k.dma("sp", wg_[:, :, 0:fw], V(wgb[l][0].t[e, jt][:, :, 0:fw], (wgb[l][1][e][jt],)))
                            k.dma("pool", wu_[:, :, 0:fw], V(wub[l][0].t[e, jt][:, :, 0:fw], (wub[l][1][e][jt],)))
                            for j0 in range(0, fw, 128):
                                fc = (f0 + j0) // 128
                                pg = k.ps()
                                for c in range(KC):
                                    k.mm(pg[:, 0:bw], wg_[:, c, j0:j0 + 128], hb[:, c, 0:bw], start=(c == 0), stop=(c == KC - 1))
                                pu = k.ps()
                                for c in range(KC):
                                    k.mm(pu[:, 0:bw], wu_[:, c, j0:j0 + 128], hb[:, c, 0:bw], start=(c == 0), stop=(c == KC - 1))
                                s_ = sl[n % 2]
                                n += 1
                                k.act(s_[:, 0:bw], pg[:, 0:bw], AF.Silu)
                                k.tt(hid[:, fc, 0:bw], pu[:, 0:bw], s_[:, 0:bw], ALU.mult)
                        for ct in range(D // 512):
                            w = wd[di % 2]
                            di += 1
                            k.dma("sp" if di % 2 else "pool", w[:, :, :], V(wdb[l][0].t[e, ct], (wdb[l][1][e][ct],)))
                            for sub in range(ns):
                                p = k.ps()
                                for fc in range(FC):
                                    k.mm(p[:, :], hid[:, fc, sub * 128:(sub + 1) * 128], w[:, fc, :], start=(fc == 0), stop=(fc == FC - 1))
                                av = acc[sub][:, ct * 512:(ct + 1) * 512]
                                if e == 0:
                                    k.ts(av, p[:, :], dws[:, sub, e:e + 1], ALU.mult)
                                else:
                                    k.stt(av, p[:, :], dws[:, sub, e:e + 1], av, ALU.mult, ALU.add)
                    for hc in range(0, D, HC):
                        k.dma("sp", G2[:, :], modbc[l][r][5][:, hc:hc + HC])
                        for sub in range(ns):
                            ti = t0 // 128 + sub
                            k.dma("sp", xh[:, :], V(xres.t[ti * 128:(ti + 1) * 128, hc:hc + HC], (xres_t[ti],)))
                            k.tt(acc[sub][:, hc:hc + HC], acc[sub][:, hc:hc + HC], G2[:, :], ALU.mult, eng="pool")
                            k.tt(xh[:, :], xh[:, :], acc[sub][:, hc:hc + HC], ALU.add)
                            k.dma("sp", V(xres.t[ti * 128:(ti + 1) * 128, hc:hc + HC], (xres_t[ti],)), xh[:, :])

        def stage_final():
            with k.scope() as ls:
                gb_ = k.sb([128, D], F32, "gfin", ls)
                grow = k.sb([1, D], F32, "grow", ls)
                xt = [k.sb([128, D], F32, "xt", ls) for _ in range(2)]
                junk = k.sb([128, D], BF16, "junk", ls)
                sm = k.sb([128, 4], F32, "sm", ls)
                k.dma("sp", grow[:, :], I["final_norm"][0:1, :])
                for ct in range(D // 512):
                    p = k.ps()
                    k.mm(p[:, :], ones[0:1, :], grow[0:1, ct * 512:(ct + 1) * 512])
                    k.copy(gb_[:, ct * 512:(ct + 1) * 512], p[:, :])
                for ti in range(N // 128):
                    x = xt[ti % 2]
                    k.dma("sp", x[:, :], xv(ti))
                    k.act(junk[:, :], x[:, :], AF.Square, accum=sm[:, 0:1])
                    k.rstd(sm[:, 1:2], sm[:, 0:1], D, sm[:, 2:3])
                    k.stt(x[:, :], x[:, :], sm[:, 1:2], gb_[:, :], ALU.mult, ALU.mult)
                    k.dma("sp", OUT[ti * 128:(ti + 1) * 128, :], x[:, :])

        def run_all():
            for l in range(L):
                convert_weights(l)
            stage_ada()
            allb = list(range(cfg.NTB))
            for l in range(L):
                need_ctx = l < L - 1
                ob = [bi for bi in allb if (need_ctx or not cfg.blocks[bi][2])]
                stage_norm(l, 0, allb, False)
                stage_proj(l)
                stage_diff(l, need_ctx)
                stage_s5(l, need_ctx)
                stage_mla(l, need_ctx)
                stage_ret(l, need_ctx)
                stage_wout(l, ob)
                stage_norm(l, 1, ob, True)
                stage_moe(l, ob)
            stage_final()

        def dbg_out(name, d, subs, dt):
            shape = list(d.t.shape)
            o = Buf(nc.dram_tensor("o_" + name, shape, dt, kind="ExternalOutput"), "o_" + name)
            idx = tuple(slice(None) for _ in shape)
            k.dma("sp", V(o.t[idx], (o,)), V(d.t[idx], tuple(subs)))
            return o

        outs = [OUT]
        run_all()
        k.finish(outs)
    return nc


def host_consts(cfg):
    ident = np.eye(128, dtype=np.float32)
    R = np.zeros((64, 64), np.float32)
    for i in range(16):
        R[i + 16, i] = -1.0
        R[i, i + 16] = 1.0
        R[i + 48, i + 32] = -1.0
        R[i + 32, i + 48] = 1.0
    rp = np.zeros((128, 128), np.float32)
    rp[:64, :64] = R
    rp[64:, 64:] = R
    rows = cfg.N // 64
    row = np.repeat(np.arange(rows, dtype=np.float32), 64)
    col = np.tile(np.arange(64, dtype=np.float32), rows)
    inv = (10000.0 ** (-np.arange(16, dtype=np.float32) / 16)).astype(np.float32)
    ar = row[:, None] * inv
    ac = col[:, None] * inv
    ang = np.concatenate([ar, ar, ac, ac], axis=-1)
    cs = np.stack([np.cos(ang).T, np.sin(ang).T]).astype(np.float32)
    cs = np.concatenate([cs, cs], axis=1)
    return {"ident": ident, "rperm": rp, "ropecs": np.ascontiguousarray(cs)}


def make_in_maps(cfg, inp, ncores):
    L = cfg.DEPTH
    f = lambda a: np.ascontiguousarray(np.asarray(a, dtype=np.float32))
    shared = {
        "c_ctx": f(inp["c_ctx"]).reshape(cfg.KC, 128),
        "ada_w": f(inp["ada_w"]), "ada_b": f(inp["ada_b"]),
        "norm_mix": f(inp["norm_mix"]), "norm_ffn": f(inp["norm_ffn"]),
        "w_in": f(inp["w_in"]), "w_out": f(inp["w_out"]),
        "diff_lambda": f(inp["diff_lambda"]).reshape(L, 256), "diff_subln": f(inp["diff_subln"]),
        "s5_a_re": f(inp["s5_a_re"]).reshape(L, 2, -1), "s5_a_im": f(inp["s5_a_im"]).reshape(L, 2, -1),
        "s5_log_dt": f(inp["s5_log_dt"]),
        "s5_b_re": f(inp["s5_b_re"]).reshape(L, 2, -1, 16), "s5_b_im": f(inp["s5_b_im"]).reshape(L, 2, -1, 16),
        "s5_c_re": f(inp["s5_c_re"]).reshape(L, 2, -1, 64), "s5_c_im": f(inp["s5_c_im"]).reshape(L, 2, -1, 64),
        "s5_d": f(inp["s5_d"]).reshape(L, -1), "s5_glu_w": f(inp["s5_glu_w"]), "s5_glu_b": f(inp["s5_glu_b"]),
        "mla_q_norm": f(inp["mla_q_norm"]), "mla_kv_norm": f(inp["mla_kv_norm"]),
        "mla_w_uq": f(inp["mla_w_uq"]), "mla_w_ukv": f(inp["mla_w_ukv"]),
        "ret_decay": f(inp["ret_decay"]).reshape(L, -1), "ret_norm": f(inp["ret_norm"]),
        "moe_wr": np.ascontiguousarray(np.concatenate([f(inp["moe_wg"]), f(inp["moe_we"])], axis=-1)),
        "moe_br": np.ascontiguousarray(np.concatenate([f(inp["moe_bg"]), f(inp["moe_be"])], axis=-1)),
        "moe_w_gate": f(inp["moe_w_gate"]), "moe_w_up": f(inp["moe_w_up"]), "moe_w_down": f(inp["moe_w_down"]),
        "final_norm": f(inp["final_norm"]).reshape(1, -1),
    }
    shared.update(host_consts(cfg))
    maps = []
    for b in range(ncores):
        m = dict(shared)
        m["x"] = f(inp["x"][b])
        m["ctx"] = f(inp["ctx"][b])
        m["c"] = f(inp["c"][b]).reshape(cfg.KC, 128)
        maps.append(m)
    return maps


def kernel(**inputs):
    x = np.asarray(inputs["x"])
    B, N, D = x.shape
    cfg = Cfg(D=D, N=N, LC=np.asarray(inputs["ctx"]).shape[1], DEPTH=np.asarray(inputs["ada_w"]).shape[0], B=B)
    nc = build_program(cfg)
    maps = make_in_maps(cfg, inputs, B)
    res = run_bass_kernel_spmd(nc, maps, core_ids=list(range(B)))
    return np.stack([np.asarray(r["out"]) for r in res.results]).astype(np.float32)
```

```python
import math
import os
from contextlib import ExitStack
import numpy as np
import concourse.bass as bass
import concourse.mybir as mybir
from concourse.bass_utils import run_bass_kernel_spmd

F32 = mybir.dt.float32
BF16 = mybir.dt.bfloat16
AF = mybir.ActivationFunctionType
ALU = mybir.AluOpType
AX = mybir.AxisListType
NORM_EPS = 1e-6


class Cfg:
    def __init__(s, D=4096, N=4096, LC=256, DEPTH=2, B=4):
        s.D, s.N, s.LC, s.DEPTH, s.B = D, N, LC, DEPTH, B
        s.T = N + LC
        s.GW = D // 4
        s.DH = 64
        s.DIFF_HEADS = s.GW // 128
        s.S5_CH, s.S5_P = 16, 64
        s.S5_G = s.GW // 16
        s.MLA_HEADS = s.GW // 128
        s.QR = 3 * D // 16
        s.KVR = D // 16
        s.RET_HEADS = s.GW // 128
        s.RQK = s.RET_HEADS * 64
        s.E, s.FF = 16, D // 4
        s.splits = [s.GW, s.GW, s.GW, s.GW, s.QR, s.KVR, 64, s.RQK, s.RQK, s.GW, s.GW]
        s.names = ["dq", "dk", "dv", "su", "cq", "ckv", "kr", "rq", "rk", "rv", "rg"]
        s.off = {}
        o = 0
        for n, w in zip(s.names, s.splits):
            s.off[n] = (o, w)
            o += w
        s.INW = o
        s.KC = D // 128
        s.TB = 512
        s.blocks = [(i * 512, 512, False) for i in range(N // 512)]
        c0 = N
        while c0 < s.T:
            w = min(512, s.T - c0)
            s.blocks.append((c0, w, True))
            c0 += w
        s.NTB = len(s.blocks)
        s.KT = s.T // 128


class Buf:
    __slots__ = ("t", "w", "r", "name")

    def __init__(s, t, name=""):
        s.t, s.w, s.r, s.name = t, None, {}, name

    def __getitem__(s, idx):
        return V(s.t[idx], (s,))

    def sub(s):
        return Buf(s.t, s.name)


class V:
    __slots__ = ("ap", "bufs")

    def __init__(s, ap, bufs):
        s.ap, s.bufs = ap, bufs

    def __getitem__(s, idx):
        return V(s.ap[idx], s.bufs)

    def rearrange(s, pat, **kw):
        return V(s.ap.rearrange(pat, **kw), s.bufs)


class K:
    ND = 12

    def __init__(s, nc, st):
        s.nc, s.st = nc, st
        s.E = {"pe": nc.tensor, "act": nc.scalar, "dve": nc.vector, "pool": nc.gpsimd, "sp": nc.sync}
        s.esem = {e: st.enter_context(nc.semaphore("es_" + e)) for e in ("pe", "act", "dve", "pool")}
        s.cnt = {e: 0 for e in s.esem}
        s.known = {e: {} for e in s.E}
        s.dsl = {q: [[st.enter_context(nc.semaphore("ds_%s%d" % (q, i))), 0] for i in range(s.ND)] for q in ("sp", "pool", "act")}
        s.dnext = {"sp": 0, "pool": 0, "act": 0}
        s.nbuf = 0
        s.psb = None
        s.psi = 0
        s.held = []

    def sb(s, shape, dt=F32, name=None, stack=None):
        s.nbuf += 1
        nm = "%s_%d" % (name or "b", s.nbuf)
        t = (stack or s.st).enter_context(s.nc.sbuf_tensor(nm, list(shape), dt))
        return Buf(t, nm)

    def dram(s, shape, dt=F32, name=None):
        s.nbuf += 1
        nm = "%s_%d" % (name or "d", s.nbuf)
        return Buf(s.nc.dram_tensor(nm, list(shape), dt), nm)

    def init_psum(s):
        s.psb = [Buf(s.st.enter_context(s.nc.psum_tensor("ps%d" % i, [128, 512], F32)), "ps%d" % i) for i in range(8)]

    def ps(s, hold=False):
        while True:
            b = s.psb[s.psi]
            s.psi = (s.psi + 1) % 8
            if b not in s.held:
                break
        if hold:
            s.held.append(b)
        return b

    def release(s, b):
        s.held.remove(b)

    def _toks(s, reads, writes):
        toks = []
        for v in reads:
            for b in v.bufs:
                if b.w is not None:
                    toks.append(b.w)
        for v in writes:
            for b in v.bufs:
                if b.w is not None:
                    toks.append(b.w)
                toks.extend(b.r.values())
        return toks

    def _wait(s, eng, toks):
        kn = s.known[eng]
        for tok in toks:
            key = tok[0]
            val = tok[1]
            if key == eng and eng == "pe":
                continue
            if kn.get(key, 0) >= val:
                continue
            sem = s.esem[key] if isinstance(key, str) else s.dsl[key[0]][key[1]][0]
            s.E[eng].wait_ge(sem, val)
            kn[key] = val

    def _mark(s, tok, rkey, reads, writes):
        for v in writes:
            for b in v.bufs:
                b.w = tok
                b.r = {}
        for v in reads:
            for b in v.bufs:
                if b.w is tok:
                    continue
                b.r[rkey] = tok

    def op(s, eng, fn, reads, writes):
        reads = [v for v in reads if isinstance(v, V)]
        s._wait(eng, s._toks(reads, writes))
        ins = fn(s.E[eng])
        s.cnt[eng] += 1
        ins.then_inc(s.esem[eng], 1)
        tok = (eng, s.cnt[eng])
        s._mark(tok, eng, reads, writes)

    def dma(s, q, out, in_):
        si = s.dnext[q]
        s.dnext[q] = (si + 1) % s.ND
        slot = s.dsl[q][si]
        key = (q, si)
        toks = s._toks([in_], [out])
        if slot[1] > 0:
            toks.append((key, slot[1]))
        s._wait(q, toks)
        ins = s.E[q].dma_start(out=out.ap, in_=in_.ap)
        slot[1] += 16
        ins.then_inc(slot[0], 16)
        tok = (key, slot[1])
        s._mark(tok, key, [in_], [out])

    def barrier(s):
        toks = [(e, c) for e, c in s.cnt.items() if c > 0]
        for q in s.dsl:
            for i, (sem, val) in enumerate(s.dsl[q]):
                if val > 0:
                    toks.append(((q, i), val))
        for eng in s.E:
            s._wait(eng, toks)

    def scope(s):
        k = s

        class _Scope(ExitStack):
            def __exit__(self, *a):
                if a[0] is None:
                    k.barrier()
                return super().__exit__(*a)

        return _Scope()

    def finish(s, outs):
        toks = []
        for b in outs:
            if b.w is not None:
                toks.append(b.w)
        s._wait("sp", toks)

    @staticmethod
    def _a(x):
        return x.ap if isinstance(x, V) else x

    def mm(s, out, lhsT, rhs, start=True, stop=True):
        s.op("pe", lambda e: e.matmul(out.ap, lhsT.ap, rhs.ap, start=start, stop=stop), [lhsT, rhs] + ([] if start else [out]), [out])

    def tr(s, out, in_, ident):
        s.op("pe", lambda e: e.transpose(out.ap, in_.ap, ident.ap), [in_, ident], [out])

    def act(s, out, in_, func, bias=0.0, scale=1.0, accum=None):
        kw = {}
        if accum is not None:
            kw["accum_out"] = accum.ap
        s.op("act", lambda e: e.activation(out=out.ap, in_=in_.ap, func=func, bias=s._a(bias), scale=s._a(scale), **kw),
             [in_, bias, scale], [out] + ([accum] if accum is not None else []))

    def tt(s, out, a, b, op, eng="dve"):
        s.op(eng, lambda e: e.tensor_tensor(out=out.ap, in0=a.ap, in1=b.ap, op=op), [a, b], [out])

    def ts(s, out, a, s1, op0, s2=None, op1=None, eng="dve", accum=None):
        kw = {}
        if accum is not None:
            kw["accum_out"] = accum.ap
        if op1 is None:
            s.op(eng, lambda e: e.tensor_scalar(out=out.ap, in0=a.ap, scalar1=s._a(s1), scalar2=None, op0=op0, **kw), [a, s1], [out])
        else:
            s.op(eng, lambda e: e.tensor_scalar(out=out.ap, in0=a.ap, scalar1=s._a(s1), scalar2=s._a(s2), op0=op0, op1=op1, **kw),
                 [a, s1, s2], [out] + ([accum] if accum is not None else []))

    def stt(s, out, a, sc, b, op0, op1):
        s.op("dve", lambda e: e.scalar_tensor_tensor(out=out.ap, in0=a.ap, scalar=s._a(sc), in1=b.ap, op0=op0, op1=op1), [a, sc, b], [out])

    def copy(s, out, in_, eng="dve"):
        if eng == "act":
            s.op("act", lambda e: e.copy(out=out.ap, in_=in_.ap), [in_], [out])
        else:
            s.op(eng, lambda e: e.tensor_copy(out=out.ap, in_=in_.ap), [in_], [out])

    def memset(s, out, val, eng="dve"):
        s.op(eng, lambda e: e.memset(out.ap, val), [], [out])

    def recip(s, out, in_):
        s.op("dve", lambda e: e.reciprocal(out=out.ap, in_=in_.ap), [in_], [out])

    def scan(s, out, d0, d1, init, op0=ALU.mult, op1=ALU.add):
        s.op("dve", lambda e: e.tensor_tensor_scan(out.ap, d0.ap, d1.ap, s._a(init), op0, op1), [d0, d1, init], [out])

    def rmax(s, out, in_):
        s.op("dve", lambda e: e.reduce_max(out=out.ap, in_=in_.ap, axis=AX.X), [in_], [out])

    def rsum(s, out, in_):
        s.op("dve", lambda e: e.reduce_sum(out=out.ap, in_=in_.ap, axis=AX.X), [in_], [out])

    def rstd(s, out, ss, n, tmp):
        s.ts(tmp, ss, 1.0 / n, ALU.mult, NORM_EPS, ALU.add)
        s.act(tmp, tmp, AF.Sqrt)
        s.recip(out, tmp)


def chunks(n, c=128):
    return [(i, min(c, n - i)) for i in range(0, n, c)]


def build_program(cfg, debug=()):
    nc = bass.Bass("TRN2", target_bir_lowering=False)
    D, N, LC, T, KC = cfg.D, cfg.N, cfg.LC, cfg.T, cfg.KC
    L = cfg.DEPTH
    dbg = {}

    def ext(name, shape, dt=F32):
        return Buf(nc.dram_tensor(name, list(shape), dt, kind="ExternalInput"), name)

    I = {}
    I["x"] = ext("x", [N, D])
    I["ctx"] = ext("ctx", [LC, D])
    I["c"] = ext("c", [KC, 128])
    I["c_ctx"] = ext("c_ctx", [KC, 128])
    I["ada_w"] = ext("ada_w", [L, D, 6 * D])
    I["ada_b"] = ext("ada_b", [L, 6 * D])
    I["norm_mix"] = ext("norm_mix", [L, D])
    I["norm_ffn"] = ext("norm_ffn", [L, D])
    I["w_in"] = ext("w_in", [L, D, cfg.INW])
    I["w_out"] = ext("w_out", [L, D, D])
    I["diff_lambda"] = ext("diff_lambda", [L, 256])
    I["diff_subln"] = ext("diff_subln", [L, 128])
    I["s5_a_re"] = ext("s5_a_re", [L, 2, cfg.S5_G * 64])
    I["s5_a_im"] = ext("s5_a_im", [L, 2, cfg.S5_G * 64])
    I["s5_log_dt"] = ext("s5_log_dt", [L, 2, cfg.S5_G])
    I["s5_b_re"] = ext("s5_b_re", [L, 2, cfg.S5_G * 64, 16])
    I["s5_b_im"] = ext("s5_b_im", [L, 2, cfg.S5_G * 64, 16])
    I["s5_c_re"] = ext("s5_c_re", [L, 2, cfg.S5_G * 16, 64])
    I["s5_c_im"] = ext("s5_c_im", [L, 2, cfg.S5_G * 16, 64])
    I["s5_d"] = ext("s5_d", [L, cfg.GW])
    I["s5_glu_w"] = ext("s5_glu_w", [L, cfg.GW, cfg.GW])
    I["s5_glu_b"] = ext("s5_glu_b", [L, cfg.GW])
    I["mla_q_norm"] = ext("mla_q_norm", [L, cfg.QR])
    I["mla_kv_norm"] = ext("mla_kv_norm", [L, cfg.KVR])
    I["mla_w_uq"] = ext("mla_w_uq", [L, cfg.QR, cfg.MLA_HEADS * 192])
    I["mla_w_ukv"] = ext("mla_w_ukv", [L, cfg.KVR, cfg.MLA_HEADS * 256])
    I["ret_decay"] = ext("ret_decay", [L, 2 * cfg.RET_HEADS])
    I["ret_norm"] = ext("ret_norm", [L, cfg.GW])
    I["moe_wr"] = ext("moe_wr", [L, D, 20])
    I["moe_br"] = ext("moe_br", [L, 20])
    I["moe_w_gate"] = ext("moe_w_gate", [L, 16, D, cfg.FF])
    I["moe_w_up"] = ext("moe_w_up", [L, 16, D, cfg.FF])
    I["moe_w_down"] = ext("moe_w_down", [L, 16, cfg.FF, D])
    I["final_norm"] = ext("final_norm", [1, D])
    I["ident"] = ext("ident", [128, 128])
    I["rperm"] = ext("rperm", [128, 128])
    I["ropecs"] = ext("ropecs", [2, 128, N])
    OUT = Buf(nc.dram_tensor("out", [N, D], F32, kind="ExternalOutput"), "out")

    with ExitStack() as st:
        k = K(nc, st)
        k.init_psum()
        ident = k.sb([128, 128], F32, "ident")
        identb = k.sb([128, 128], BF16, "identb")
        ones = k.sb([128, 128], F32, "ones")
        onesb = k.sb([128, 128], BF16, "onesb")
        rperm = k.sb([128, 128], BF16, "rperm")
        k.dma("sp", ident[:, :], I["ident"][:, :])
        k.dma("pool", identb[:, :], I["ident"][:, :])
        k.dma("pool", rperm[:, :], I["rperm"][:, :])
        k.memset(ones[:, :], 1.0)
        k.memset(onesb[:, :], 1.0)

        xres = k.dram([T, D], F32, "xres")
        xres_t = [xres.sub() for _ in range(T // 128)]
        modbc = [[[k.dram([128, D], F32, "modbc") for j in range(6)] for r in range(2)] for l in range(L)]
        hT = k.dram([cfg.NTB, 128, KC, 512], BF16, "hT")
        hT_b = [hT.sub() for _ in range(cfg.NTB)]
        dwd = k.dram([T, 16], F32, "dw")
        dwd_t = [dwd.sub() for _ in range(T // 128)]

        for i in range(N // 128):
            k.dma("sp", V(xres.t[i * 128:(i + 1) * 128, :], (xres_t[i],)), I["x"][i * 128:(i + 1) * 128, :])
        for i in range(LC // 128):
            j = N // 128 + i
            k.dma("sp", V(xres.t[j * 128:(j + 1) * 128, :], (xres_t[j],)), I["ctx"][i * 128:(i + 1) * 128, :])

        def stage_ada():
            with k.scope() as ls:
                rep = [k.sb([128, KC, 128], F32, "rep", ls) for r in range(2)]
                crow = k.sb([KC, 128], F32, "crow", ls)
                colv = k.sb([128, KC], F32, "colv", ls)
                for r, src in enumerate((I["c"], I["c_ctx"])):
                    k.dma("sp", crow[:, :], src[:, :])
                    k.act(crow[:, :], crow[:, :], AF.Silu)
                    p = k.ps()
                    k.tr(p[:, 0:KC], crow[:, :], ident[0:KC, 0:KC])
                    k.copy(colv[:, :], p[:, 0:KC])
                    for c in range(KC):
                        k.ts(rep[r][:, c, :], ones[:, :], colv[:, c:c + 1], ALU.mult, eng=("dve" if c % 2 else "pool"))
                KB = min(8, KC)
                wt = [k.sb([128, KB, 512], F32, "adaw", ls) for _ in range(3)]
                brow = [k.sb([1, 512], F32, "brow", ls) for _ in range(2)]
                grow = [k.sb([1, 512], F32, "grow", ls) for _ in range(2)]
                stg = [k.sb([128, 512], F32, "stg", ls) for _ in range(2)]
                gbc = k.sb([128, 512], F32, "gbc", ls)
                wi = 0
                si = 0
                for l in range(L):
                    wv = I["ada_w"].t[l].rearrange("(c p) n -> p c n", p=128)
                    for ct in range(6 * D // 512):
                        j = (ct * 512) // D
                        col = ct * 512 - j * D
                        pp = [k.ps(), k.ps()]
                        br_ = brow[ct % 2]
                        k.dma("sp", br_[:, :], I["ada_b"][l:l + 1, ct * 512:(ct + 1) * 512])
                        for kb in range(0, KC, KB):
                            w = wt[wi % 3]
                            wi += 1
                            k.dma("sp", w[:, :, :], V(wv[:, kb:kb + KB, ct * 512:(ct + 1) * 512], (I["ada_w"],)))
                            for c in range(KB):
                                for r in range(2):
                                    k.mm(pp[r][:, :], rep[r][:, kb + c, :], w[:, c, :], start=(kb + c == 0), stop=False)
                        for r in range(2):
                            k.mm(pp[r][:, :], ones[0:1, :], br_[0:1, :], start=False, stop=True)
                        if j in (1, 4):
                            pg = k.ps()
                            gr_ = grow[ct % 2]
                            k.dma("sp", gr_[:, :], (I["norm_mix"] if j == 1 else I["norm_ffn"])[l:l + 1, col:col + 512])
                            k.mm(pg[:, :], ones[0:1, :], gr_[0:1, :])
                            k.copy(gbc[:, :], pg[:, :], eng="act")
                        for r in range(2):
                            sg = stg[si % 2]
                            si += 1
                            if j in (1, 4):
                                k.stt(sg[:, :], pp[r][:, :], 1.0, gbc[:, :], ALU.add, ALU.mult)
                            else:
                                k.copy(sg[:, :], pp[r][:, :], eng="act")
                            k.dma("sp", modbc[l][r][j][:, col:col + 512], sg[:, :])

        def stage_norm(l, which, blocks, router):
            jS, jA = (0, 1) if which == 0 else (3, 4)
            with k.scope() as ls:
                A = k.sb([128, D], F32, "A", ls)
                S = k.sb([128, D], F32, "S", ls)
                xt = [k.sb([128, D], F32, "xt", ls) for _ in range(2)]
                junk = k.sb([128, D], BF16, "junk", ls)
                hb = [k.sb([128, KC, 512], BF16, "hb", ls) for _ in range(2)]
                sm = k.sb([128, 8], F32, "sm", ls)
                if router:
                    hf = k.sb([128, KC * 128], F32, "hf", ls)
                    wr = k.sb([128, KC, 20], F32, "wr", ls)
                    brr = k.sb([1, 20], F32, "brr", ls)
                    k.dma("sp", wr[:, :, :], V(I["moe_wr"].t[l].rearrange("(c p) n -> p c n", p=128), (I["moe_wr"],)))
                    k.dma("sp", brr[:, :], I["moe_br"][l:l + 1, :])
                    rt = k.sb([128, 64], F32, "rt", ls)
                    dwt = k.sb([128, 16], F32, "dwt", ls)
                cur_r = None
                xi = 0
                for bi in blocks:
                    t0, bw, isc = cfg.blocks[bi]
                    r = 1 if isc else 0
                    if r != cur_r:
                        k.dma("sp", A[:, :], modbc[l][r][jA][:, :])
                        k.dma("sp", S[:, :], modbc[l][r][jS][:, :])
                        cur_r = r
                    hbb = hb[bi % 2]
                    for sub in range(bw // 128):
                        ti = (t0 + sub * 128) // 128
                        x = xt[xi % 2]
                        xi += 1
                        k.dma("sp", x[:, :], V(xres.t[ti * 128:(ti + 1) * 128, :], (xres_t[ti],)))
                        k.act(junk[:, :], x[:, :], AF.Square, accum=sm[:, 0:1])
                        k.rstd(sm[:, 1:2], sm[:, 0:1], D, sm[:, 2:3])
                        k.stt(x[:, :], x[:, :], sm[:, 1:2], A[:, :], ALU.mult, ALU.mult)
                        k.tt(x[:, :], x[:, :], S[:, :], ALU.add, eng="pool")
                        for c4 in range(0, KC, 4):
                            p = k.ps()
                            for c in range(c4, min(c4 + 4, KC)):
                                k.tr(p[:, (c - c4) * 128:(c - c4 + 1) * 128], x[:, c * 128:(c + 1) * 128], ident[:, :])
                            nn = min(4, KC - c4)
                            pv = p[:, 0:nn * 128].rearrange("p (c t) -> p c t", c=nn)
                            k.copy(hbb[:, c4:c4 + nn, sub * 128:(sub + 1) * 128], pv, eng=("dve" if router or (c4 // 4) % 2 else "act"))
                            if router:
                                k.ts(hf[:, c4 * 128:(c4 + nn) * 128], p[:, 0:nn * 128], 1.0, ALU.mult)
                        if router:
                            pr = k.ps()
                            for c in range(KC):
                                k.mm(pr[:, 0:20], hf[:, c * 128:(c + 1) * 128], wr[:, c, :], start=(c == 0), stop=False)
                            k.mm(pr[:, 0:20], ones[0:1, :], brr[0:1, :], start=False, stop=True)
                            lg = rt[:, 0:20]
                            k.copy(lg, pr[:, 0:20])
                            gmx = rt[:, 20:21]
                            k.rmax(gmx, rt[:, 0:4])
                            k.ts(rt[:, 24:28], rt[:, 0:4], gmx, ALU.subtract)
                            k.act(rt[:, 28:32], rt[:, 24:28], AF.Exp, accum=rt[:, 21:22])
                            k.recip(rt[:, 22:23], rt[:, 21:22])
                            k.ts(rt[:, 24:28], rt[:, 24:28], 0.0, ALU.is_ge)
                            k.ts(rt[:, 28:32], rt[:, 24:28], 1.0, ALU.subtract, 1e30, ALU.mult)
                            for g in range(4):
                                k.ts(rt[:, 32 + 4 * g:36 + 4 * g], rt[:, 4 + 4 * g:8 + 4 * g], rt[:, 28 + g:29 + g], ALU.add)
                            em = rt[:, 32:48]
                            k.rmax(rt[:, 48:49], em)
                            k.ts(dwt[:, :], em, rt[:, 48:49], ALU.is_ge)
                            k.stt(rt[:, 4:20], dwt[:, :], -1e30, em, ALU.mult, ALU.add)
                            k.rmax(rt[:, 49:50], rt[:, 4:20])
                            k.ts(rt[:, 32:48], rt[:, 4:20], rt[:, 49:50], ALU.is_ge)
                            k.tt(rt[:, 50:51], rt[:, 49:50], rt[:, 48:49], ALU.subtract)
                            k.act(rt[:, 51:52], rt[:, 50:51], AF.Exp)
                            k.ts(rt[:, 52:53], rt[:, 51:52], 1.0, ALU.add)
                            k.recip(rt[:, 52:53], rt[:, 52:53])
                            k.tt(rt[:, 53:54], rt[:, 52:53], rt[:, 22:23], ALU.mult)
                            k.tt(rt[:, 54:55], rt[:, 22:23], rt[:, 53:54], ALU.subtract)
                            k.ts(dwt[:, :], dwt[:, :], rt[:, 53:54], ALU.mult)
                            k.stt(dwt[:, :], rt[:, 32:48], rt[:, 54:55], dwt[:, :], ALU.mult, ALU.add)
                            k.dma("sp", V(dwd.t[ti * 128:(ti + 1) * 128, :], (dwd_t[ti],)), dwt[:, :])
                    k.dma("sp", V(hT.t[bi][:, :, 0:bw], (hT_b[bi],)), hbb[:, :, 0:bw])

        def wtiles_in():
            tl = []
            for nm in cfg.names:
                g0, gw = cfg.off[nm]
                for c0 in range(0, gw, 256):
                    tl.append((nm, c0, g0 + c0, min(256, gw - c0)))
            return tl

        WIN_T = wtiles_in()
        WIN_IDX = {(nm, c0): i for i, (nm, c0, a, w) in enumerate(WIN_T)}
        FC_ = cfg.FF // 128
        NFT = (cfg.FF + 255) // 256
        winb, woutb, wgb, wub, wdb = [], [], [], [], []
        for l in range(L):
            d = k.dram([len(WIN_T), 128, KC, 256], BF16, "winb")
            winb.append((d, [d.sub() for _ in WIN_T]))
            d = k.dram([D // 256, 128, KC, 256], BF16, "woutb")
            woutb.append((d, [d.sub() for _ in range(D // 256)]))
            d = k.dram([16, NFT, 128, KC, 256], BF16, "wgb")
            wgb.append((d, [[d.sub() for _ in range(NFT)] for _ in range(16)]))
            d = k.dram([16, NFT, 128, KC, 256], BF16, "wub")
            wub.append((d, [[d.sub() for _ in range(NFT)] for _ in range(16)]))
            d = k.dram([16, D // 512, 128, FC_, 512], BF16, "wdb")
            wdb.append((d, [[d.sub() for _ in range(D // 512)] for _ in range(16)]))

        bgq = []

        def bg_step(n):
            for _ in range(min(n, len(bgq))):
                bgq.pop(0)()

        def bg_flush():
            bg_step(len(bgq))

        def conv_list(l, with_win):
            out = []
            if with_win:
                wv0 = I["w_in"].t[l].rearrange("(c p) n -> p c n", p=128)
                for i, (nm, c0, a0, w) in enumerate(WIN_T):
                    out.append(lambda i=i, a0=a0, w=w, wv0=wv0: k.dma("pool", V(winb[l][0].t[i][:, :, 0:w], (winb[l][1][i],)), V(wv0[:, :, a0:a0 + w], (I["w_in"],))))
            wv1 = I["w_out"].t[l].rearrange("(c p) n -> p c n", p=128)
            for ct in range(D // 256):
                out.append(lambda ct=ct, wv1=wv1: k.dma("pool", V(woutb[l][0].t[ct], (woutb[l][1][ct],)), V(wv1[:, :, ct * 256:(ct + 1) * 256], (I["w_out"],))))
            for e in range(16):
                wgv = I["moe_w_gate"].t[l, e].rearrange("(c p) f -> p c f", p=128)
                wuv = I["moe_w_up"].t[l, e].rearrange("(c p) f -> p c f", p=128)
                wdv = I["moe_w_down"].t[l, e].rearrange("(c p) n -> p c n", p=128)
                for j in range(NFT):
                    f0 = j * 256
                    fw = min(256, cfg.FF - f0)
                    out.append(lambda e=e, j=j, f0=f0, fw=fw, wgv=wgv: k.dma("pool", V(wgb[l][0].t[e, j][:, :, 0:fw], (wgb[l][1][e][j],)), V(wgv[:, :, f0:f0 + fw], (I["moe_w_gate"],))))
                    out.append(lambda e=e, j=j, f0=f0, fw=fw, wuv=wuv: k.dma("pool", V(wub[l][0].t[e, j][:, :, 0:fw], (wub[l][1][e][j],)), V(wuv[:, :, f0:f0 + fw], (I["moe_w_up"],))))
                for ct in range(D // 512):
                    out.append(lambda e=e, ct=ct, wdv=wdv: k.dma("pool", V(wdb[l][0].t[e, ct], (wdb[l][1][e][ct],)), V(wdv[:, :, ct * 512:(ct + 1) * 512], (I["moe_w_down"],))))
            return out

        def fm_alloc(nrows, name, dt=BF16):
            nch = (nrows + 127) // 128
            d = k.dram([nch, cfg.NTB, 128, 512], dt, name)
            return d, [[d.sub() for _ in range(cfg.NTB)] for _ in range(nch)]

        FM = {}
        for nm in ("dq", "dk", "su", "cq", "ckv", "kr", "rq", "rk", "rg"):
            FM[nm] = fm_alloc(cfg.off[nm][1], "fm_" + nm)

        def fmv(nm, ch, tb, rows=128, w=512):
            d, subs = FM[nm]
            return V(d.t[ch, tb][0:rows, 0:w], (subs[ch][tb],))

        TMV = {}
        for nm, nh in (("dv", cfg.DIFF_HEADS), ("rv", cfg.RET_HEADS)):
            d = k.dram([nh, 128, cfg.KT, 128], BF16, "tm_" + nm)
            TMV[nm] = (d, [[d.sub() for _ in range(cfg.KT)] for _ in range(nh)])
        rq_bc = k.dram([cfg.NTB, 128, 512], F32, "rq_bc")
        rq_bc_s = [rq_bc.sub() for _ in range(cfg.NTB)]
        rkv_bc = k.dram([cfg.NTB, 128, 512], F32, "rkv_bc")
        rkv_bc_s = [rkv_bc.sub() for _ in range(cfg.NTB)]
        rkv_tm = k.dram([128, cfg.KT], F32, "rkv_tm")
        rkv_tm_s = [rkv_tm.sub() for _ in range(cfg.NTB)]

        def row_to_cols(dst, src_row, n, tmp_row):
            k.dma("sp", tmp_row[0:1, 0:n], src_row)
            p = k.ps()
            for i, (c0, cw) in enumerate(chunks(n)):
                k.tr(p[0:cw, i:i + 1], tmp_row[0:1, c0:c0 + cw], ident[0:1, 0:1])
                k.copy(dst[0:cw, i:i + 1], p[0:cw, i:i + 1])

        def stage_proj(l):
            wv = I["w_in"].t[l].rearrange("(c p) n -> p c n", p=128)
            csv = I["ropecs"].t.rearrange("a p n -> p a n")
            with k.scope() as ls:
                hb = [k.sb([128, KC, 512], BF16, "hb", ls) for _ in range(2)]
                wt = [k.sb([128, KC, 256], BF16, "wt", ls) for _ in range(3)]
                cs = [k.sb([128, 2, 512], F32, "cs", ls) for _ in range(2)]
                stg = [k.sb([128, 512], BF16, "stg", ls) for _ in range(3)]
                xs = [k.sb([128, 512], BF16, "xs", ls) for _ in range(2)]
                t1 = [k.sb([128, 512], F32, "t1", ls) for _ in range(2)]
                t2 = [k.sb([128, 512], F32, "t2", ls) for _ in range(2)]
                sqf = [k.sb([128, 512], F32, "sqf", ls) for _ in range(2)]
                rbc = k.sb([128, 512], F32, "rbc", ls)
                rtmp = k.sb([128, 512], F32, "rtmp", ls)
                qn = k.sb([128, 16], F32, "qn", ls)
                rtm = k.sb([128, 4], F32, "rtm", ls)
                trow = k.sb([1, max(cfg.QR, 128)], F32, "trow", ls)
                row_to_cols(qn[:, 0:8], I["mla_q_norm"][l:l + 1, :], cfg.QR, trow)
                row_to_cols(qn[:, 8:16], I["mla_kv_norm"][l:l + 1, :], cfg.KVR, trow)
                cnt = {"w": 0, "s": 0, "x": 0}

                def load_w(c0, w, nm_=None, c0rel=None):
                    t = wt[cnt["w"] % 3]
                    cnt["w"] += 1
                    i = WIN_IDX[(nm_, c0rel)]
                    k.dma("sp" if cnt["w"] % 2 else "pool", t[:, :, 0:w], V(winb[l][0].t[i][:, :, 0:w], (winb[l][1][i],)))
                    return t

                for bi, (t0, bw, isc) in enumerate(cfg.blocks):
                    h = hb[bi % 2]
                    k.dma("sp", h[:, :, 0:bw], V(hT.t[bi][:, :, 0:bw], (hT_b[bi],)))
                    c_ = cs[bi % 2]
                    if not isc:
                        k.dma("sp", c_[:, :, :], V(csv[:, :, t0:t0 + 512], (I["ropecs"],)))
                    for nm in cfg.names:
                        g0, gw = cfg.off[nm]
                        if nm in ("dv", "rv"):
                            d, subs = TMV[nm]
                            for c0 in range(0, gw, 256):
                                w = min(256, gw - c0)
                                t = load_w(g0 + c0, w, nm, c0)
                                for sub in range(bw // 128):
                                    p = k.ps()
                                    for c in range(KC):
                                        k.mm(p[:, 0:w], h[:, c, sub * 128:(sub + 1) * 128], t[:, c, 0:w], start=(c == 0), stop=(c == KC - 1))
                                    sg = stg[cnt["s"] % 3]
                                    cnt["s"] += 1
                                    k.copy(sg[:, 0:w], p[:, 0:w], eng=("act" if cnt["s"] % 2 else "dve"))
                                    kt = (t0 + sub * 128) // 128
                                    for hh in range(w // 128):
                                        head = (c0 + hh * 128) // 128
                                        k.dma("sp", V(d.t[head][:, kt, :], (subs[head][kt],)), sg[:, hh * 128:(hh + 1) * 128])
                            continue
                        nch = (gw + 127) // 128
                        pss = k.ps(hold=True) if nm in ("cq", "ckv") else None
                        for c0 in range(0, gw, 256):
                            w = min(256, gw - c0)
                            t = load_w(g0 + c0, w, nm, c0)
                            for j0 in range(0, w, 128):
                                cw = min(128, w - j0)
                                ch = (c0 + j0) // 128
                                p = k.ps()
                                for c in range(KC):
                                    k.mm(p[0:cw, 0:bw], t[:, c, j0:j0 + cw], h[:, c, 0:bw], start=(c == 0), stop=(c == KC - 1))
                                sg = stg[cnt["s"] % 3]
                                cnt["s"] += 1
                                if nm in ("dq", "dk", "kr", "rq", "rk") and not isc:
                                    x_ = xs[cnt["x"] % 2]
                                    a1 = t1[cnt["x"] % 2]
                                    a2 = t2[cnt["x"] % 2]
                                    cnt["x"] += 1
                                    k.copy(x_[0:cw, 0:bw], p[0:cw, 0:bw], eng="act")
                                    p2 = k.ps()
                                    k.mm(p2[0:cw, 0:bw], rperm[0:cw, 0:cw], x_[0:cw, 0:bw])
                                    k.tt(a1[0:cw, 0:bw], x_[0:cw, 0:bw], c_[0:cw, 0, 0:bw], ALU.mult, eng="pool")
                                    k.tt(a2[0:cw, 0:bw], p2[0:cw, 0:bw], c_[0:cw, 1, 0:bw], ALU.mult)
                                    k.tt(sg[0:cw, 0:bw], a1[0:cw, 0:bw], a2[0:cw, 0:bw], ALU.add, eng="pool")
                                elif nm in ("cq", "ckv"):
                                    qi = (0 if nm == "cq" else 8) + ch
                                    k.act(sg[0:cw, 0:bw], p[0:cw, 0:bw], AF.Copy, scale=qn[0:cw, qi:qi + 1])
                                    sq = sqf[ch % 2]
                                    k.act(sq[0:cw, 0:bw], p[0:cw, 0:bw], AF.Square)
                                    k.mm(pss[:, 0:bw], ones[0:cw, :], sq[0:cw, 0:bw], start=(ch == 0), stop=(ch == nch - 1))
                                elif nm == "rg":
                                    k.act(sg[0:cw, 0:bw], p[0:cw, 0:bw], AF.Silu)
                                else:
                                    k.copy(sg[0:cw, 0:bw], p[0:cw, 0:bw], eng=("act" if cnt["s"] % 2 else "dve"))
                                k.dma("sp", fmv(nm, ch, bi, cw, bw), sg[0:cw, 0:bw])
                        if pss is not None:
                            k.rstd(rbc[:, 0:bw], pss[:, 0:bw], gw, rtmp[:, 0:bw])
                            k.release(pss)
                            if nm == "cq":
                                k.dma("sp", V(rq_bc.t[bi][:, 0:bw], (rq_bc_s[bi],)), rbc[:, 0:bw])
                            else:
                                k.dma("sp", V(rkv_bc.t[bi][:, 0:bw], (rkv_bc_s[bi],)), rbc[:, 0:bw])
                                pt = k.ps()
                                ns = bw // 128
                                for sub in range(ns):
                                    k.tr(pt[:, sub:sub + 1], rbc[0:1, sub * 128:(sub + 1) * 128], ident[0:1, 0:1])
                                k.copy(rtm[:, 0:ns], pt[:, 0:ns])
                                kt0 = t0 // 128
                                k.dma("sp", V(rkv_tm.t[:, kt0:kt0 + ns], (rkv_tm_s[bi],)), rtm[:, 0:ns])

        GWc = cfg.GW // 128
        FM["mix"] = fm_alloc(D, "fm_mix")

        def bcast_col(dst, src11):
            p = k.ps()
            k.mm(p[:, 0:1], ones[0:1, :], src11)
            k.copy(dst, p[:, 0:1])

        def attn_core(ls_bufs, q_parts, k_parts, vT, ktiles, scale, bw):
            pT = ls_bufs
            po = k.ps(hold=True)
            pd = k.ps(hold=True)
            n = len(ktiles)
            for i, kt in enumerate(ktiles):
                p = k.ps()
                for j, (qv, kf) in enumerate(zip(q_parts, k_parts)):
                    k.mm(p[:, 0:bw], kf(kt), qv, start=(j == 0), stop=(j == len(q_parts) - 1))
                pt = pT[i % len(pT)]
                k.act(pt[:, 0:bw], p[:, 0:bw], AF.Exp, scale=scale)
                k.mm(po[:, 0:bw], vT[:, kt, :], pt[:, 0:bw], start=(i == 0), stop=(i == n - 1))
                k.mm(pd[:, 0:bw], onesb[:, :], pt[:, 0:bw], start=(i == 0), stop=(i == n - 1))
            return po, pd

        def q_blocks(need_ctx):
            return [(bi, t0, bw, isc) for bi, (t0, bw, isc) in enumerate(cfg.blocks) if (need_ctx or not isc)]

        def key_tiles(isc):
            return list(range(N // 128, cfg.KT)) if isc else list(range(cfg.KT))

        def stage_diff(l, need_ctx):
            lam_init = 0.8 - 0.6 * math.exp(-0.3 * l)
            with k.scope() as ls:
                kT = k.sb([128, T], BF16, "kT", ls)
                vT = k.sb([128, cfg.KT, 128], BF16, "vT", ls)
                qT = [k.sb([128, 512], BF16, "qT", ls) for _ in range(2)]
                pT = [k.sb([128, 512], BF16, "pT", ls) for _ in range(3)]
                a0 = k.sb([128, 512], F32, "a0", ls)
                a1 = k.sb([128, 512], F32, "a1", ls)
                rr = k.sb([128, 512], F32, "rr", ls)
                og = k.sb([128, 512], BF16, "og", ls)
                lr = k.sb([1, 256], F32, "lr", ls)
                lt = k.sb([1, 128], F32, "lt", ls)
                sc_ = k.sb([128, 8], F32, "sc", ls)
                trow = k.sb([1, 128], F32, "trow", ls)
                k.dma("sp", lr[:, :], I["diff_lambda"][l:l + 1, :])
                k.tt(lt[0:1, 0:64], lr[0:1, 0:64], lr[0:1, 64:128], ALU.mult)
                k.tt(lt[0:1, 64:128], lr[0:1, 128:192], lr[0:1, 192:256], ALU.mult)
                k.rsum(lr[0:1, 0:1], lt[0:1, 0:64])
                k.rsum(lr[0:1, 1:2], lt[0:1, 64:128])
                k.act(lr[0:1, 2:4], lr[0:1, 0:2], AF.Exp)
                k.tt(lr[0:1, 4:5], lr[0:1, 3:4], lr[0:1, 2:3], ALU.subtract)
                k.ts(lr[0:1, 5:6], lr[0:1, 4:5], -lam_init, ALU.add)
                bcast_col(sc_[:, 0:1], lr[0:1, 5:6])
                row_to_cols(sc_[:, 1:2], I["diff_subln"][l:l + 1, :], 128, trow)
                k.ts(sc_[:, 2:3], sc_[:, 1:2], 1.0 - lam_init, ALU.mult)
                qi = 0
                for h in range(cfg.DIFF_HEADS):
                    for bi, (t0, bw, isc) in enumerate(cfg.blocks):
                        k.dma("sp", kT[:, t0:t0 + bw], fmv("dk", h, bi, 128, bw))
                    dv, dvs = TMV["dv"]
                    k.dma("sp", vT[:, :, :], V(dv.t[h], tuple(dvs[h])))
                    for bi, t0, bw, isc in q_blocks(need_ctx):
                        q = qT[qi % 2]
                        qi += 1
                        k.dma("sp", q[:, 0:bw], fmv("dq", h, bi, 128, bw))
                        kts = key_tiles(isc)
                        for j in range(2):
                            r0, r1 = j * 64, (j + 1) * 64
                            po, pd = attn_core(pT, [q[r0:r1, 0:bw]], [lambda kt, r0=r0, r1=r1: kT[r0:r1, kt * 128:(kt + 1) * 128]],
                                               vT, kts, 64 ** -0.5, bw)
                            dst = a0 if j == 0 else a1
                            k.recip(rr[:, 0:bw], pd[:, 0:bw])
                            k.tt(dst[:, 0:bw], po[:, 0:bw], rr[:, 0:bw], ALU.mult)
                            k.release(po)
                            k.release(pd)
                        k.stt(a0[:, 0:bw], a1[:, 0:bw], sc_[:, 0:1], a0[:, 0:bw], ALU.mult, ALU.add)
                        k.act(a1[:, 0:bw], a0[:, 0:bw], AF.Square)
                        pss = k.ps()
                        k.mm(pss[:, 0:bw], ones[:, :], a1[:, 0:bw])
                        k.rstd(rr[:, 0:bw], pss[:, 0:bw], 128, a1[:, 0:bw])
                        k.stt(og[:, 0:bw], a0[:, 0:bw], sc_[:, 2:3], rr[:, 0:bw], ALU.mult, ALU.mult)
                        k.dma("sp", fmv("mix", h, bi, 128, bw), og[:, 0:bw])
                        bg_step(2)

        MH = cfg.MLA_HEADS
        mqn = fm_alloc(MH * 128, "mqn")
        mqr = fm_alloc(MH * 128, "mqr")
        mkn = fm_alloc(MH * 128, "mkn")
        mvd = k.dram([MH, 128, cfg.KT, 128], BF16, "mv")
        mvs = [[mvd.sub() for _ in range(cfg.KT)] for _ in range(MH)]

        def stage_mla(l, need_ctx):
            qch = chunks(cfg.QR)
            kch = chunks(cfg.KVR)
            csv = I["ropecs"].t.rearrange("a p n -> p a n")
            with k.scope() as ls:
                wq = k.sb([128, len(qch), MH * 192], BF16, "wq", ls)
                wkv = k.sb([128, len(kch), MH * 256], BF16, "wkv", ls)
                for ci, (c0, cw) in enumerate(qch):
                    k.dma("pool", wq[0:cw, ci, :], I["mla_w_uq"][l, c0:c0 + cw, :])
                for ci, (c0, cw) in enumerate(kch):
                    k.dma("pool", wkv[0:cw, ci, :], I["mla_w_ukv"][l, c0:c0 + cw, :])
                rtm_sb = k.sb([128, cfg.KT], F32, "rtm_sb", ls)
                k.dma("sp", rtm_sb[:, :], V(rkv_tm.t[:, :], tuple(rkv_tm_s)))
                cqs = k.sb([128, len(qch), 512], BF16, "cqs", ls)
                cks = k.sb([128, len(kch), 512], BF16, "cks", ls)
                rqt = k.sb([128, 512], F32, "rqt", ls)
                rkt = k.sb([128, 512], F32, "rkt", ls)
                cs_ = k.sb([128, 2, 512], F32, "cs", ls)
                stg = [k.sb([128, 512], BF16, "stg", ls) for _ in range(3)]
                xs = k.sb([128, 512], BF16, "xs", ls)
                b1 = k.sb([128, 512], F32, "b1", ls)
                b2 = k.sb([128, 512], F32, "b2", ls)
                si = 0
                for bi, (t0, bw, isc) in enumerate(cfg.blocks):
                    for ci, (c0, cw) in enumerate(qch):
                        k.dma("sp", cqs[0:cw, ci, 0:bw], fmv("cq", ci, bi, cw, bw))
                    for ci, (c0, cw) in enumerate(kch):
                        k.dma("sp", cks[0:cw, ci, 0:bw], fmv("ckv", ci, bi, cw, bw))
                    k.dma("sp", rqt[:, 0:bw], V(rq_bc.t[bi][:, 0:bw], (rq_bc_s[bi],)))
                    k.dma("sp", rkt[:, 0:bw], V(rkv_bc.t[bi][:, 0:bw], (rkv_bc_s[bi],)))
                    if not isc:
                        k.dma("sp", cs_[:, :, :], V(csv[:, :, t0:t0 + 512], (I["ropecs"],)))
                    for h in range(MH):
                        if need_ctx or not isc:
                            p = k.ps()
                            for ci, (c0, cw) in enumerate(qch):
                                k.mm(p[:, 0:bw], wq[0:cw, ci, h * 192:h * 192 + 128], cqs[0:cw, ci, 0:bw], start=(ci == 0), stop=(ci == len(qch) - 1))
                            sg = stg[si % 3]
                            si += 1
                            k.tt(sg[:, 0:bw], p[:, 0:bw], rqt[:, 0:bw], ALU.mult)
                            k.dma("sp", V(mqn[0].t[h, bi][:, 0:bw], (mqn[1][h][bi],)), sg[:, 0:bw])
                            p = k.ps()
                            for ci, (c0, cw) in enumerate(qch):
                                k.mm(p[0:64, 0:bw], wq[0:cw, ci, h * 192 + 128:h * 192 + 192], cqs[0:cw, ci, 0:bw], start=(ci == 0), stop=(ci == len(qch) - 1))
                            sg = stg[si % 3]
                            si += 1
                            if isc:
                                k.tt(sg[0:64, 0:bw], p[0:64, 0:bw], rqt[0:64, 0:bw], ALU.mult)
                            else:
                                k.tt(xs[0:64, 0:bw], p[0:64, 0:bw], rqt[0:64, 0:bw], ALU.mult)
                                p2 = k.ps()
                                k.mm(p2[0:64, 0:bw], rperm[0:64, 0:64], xs[0:64, 0:bw])
                                k.tt(b1[0:64, 0:bw], xs[0:64, 0:bw], cs_[0:64, 0, 0:bw], ALU.mult, eng="pool")
                                k.tt(b2[0:64, 0:bw], p2[0:64, 0:bw], cs_[0:64, 1, 0:bw], ALU.mult)
                                k.tt(sg[0:64, 0:bw], b1[0:64, 0:bw], b2[0:64, 0:bw], ALU.add, eng="pool")
                            k.dma("sp", V(mqr[0].t[h, bi][0:64, 0:bw], (mqr[1][h][bi],)), sg[0:64, 0:bw])
                        p = k.ps()
                        for ci, (c0, cw) in enumerate(kch):
                            k.mm(p[:, 0:bw], wkv[0:cw, ci, h * 256:h * 256 + 128], cks[0:cw, ci, 0:bw], start=(ci == 0), stop=(ci == len(kch) - 1))
                        sg = stg[si % 3]
                        si += 1
                        k.tt(sg[:, 0:bw], p[:, 0:bw], rkt[:, 0:bw], ALU.mult)
                        k.dma("sp", V(mkn[0].t[h, bi][:, 0:bw], (mkn[1][h][bi],)), sg[:, 0:bw])
                        for sub in range(bw // 128):
                            kt = t0 // 128 + sub
                            p = k.ps()
                            for ci, (c0, cw) in enumerate(kch):
                                k.mm(p[:, 0:128], cks[0:cw, ci, sub * 128:(sub + 1) * 128], wkv[0:cw, ci, h * 256 + 128:h * 256 + 256], start=(ci == 0), stop=(ci == len(kch) - 1))
                            sg = stg[si % 3]
                            si += 1
                            k.ts(sg[:, 0:128], p[:, 0:128], rtm_sb[:, kt:kt + 1], ALU.mult)
                            k.dma("sp", V(mvd.t[h][:, kt, :], (mvs[h][kt],)), sg[:, 0:128])
            with k.scope() as ls:
                kTn = k.sb([128, T], BF16, "kTn", ls)
                kTr = k.sb([64, T], BF16, "kTr", ls)
                vT = k.sb([128, cfg.KT, 128], BF16, "vT", ls)
                qn = [k.sb([128, 512], BF16, "qn", ls) for _ in range(2)]
                qr = [k.sb([64, 512], BF16, "qr", ls) for _ in range(2)]
                pT = [k.sb([128, 512], BF16, "pT", ls) for _ in range(3)]
                rr = k.sb([128, 512], F32, "rr", ls)
                og = k.sb([128, 512], BF16, "og", ls)
                for bi, (t0, bw, isc) in enumerate(cfg.blocks):
                    k.dma("sp", kTr[:, t0:t0 + bw], fmv("kr", 0, bi, 64, bw))
                qi = 0
                for h in range(MH):
                    for bi, (t0, bw, isc) in enumerate(cfg.blocks):
                        k.dma("sp", kTn[:, t0:t0 + bw], V(mkn[0].t[h, bi][:, 0:bw], (mkn[1][h][bi],)))
                    k.dma("sp", vT[:, :, :], V(mvd.t[h], tuple(mvs[h])))
                    for bi, t0, bw, isc in q_blocks(need_ctx):
                        qa, qb_ = qn[qi % 2], qr[qi % 2]
                        qi += 1
                        k.dma("sp", qa[:, 0:bw], V(mqn[0].t[h, bi][:, 0:bw], (mqn[1][h][bi],)))
                        k.dma("sp", qb_[:, 0:bw], V(mqr[0].t[h, bi][0:64, 0:bw], (mqr[1][h][bi],)))
                        po, pd = attn_core(pT, [qa[:, 0:bw], qb_[0:64, 0:bw]],
                                           [lambda kt: kTn[:, kt * 128:(kt + 1) * 128], lambda kt: kTr[0:64, kt * 128:(kt + 1) * 128]],
                                           vT, key_tiles(isc), 192 ** -0.5, bw)
                        k.recip(rr[:, 0:bw], pd[:, 0:bw])
                        k.tt(og[:, 0:bw], po[:, 0:bw], rr[:, 0:bw], ALU.mult)
                        k.release(po)
                        k.release(pd)
                        k.dma("sp", fmv("mix", 2 * GWc + h, bi, 128, bw), og[:, 0:bw])
                        bg_step(2)

        def stage_ret(l, need_ctx):
            RH = cfg.RET_HEADS
            ksc = 64 ** -0.5
            NI = cfg.KT + 8
            with k.scope() as ls:
                dr_ = k.sb([1, 2 * RH], F32, "dr", ls)
                lgb_ = k.sb([128, 2 * RH], F32, "lg", ls)
                nlg = k.sb([128, 2 * RH], F32, "nlg", ls)
                d0i = k.sb([128, 512], mybir.dt.int32, "d0i", ls)
                D0 = k.sb([128, 512], F32, "D0", ls)
                ioi = k.sb([128, NI], mybir.dt.int32, "ioi", ls)
                iof = k.sb([128, NI], F32, "iof", ls)
                ctf = k.sb([128, NI], F32, "ctf", ls)
                ctb = k.sb([128, NI], F32, "ctb", ls)
                Ef = k.sb([128, 512], F32, "Ef", ls)
                Eb = k.sb([128, 512], F32, "Eb", ls)
                Wd = [k.sb([128, 512], F32, "Wd", ls) for _ in range(4)]
                w1 = k.sb([128, 512], F32, "w1", ls)
                w2 = k.sb([128, 512], F32, "w2", ls)
                w3 = k.sb([128, 512], F32, "w3", ls)
                kT = k.sb([128, T], BF16, "kT", ls)
                vT = k.sb([128, cfg.KT, 128], BF16, "vT", ls)
                qT = [k.sb([128, 512], BF16, "qT", ls) for _ in range(2)]
                gT = [k.sb([128, 512], BF16, "gT", ls) for _ in range(2)]
                pT = [k.sb([128, 512], BF16, "pT", ls) for _ in range(3)]
                ya = k.sb([128, 512], F32, "ya", ls)
                yb = k.sb([128, 512], F32, "yb", ls)
                rr = k.sb([128, 512], F32, "rr", ls)
                og = k.sb([128, 512], BF16, "og", ls)
                gcol = k.sb([128, GWc], F32, "gcol", ls)
                trow = k.sb([1, cfg.GW], F32, "trow", ls)
                row_to_cols(gcol, I["ret_norm"][l:l + 1, :], cfg.GW, trow)
                k.dma("sp", dr_[:, :], I["ret_decay"][l:l + 1, :])
                k.act(dr_[:, :], dr_[:, :], AF.Exp, scale=-1.0)
                k.ts(dr_[:, :], dr_[:, :], 1.0, ALU.add)
                k.act(dr_[:, :], dr_[:, :], AF.Ln)
                p = k.ps()
                k.mm(p[:, 0:2 * RH], ones[0:1, :], dr_[0:1, :])
                k.ts(lgb_[:, :], p[:, 0:2 * RH], -1.0, ALU.mult)
                k.ts(nlg[:, :], lgb_[:, :], -1.0, ALU.mult)
                k.op("pool", lambda e: e.iota(d0i.t[:, :], [[1, 512]], base=0, channel_multiplier=-1), [], [d0i[:, :]])
                k.copy(D0[:, :], d0i[:, :])
                k.op("pool", lambda e: e.iota(ioi.t[:, :], [[128, NI]], base=0, channel_multiplier=0), [], [ioi[:, :]])
                k.copy(iof[:, :], ioi[:, :])
                lnk = math.log(ksc)
                qi = 0
                for h in range(RH):
                    lf, lb = lgb_[:, h:h + 1], lgb_[:, RH + h:RH + h + 1]
                    nlb = nlg[:, RH + h:RH + h + 1]
                    k.act(ctf[:, :], iof[:, :], AF.Exp, scale=lf)
                    k.act(ctb[:, :], iof[:, :], AF.Exp, scale=lb)
                    k.act(Ef[:, :], D0[:, :], AF.Exp, scale=lf, bias=lnk)
                    k.act(Eb[:, :], D0[:, :], AF.Exp, scale=nlb, bias=lnk)
                    for oi in range(4):
                        off = float(oi * 128)
                        k.ts(w1[:, :], D0[:, :], -off, ALU.add, 0.0, ALU.max)
                        k.act(w1[:, :], w1[:, :], AF.Exp, scale=lf, bias=lnk)
                        k.ts(w2[:, :], D0[:, :], -off, ALU.add, 0.0, ALU.min)
                        k.act(w2[:, :], w2[:, :], AF.Exp, scale=nlb, bias=lnk)
                        k.ts(w3[:, :], D0[:, :], -off, ALU.add, 0.0, ALU.is_ge)
                        k.tt(w1[:, :], w1[:, :], w2[:, :], ALU.subtract)
                        k.tt(w1[:, :], w1[:, :], w3[:, :], ALU.mult)
                        k.tt(Wd[oi][:, :], w1[:, :], w2[:, :], ALU.add)
                    rch, rro = h // 2, (h % 2) * 64
                    for bi, (t0, bw, isc) in enumerate(cfg.blocks):
                        k.dma("sp", kT[:, t0:t0 + bw], fmv("rk", rch, bi, 128, bw))
                    rv_, rvs = TMV["rv"]
                    k.dma("sp", vT[:, :, :], V(rv_.t[h], tuple(rvs[h])))
                    for bi, t0, bw, isc in q_blocks(need_ctx):
                        q, g = qT[qi % 2], gT[qi % 2]
                        qi += 1
                        k.dma("sp", q[:, 0:bw], fmv("rq", rch, bi, 128, bw))
                        k.dma("sp", g[:, 0:bw], fmv("rg", h, bi, 128, bw))
                        kts = key_tiles(isc)
                        po = k.ps(hold=True)
                        for i, kt in enumerate(kts):
                            p = k.ps()
                            k.mm(p[:, 0:bw], kT[rro:rro + 64, kt * 128:(kt + 1) * 128], q[rro:rro + 64, 0:bw])
                            pt = pT[i % 3]
                            kctx = kt >= N // 128
                            if kctx == isc:
                                s0 = (kt * 128 - N) if isc else kt * 128
                                tq = 0 if isc else t0
                                if s0 + 128 <= tq:
                                    ci_ = (tq - s0) // 128
                                    k.stt(pt[:, 0:bw], p[:, 0:bw], ctf[:, ci_:ci_ + 1], Ef[:, 0:bw], ALU.mult, ALU.mult)
                                elif s0 >= tq + bw:
                                    ci_ = (s0 - tq) // 128
                                    k.stt(pt[:, 0:bw], p[:, 0:bw], ctb[:, ci_:ci_ + 1], Eb[:, 0:bw], ALU.mult, ALU.mult)
                                else:
                                    k.tt(pt[:, 0:bw], p[:, 0:bw], Wd[(s0 - tq) // 128][:, 0:bw], ALU.mult)
                            else:
                                c0 = kt * 128 - N
                                cf = (t0 - (c0 - LC)) // 128
                                cb = (N + c0 - t0) // 128
                                k.ts(w1[:, 0:bw], Ef[:, 0:bw], ctf[:, cf:cf + 1], ALU.mult)
                                k.stt(w1[:, 0:bw], Eb[:, 0:bw], ctb[:, cb:cb + 1], w1[:, 0:bw], ALU.mult, ALU.add)
                                k.tt(pt[:, 0:bw], p[:, 0:bw], w1[:, 0:bw], ALU.mult)
                            k.mm(po[:, 0:bw], vT[:, kt, :], pt[:, 0:bw], start=(i == 0), stop=(i == len(kts) - 1))
                        k.copy(ya[:, 0:bw], po[:, 0:bw])
                        k.release(po)
                        k.act(yb[:, 0:bw], ya[:, 0:bw], AF.Square)
                        pss = k.ps()
                        k.mm(pss[:, 0:bw], ones[:, :], yb[:, 0:bw])
                        k.rstd(rr[:, 0:bw], pss[:, 0:bw], 128, yb[:, 0:bw])
                        k.stt(ya[:, 0:bw], ya[:, 0:bw], gcol[:, h:h + 1], rr[:, 0:bw], ALU.mult, ALU.mult)
                        k.tt(og[:, 0:bw], ya[:, 0:bw], g[:, 0:bw], ALU.mult)
                        k.dma("sp", fmv("mix", 3 * GWc + h, bi, 128, bw), og[:, 0:bw])
                        bg_step(2)

        s5g = fm_alloc(cfg.GW, "s5g")
        PI = math.pi

        def stage_s5(l, need_ctx):
            G = cfg.S5_G
            NP = G // 2
            lat_b = [(bi, t0, bw) for bi, (t0, bw, isc) in enumerate(cfg.blocks) if not isc]
            ctx_b = [(bi, t0, bw) for bi, (t0, bw, isc) in enumerate(cfg.blocks) if isc]
            with k.scope() as ls:
                trow = k.sb([1, max(G * 64, cfg.GW)], F32, "trow", ls)
                bc = k.sb([128, G], F32, "bc", ls)
                are = k.sb([128, 2, NP], F32, "are", ls)
                aim = k.sb([128, 2, NP], F32, "aim", ls)
                dtc = k.sb([128, 2, NP], F32, "dtc", ls)
                rr = k.sb([128, 2, NP], F32, "rr", ls)
                th = k.sb([128, 2, NP], F32, "th", ls)
                cr = k.sb([128, 2, NP], F32, "cr", ls)
                ci = k.sb([128, 2, NP], F32, "ci", ls)
                ncr = k.sb([128, 2, NP], F32, "ncr", ls)
                q1 = k.sb([128, 2, NP], F32, "q1", ls)
                q2 = k.sb([128, 2, NP], F32, "q2", ls)
                q3 = k.sb([128, 2, NP], F32, "q3", ls)
                q4 = k.sb([128, 2, NP], F32, "q4", ls)
                dcol = k.sb([32, NP], F32, "dcol", ls)
                jfi = k.sb([128, 512], mybir.dt.int32, "jfi", ls)
                jf = k.sb([128, 512], F32, "jf", ls)

                def sincos(out_s, out_c, ang, tmp, tmpi):
                    k.ts(tmp, ang, 1.0 / (2 * PI), ALU.mult)
                    k.copy(tmpi, tmp)
                    k.copy(tmp, tmpi)
                    k.stt(tmp, tmp, -2 * PI, ang, ALU.mult, ALU.add)
                    k.ts(out_c, tmp, PI, ALU.is_gt)
                    k.stt(tmp, out_c, -2 * PI, tmp, ALU.mult, ALU.add)
                    k.ts(out_c, tmp, -PI, ALU.is_lt)
                    k.stt(tmp, out_c, 2 * PI, tmp, ALU.mult, ALU.add)
                    k.act(out_s, tmp, AF.Sin)
                    k.ts(tmp, tmp, 0.5 * PI, ALU.add)
                    k.ts(out_c, tmp, PI, ALU.is_gt)
                    k.stt(tmp, out_c, -2 * PI, tmp, ALU.mult, ALU.add)
                    k.act(out_c, tmp, AF.Sin)

                negpi = k.sb([128, 1], F32, "negpi", ls)
                k.memset(negpi[:, :], -PI)
                k.op("pool", lambda e: e.iota(jfi.t[:, :], [[1, 512]], base=1, channel_multiplier=0), [], [jfi[:, :]])
                k.copy(jf[:, :], jfi[:, :])
                for dr in range(2):
                    row_to_cols(are[:, dr, :], I["s5_a_re"][l, dr:dr + 1, :], G * 64, trow)
                    row_to_cols(aim[:, dr, :], I["s5_a_im"][l, dr:dr + 1, :], G * 64, trow)
                    k.dma("sp", trow[0:1, 0:G], I["s5_log_dt"][l, dr:dr + 1, :])
                    k.act(trow[0:1, 0:G], trow[0:1, 0:G], AF.Exp)
                    p = k.ps()
                    k.mm(p[:, 0:G], ones[0:1, :], trow[0:1, 0:G])
                    k.copy(bc[:, :], p[:, 0:G])
                    bcv = bc[:, :].rearrange("p (n two) -> p n two", two=2)
                    k.copy(dtc[0:64, dr, :], bcv[0:64, :, 0])
                    k.copy(dtc[64:128, dr, :], bcv[64:128, :, 1])
                k.dma("sp", trow[0:1, 0:cfg.GW], I["s5_d"][l:l + 1, :])
                p = k.ps()
                for i in range(NP):
                    k.tr(p[0:32, i:i + 1], trow[0:1, i * 32:(i + 1) * 32], ident[0:1, 0:1])
                k.copy(dcol[:, :], p[0:32, 0:NP])
                fl = lambda b: b[:, :, :].rearrange("p a n -> p (a n)")
                k.tt(fl(q1), fl(are), fl(dtc), ALU.mult)
                k.act(fl(rr), fl(q1), AF.Exp)
                k.tt(fl(th), fl(aim), fl(dtc), ALU.mult)
                qi32 = k.sb([128, 2 * NP], mybir.dt.int32, "qi32", ls)
                sincos(fl(q1), fl(q2), fl(th), fl(q3), qi32[:, :])
                k.tt(fl(q1), fl(q1), fl(rr), ALU.mult)
                k.tt(fl(q2), fl(q2), fl(rr), ALU.mult)
                k.ts(fl(q2), fl(q2), -1.0, ALU.add)
                k.tt(fl(q3), fl(are), fl(are), ALU.mult)
                k.tt(fl(q4), fl(aim), fl(aim), ALU.mult)
                k.tt(fl(q3), fl(q3), fl(q4), ALU.add)
                k.recip(fl(q3), fl(q3))
                k.tt(fl(cr), fl(q2), fl(are), ALU.mult)
                k.tt(fl(q4), fl(q1), fl(aim), ALU.mult)
                k.tt(fl(cr), fl(cr), fl(q4), ALU.add)
                k.tt(fl(cr), fl(cr), fl(q3), ALU.mult)
                k.tt(fl(ci), fl(q1), fl(are), ALU.mult)
                k.tt(fl(q4), fl(q2), fl(aim), ALU.mult)
                k.tt(fl(ci), fl(ci), fl(q4), ALU.subtract)
                k.tt(fl(ci), fl(ci), fl(q3), ALU.mult)
                k.ts(fl(ncr), fl(cr), -1.0, ALU.mult)

                ang = k.sb([128, 512], F32, "ang", ls)
                atmp = k.sb([128, 512], F32, "atmp", ls)
                cosJ = k.sb([128, 512], F32, "cosJ", ls)
                sinJ = k.sb([128, 512], F32, "sinJ", ls)
                tre = k.sb([128, 512], F32, "tre", ls)
                tim = k.sb([128, 512], F32, "tim", ls)
                rJ = k.sb([128, 512], F32, "rJ", ls)
                z1 = k.sb([128, 512], F32, "z1", ls)
                z2 = k.sb([128, 512], F32, "z2", ls)
                zr = k.sb([128, 512], F32, "zr", ls)
                zi = k.sb([128, 512], F32, "zi", ls)
                xr = k.sb([128, 512], F32, "xr", ls)
                xi = k.sb([128, 512], F32, "xi", ls)
                zp = k.sb([128, 2], F32, "zp", ls)
                Bw = [k.sb([128, 32], F32, "Bw", ls) for _ in range(2)]
                Bl = [k.sb([32, 128], F32, "Bl", ls) for _ in range(2)]
                Cw = [k.sb([32, 128], F32, "Cw", ls) for _ in range(2)]
                Cl = [k.sb([128, 32], F32, "Cl", ls) for _ in range(2)]
                uf = k.sb([32, T], F32, "uf", ls)
                ya = k.sb([32, T], F32, "ya", ls)
                g1 = k.sb([32, T], F32, "g1", ls)
                gb = k.sb([32, T], BF16, "gb", ls)
                for bw_ in Bw:
                    k.memset(bw_[:, :], 0.0)
                for cw_ in Cw:
                    k.memset(cw_[:, :], 0.0)
                GC = math.sqrt(2.0 / math.pi) * 2.0
                for pk in range(NP):
                    ch, ro = (pk * 32) // 128, (pk * 32) % 128
                    for bi, (t0, bw, isc) in enumerate(cfg.blocks):
                        d_, subs_ = FM["su"]
                        k.dma("pool", uf[:, t0:t0 + bw], V(d_.t[ch, bi][ro:ro + 32, 0:bw], (subs_[ch][bi],)))
                    for dr in range(2):
                        for ri, nm in enumerate(("s5_b_re", "s5_b_im")):
                            src = I[nm]
                            k.dma("sp", Bw[ri][0:64, 0:16], src[l, dr, pk * 128:pk * 128 + 64, :])
                            k.dma("sp", Bw[ri][64:128, 16:32], src[l, dr, pk * 128 + 64:pk * 128 + 128, :])
                            p = k.ps()
                            k.tr(p[0:32, 0:128], Bw[ri][:, :], ident[:, :])
                            k.copy(Bl[ri][:, :], p[0:32, 0:128])
                        for ri, nm in enumerate(("s5_c_re", "s5_c_im")):
                            src = I[nm]
                            k.dma("sp", Cw[ri][0:16, 0:64], src[l, dr, pk * 32:pk * 32 + 16, :])
                            k.dma("sp", Cw[ri][16:32, 64:128], src[l, dr, pk * 32 + 16:pk * 32 + 32, :])
                            p = k.ps()
                            k.tr(p[:, 0:32], Cw[ri][:, :], ident[0:32, 0:32])
                            if ri == 0:
                                k.copy(Cl[ri][:, :], p[:, 0:32])
                            else:
                                k.ts(Cl[ri][:, :], p[:, 0:32], -1.0, ALU.mult)
                        k.ts(ang[:, :], jf[:, :], th[:, dr, pk:pk + 1], ALU.mult)
                        sincos(sinJ[:, :], cosJ[:, :], ang[:, :], atmp[:, :], jfi[:, :])
                        k.ts(tre[:, :], cosJ[:, :], cr[:, dr, pk:pk + 1], ALU.mult)
                        k.stt(tre[:, :], sinJ[:, :], ci[:, dr, pk:pk + 1], tre[:, :], ALU.mult, ALU.add)
                        k.ts(tim[:, :], cosJ[:, :], ci[:, dr, pk:pk + 1], ALU.mult)
                        k.stt(tim[:, :], sinJ[:, :], ncr[:, dr, pk:pk + 1], tim[:, :], ALU.mult, ALU.add)
                        k.memset(rJ[:, :], 1.0)
                        k.ts(rJ[:, :], rJ[:, :], rr[:, dr, pk:pk + 1], ALU.mult)
                        k.memset(zp[:, :], 0.0)
                        seq = (ctx_b + lat_b) if dr == 0 else (ctx_b[::-1] + lat_b[::-1])
                        for bi, t0, bw in seq:
                            isc = cfg.blocks[bi][2]
                            rv = (lambda v: v[:, ::-1]) if dr == 1 else (lambda v: v)
                            pbr = k.ps()
                            k.mm(pbr[:, 0:bw], Bl[0][:, :], uf[:, t0:t0 + bw])
                            pbi = k.ps()
                            k.mm(pbi[:, 0:bw], Bl[1][:, :], uf[:, t0:t0 + bw])
                            br_, bi_ = rv(pbr[:, 0:bw]), rv(pbi[:, 0:bw])
                            k.tt(z1[:, 0:bw], tre[:, 0:bw], br_, ALU.mult)
                            k.tt(z2[:, 0:bw], tim[:, 0:bw], bi_, ALU.mult)
                            k.tt(zr[:, 0:bw], z1[:, 0:bw], z2[:, 0:bw], ALU.subtract, eng="pool")
                            k.tt(z1[:, 0:bw], tre[:, 0:bw], bi_, ALU.mult)
                            k.tt(z2[:, 0:bw], tim[:, 0:bw], br_, ALU.mult)
                            k.tt(zi[:, 0:bw], z1[:, 0:bw], z2[:, 0:bw], ALU.add, eng="pool")
                            k.scan(zr[:, 0:bw], rJ[:, 0:bw], zr[:, 0:bw], zp[:, 0:1])
                            k.scan(zi[:, 0:bw], rJ[:, 0:bw], zi[:, 0:bw], zp[:, 1:2])
                            k.tt(z1[:, 0:bw], cosJ[:, 0:bw], zr[:, 0:bw], ALU.mult)
                            k.tt(z2[:, 0:bw], sinJ[:, 0:bw], zi[:, 0:bw], ALU.mult, eng="pool")
                            k.tt(xr[:, 0:bw], z1[:, 0:bw], z2[:, 0:bw], ALU.subtract)
                            k.tt(z1[:, 0:bw], sinJ[:, 0:bw], zr[:, 0:bw], ALU.mult, eng="pool")
                            k.tt(z2[:, 0:bw], cosJ[:, 0:bw], zi[:, 0:bw], ALU.mult)
                            k.tt(xi[:, 0:bw], z1[:, 0:bw], z2[:, 0:bw], ALU.add, eng="pool")
                            k.copy(zp[:, 0:1], xr[:, bw - 1:bw])
                            k.copy(zp[:, 1:2], xi[:, bw - 1:bw])
                            if isc and not need_ctx:
                                continue
                            py = k.ps()
                            k.mm(py[0:32, 0:bw], Cl[0][:, :], xr[:, 0:bw], start=True, stop=False)
                            k.mm(py[0:32, 0:bw], Cl[1][:, :], xi[:, 0:bw], start=False, stop=True)
                            if dr == 0:
                                k.stt(ya[:, t0:t0 + bw], uf[:, t0:t0 + bw], dcol[:, pk:pk + 1], py[0:32, 0:bw], ALU.mult, ALU.add)
                            else:
                                k.tt(ya[:, t0:t0 + bw], ya[:, t0:t0 + bw], py[0:32, 0:bw][:, ::-1], ALU.add)
                    tr_ = [(bi, t0, bw) for bi, (t0, bw, isc) in enumerate(cfg.blocks) if (need_ctx or not isc)]
                    t_lo, t_hi = tr_[0][1], tr_[-1][1] + tr_[-1][2]
                    yv = ya[:, t_lo:t_hi]
                    gv = g1[:, t_lo:t_hi]
                    k.tt(gv, yv, yv, ALU.mult)
                    k.ts(gv, gv, 0.044715, ALU.mult, 1.0, ALU.add)
                    k.tt(gv, gv, yv, ALU.mult, eng="pool")
                    k.act(gv, gv, AF.Sigmoid, scale=GC)
                    k.tt(gb[:, t_lo:t_hi], gv, yv, ALU.mult)
                    for bi, t0, bw in tr_:
                        k.dma("sp", V(s5g[0].t[ch, bi][ro:ro + 32, 0:bw], (s5g[1][ch][bi],)), gb[:, t0:t0 + bw])
            with k.scope() as ls:
                gw = k.sb([128, GWc, cfg.GW], BF16, "gw", ls)
                k.dma("pool", gw[:, :, :], V(I["s5_glu_w"].t[l].rearrange("(c p) n -> p c n", p=128), (I["s5_glu_w"],)))
                bcol = k.sb([128, GWc], F32, "bcol", ls)
                trow = k.sb([1, cfg.GW], F32, "trow", ls)
                row_to_cols(bcol, I["s5_glu_b"][l:l + 1, :], cfg.GW, trow)
                gin = [k.sb([128, GWc, 512], BF16, "gin", ls) for _ in range(2)]
                gt = [k.sb([128, 512], F32, "gt", ls) for _ in range(2)]
                og = [k.sb([128, 512], BF16, "og", ls) for _ in range(2)]
                n = 0
                for bi, t0, bw, isc in q_blocks(need_ctx):
                    gi = gin[bi % 2]
                    for c in range(GWc):
                        k.dma("sp", gi[:, c, 0:bw], V(s5g[0].t[c, bi][:, 0:bw], (s5g[1][c][bi],)))
                    for oc in range(GWc):
                        p = k.ps()
                        for c in range(GWc):
                            k.mm(p[:, 0:bw], gw[:, c, oc * 128:(oc + 1) * 128], gi[:, c, 0:bw], start=(c == 0), stop=(c == GWc - 1))
                        g_, o_ = gt[n % 2], og[n % 2]
                        n += 1
                        k.act(g_[:, 0:bw], p[:, 0:bw], AF.Sigmoid, bias=bcol[:, oc:oc + 1])
                        k.tt(o_[:, 0:bw], g_[:, 0:bw], gi[:, oc, 0:bw], ALU.mult)
                        k.dma("sp", fmv("mix", GWc + oc, bi, 128, bw), o_[:, 0:bw])

        def xv(ti):
            return V(xres.t[ti * 128:(ti + 1) * 128, :], (xres_t[ti],))

        def stage_wout(l, blocks):
            wv = I["w_out"].t[l].rearrange("(c p) n -> p c n", p=128)
            with k.scope() as ls:
                mb = k.sb([128, KC, 512], BF16, "mb", ls)
                xt = [k.sb([128, D], F32, "xt", ls) for _ in range(4)]
                wt = [k.sb([128, KC, 256], BF16, "wt", ls) for _ in range(3)]
                G1 = k.sb([128, D], F32, "G1", ls)
                tmp = [k.sb([128, 256], F32, "tmp", ls) for _ in range(2)]
                cur_r = None
                wi = 0
                n = 0
                for bi in blocks:
                    t0, bw, isc = cfg.blocks[bi]
                    r = 1 if isc else 0
                    if r != cur_r:
                        k.dma("sp", G1[:, :], modbc[l][r][2][:, :])
                        cur_r = r
                    for c in range(KC):
                        k.dma("sp", mb[:, c, 0:bw], fmv("mix", c, bi, 128, bw))
                    ns = bw // 128
                    for sub in range(ns):
                        k.dma("sp", xt[sub][:, :], xv(t0 // 128 + sub))
                    for ct in range(D // 256):
                        w = wt[wi % 3]
                        wi += 1
                        k.dma("sp" if wi % 2 else "pool", w[:, :, :], V(woutb[l][0].t[ct], (woutb[l][1][ct],)))
                        for sub in range(ns):
                            p = k.ps()
                            for c in range(KC):
                                k.mm(p[:, 0:256], mb[:, c, sub * 128:(sub + 1) * 128], w[:, c, :], start=(c == 0), stop=(c == KC - 1))
                            tm = tmp[n % 2]
                            n += 1
                            k.tt(tm[:, :], p[:, 0:256], G1[:, ct * 256:(ct + 1) * 256], ALU.mult)
                            k.tt(xt[sub][:, ct * 256:(ct + 1) * 256], xt[sub][:, ct * 256:(ct + 1) * 256], tm[:, :], ALU.add, eng="pool")
                    for sub in range(ns):
                        k.dma("sp", xv(t0 // 128 + sub), xt[sub][:, :])

        def stage_moe(l, blocks):
            FC = cfg.FF // 128
            with k.scope() as ls:
                hb = k.sb([128, KC, 512], BF16, "hb", ls)
                acc = [k.sb([128, D], F32, "acc", ls) for _ in range(4)]
                wt = [k.sb([128, KC, 256], BF16, "wt", ls) for _ in range(4)]
                hid = k.sb([128, FC, 512], BF16, "hid", ls)
                wd = [k.sb([128, FC, 512], BF16, "wd", ls) for _ in range(3)]
                HC = min(1024, D)
                G2 = k.sb([128, HC], F32, "G2", ls)
                xh = k.sb([128, HC], F32, "xh", ls)
                sl = [k.sb([128, 512], F32, "sl", ls) for _ in range(2)]
                dws = k.sb([128, 4, 16], F32, "dws", ls)
                wi = 0
                di = 0
                n = 0
                for bi in blocks:
                    t0, bw, isc = cfg.blocks[bi]
                    r = 1 if isc else 0
                    ns = bw // 128
                    k.dma("sp", hb[:, :, 0:bw], V(hT.t[bi][:, :, 0:bw], (hT_b[bi],)))
                    for sub in range(ns):
                        ti = t0 // 128 + sub
                        k.dma("sp", dws[:, sub, :], V(dwd.t[ti * 128:(ti + 1) * 128, :], (dwd_t[ti],)))
                    for e in range(16):
                        bg_step(3)
                        wgv = I["moe_w_gate"].t[l, e].rearrange("(c p) f -> p c f", p=128)
                        wuv = I["moe_w_up"].t[l, e].rearrange("(c p) f -> p c f", p=128)
                        wdv = I["moe_w_down"].t[l, e].rearrange("(c p) n -> p c n", p=128)
                        for f0 in range(0, cfg.FF, 256):
                            fw = min(256, cfg.FF - f0)
                            wg_ = wt[wi % 4]
                            wu_ = wt[(wi + 1) % 4]
                            wi += 2
                            jt = f0 // 256
                            k.dma("sp", wg_[:, :, 0:fw], V(wgb[l][0].t[e, jt][:, :, 0:fw], (wgb[l][1][e][jt],)))
                            k.dma("sp", wu_[:, :, 0:fw], V(wub[l][0].t[e, jt][:, :, 0:fw], (wub[l][1][e][jt],)))
                            for j0 in range(0, fw, 128):
                                fc = (f0 + j0) // 128
                                pg = k.ps()
                                for c in range(KC):
                                    k.mm(pg[:, 0:bw], wg_[:, c, j0:j0 + 128], hb[:, c, 0:bw], start=(c == 0), stop=(c == KC - 1))
                                pu = k.ps()
                                for c in range(KC):
                                    k.mm(pu[:, 0:bw], wu_[:, c, j0:j0 + 128], hb[:, c, 0:bw], start=(c == 0), stop=(c == KC - 1))
                                s_ = sl[n % 2]
                                n += 1
                                k.act(s_[:, 0:bw], pg[:, 0:bw], AF.Silu)
                                k.tt(hid[:, fc, 0:bw], pu[:, 0:bw], s_[:, 0:bw], ALU.mult)
                        for ct in range(D // 512):
                            w = wd[di % 3]
                            di += 1
                            k.dma("sp", w[:, :, :], V(wdb[l][0].t[e, ct], (wdb[l][1][e][ct],)))
                            for sub in range(ns):
                                p = k.ps()
                                for fc in range(FC):
                                    k.mm(p[:, :], hid[:, fc, sub * 128:(sub + 1) * 128], w[:, fc, :], start=(fc == 0), stop=(fc == FC - 1))
                                av = acc[sub][:, ct * 512:(ct + 1) * 512]
                                if e == 0:
                                    k.ts(av, p[:, :], dws[:, sub, e:e + 1], ALU.mult)
                                else:
                                    k.stt(av, p[:, :], dws[:, sub, e:e + 1], av, ALU.mult, ALU.add)
                    for hc in range(0, D, HC):
                        k.dma("sp", G2[:, :], modbc[l][r][5][:, hc:hc + HC])
                        for sub in range(ns):
                            ti = t0 // 128 + sub
                            k.dma("sp", xh[:, :], V(xres.t[ti * 128:(ti + 1) * 128, hc:hc + HC], (xres_t[ti],)))
                            k.tt(acc[sub][:, hc:hc + HC], acc[sub][:, hc:hc + HC], G2[:, :], ALU.mult, eng="pool")
                            k.tt(xh[:, :], xh[:, :], acc[sub][:, hc:hc + HC], ALU.add)
                            k.dma("sp", V(xres.t[ti * 128:(ti + 1) * 128, hc:hc + HC], (xres_t[ti],)), xh[:, :])

        def stage_final():
            with k.scope() as ls:
                gb_ = k.sb([128, D], F32, "gfin", ls)
                grow = k.sb([1, D], F32, "grow", ls)
                xt = [k.sb([128, D], F32, "xt", ls) for _ in range(2)]
                junk = k.sb([128, D], BF16, "junk", ls)
                sm = k.sb([128, 4], F32, "sm", ls)
                k.dma("sp", grow[:, :], I["final_norm"][0:1, :])
                for ct in range(D // 512):
                    p = k.ps()
                    k.mm(p[:, :], ones[0:1, :], grow[0:1, ct * 512:(ct + 1) * 512])
                    k.copy(gb_[:, ct * 512:(ct + 1) * 512], p[:, :])
                for ti in range(N // 128):
                    x = xt[ti % 2]
                    k.dma("sp", x[:, :], xv(ti))
                    k.act(junk[:, :], x[:, :], AF.Square, accum=sm[:, 0:1])
                    k.rstd(sm[:, 1:2], sm[:, 0:1], D, sm[:, 2:3])
                    k.stt(x[:, :], x[:, :], sm[:, 1:2], gb_[:, :], ALU.mult, ALU.mult)
                    k.dma("sp", OUT[ti * 128:(ti + 1) * 128, :], x[:, :])

        def run_all():
            for fn in conv_list(0, True)[:len(WIN_T)]:
                fn()
            stage_ada()
            bgq.extend(conv_list(0, False))
            allb = list(range(cfg.NTB))
            for l in range(L):
                need_ctx = l < L - 1
                ob = [bi for bi in allb if (need_ctx or not cfg.blocks[bi][2])]
                stage_norm(l, 0, allb, False)
                stage_proj(l)
                stage_diff(l, need_ctx)
                stage_s5(l, need_ctx)
                stage_mla(l, need_ctx)
                stage_ret(l, need_ctx)
                bg_flush()
                stage_wout(l, ob)
                stage_norm(l, 1, ob, True)
                if l + 1 < L:
                    bgq.extend(conv_list(l + 1, True))
                stage_moe(l, ob)
                bg_flush()
            stage_final()

        def dbg_out(name, d, subs, dt):
            shape = list(d.t.shape)
            o = Buf(nc.dram_tensor("o_" + name, shape, dt, kind="ExternalOutput"), "o_" + name)
            idx = tuple(slice(None) for _ in shape)
            k.dma("sp", V(o.t[idx], (o,)), V(d.t[idx], tuple(subs)))
            return o

        outs = [OUT]
        run_all()
        k.finish(outs)
    return nc


def host_consts(cfg):
    ident = np.eye(128, dtype=np.float32)
    R = np.zeros((64, 64), np.float32)
    for i in range(16):
        R[i + 16, i] = -1.0
        R[i, i + 16] = 1.0
        R[i + 48, i + 32] = -1.0
        R[i + 32, i + 48] = 1.0
    rp = np.zeros((128, 128), np.float32)
    rp[:64, :64] = R
    rp[64:, 64:] = R
    rows = cfg.N // 64
    row = np.repeat(np.arange(rows, dtype=np.float32), 64)
    col = np.tile(np.arange(64, dtype=np.float32), rows)
    inv = (10000.0 ** (-np.arange(16, dtype=np.float32) / 16)).astype(np.float32)
    ar = row[:, None] * inv
    ac = col[:, None] * inv
    ang = np.concatenate([ar, ar, ac, ac], axis=-1)
    cs = np.stack([np.cos(ang).T, np.sin(ang).T]).astype(np.float32)
    cs = np.concatenate([cs, cs], axis=1)
    return {"ident": ident, "rperm": rp, "ropecs": np.ascontiguousarray(cs)}


def make_in_maps(cfg, inp, ncores):
    L = cfg.DEPTH
    f = lambda a: np.ascontiguousarray(np.asarray(a, dtype=np.float32))
    shared = {
        "c_ctx": f(inp["c_ctx"]).reshape(cfg.KC, 128),
        "ada_w": f(inp["ada_w"]), "ada_b": f(inp["ada_b"]),
        "norm_mix": f(inp["norm_mix"]), "norm_ffn": f(inp["norm_ffn"]),
        "w_in": f(inp["w_in"]), "w_out": f(inp["w_out"]),
        "diff_lambda": f(inp["diff_lambda"]).reshape(L, 256), "diff_subln": f(inp["diff_subln"]),
        "s5_a_re": f(inp["s5_a_re"]).reshape(L, 2, -1), "s5_a_im": f(inp["s5_a_im"]).reshape(L, 2, -1),
        "s5_log_dt": f(inp["s5_log_dt"]),
        "s5_b_re": f(inp["s5_b_re"]).reshape(L, 2, -1, 16), "s5_b_im": f(inp["s5_b_im"]).reshape(L, 2, -1, 16),
        "s5_c_re": f(inp["s5_c_re"]).reshape(L, 2, -1, 64), "s5_c_im": f(inp["s5_c_im"]).reshape(L, 2, -1, 64),
        "s5_d": f(inp["s5_d"]).reshape(L, -1), "s5_glu_w": f(inp["s5_glu_w"]), "s5_glu_b": f(inp["s5_glu_b"]),
        "mla_q_norm": f(inp["mla_q_norm"]), "mla_kv_norm": f(inp["mla_kv_norm"]),
        "mla_w_uq": f(inp["mla_w_uq"]), "mla_w_ukv": f(inp["mla_w_ukv"]),
        "ret_decay": f(inp["ret_decay"]).reshape(L, -1), "ret_norm": f(inp["ret_norm"]),
        "moe_wr": np.ascontiguousarray(np.concatenate([f(inp["moe_wg"]), f(inp["moe_we"])], axis=-1)),
        "moe_br": np.ascontiguousarray(np.concatenate([f(inp["moe_bg"]), f(inp["moe_be"])], axis=-1)),
        "moe_w_gate": f(inp["moe_w_gate"]), "moe_w_up": f(inp["moe_w_up"]), "moe_w_down": f(inp["moe_w_down"]),
        "final_norm": f(inp["final_norm"]).reshape(1, -1),
    }
    shared.update(host_consts(cfg))
    maps = []
    for b in range(ncores):
        m = dict(shared)
        m["x"] = f(inp["x"][b])
        m["ctx"] = f(inp["ctx"][b])
        m["c"] = f(inp["c"][b]).reshape(cfg.KC, 128)
        maps.append(m)
    return maps


def kernel(**inputs):
    x = np.asarray(inputs["x"])
    B, N, D = x.shape
    cfg = Cfg(D=D, N=N, LC=np.asarray(inputs["ctx"]).shape[1], DEPTH=np.asarray(inputs["ada_w"]).shape[0], B=B)
    nc = build_program(cfg)
    maps = make_in_maps(cfg, inputs, B)
    res = run_bass_kernel_spmd(nc, maps, core_ids=list(range(B)))
    return np.stack([np.asarray(r["out"]) for r in res.results]).astype(np.float32)
```

```python
import math
import os
from contextlib import ExitStack
import numpy as np
import concourse.bass as bass
import concourse.mybir as mybir
from concourse.bass_utils import run_bass_kernel_spmd

F32 = mybir.dt.float32
BF16 = mybir.dt.bfloat16
AF = mybir.ActivationFunctionType
ALU = mybir.AluOpType
AX = mybir.AxisListType
NORM_EPS = 1e-6


class Cfg:
    def __init__(s, D=4096, N=4096, LC=256, DEPTH=2, B=4):
        s.D, s.N, s.LC, s.DEPTH, s.B = D, N, LC, DEPTH, B
        s.T = N + LC
        s.GW = D // 4
        s.DH = 64
        s.DIFF_HEADS = s.GW // 128
        s.S5_CH, s.S5_P = 16, 64
        s.S5_G = s.GW // 16
        s.MLA_HEADS = s.GW // 128
        s.QR = 3 * D // 16
        s.KVR = D // 16
        s.RET_HEADS = s.GW // 128
        s.RQK = s.RET_HEADS * 64
        s.E, s.FF = 16, D // 4
        s.splits = [s.GW, s.GW, s.GW, s.GW, s.QR, s.KVR, 64, s.RQK, s.RQK, s.GW, s.GW]
        s.names = ["dq", "dk", "dv", "su", "cq", "ckv", "kr", "rq", "rk", "rv", "rg"]
        s.off = {}
        o = 0
        for n, w in zip(s.names, s.splits):
            s.off[n] = (o, w)
            o += w
        s.INW = o
        s.KC = D // 128
        s.TB = 512
        s.blocks = [(i * 512, 512, False) for i in range(N // 512)]
        c0 = N
        while c0 < s.T:
            w = min(512, s.T - c0)
            s.blocks.append((c0, w, True))
            c0 += w
        s.NTB = len(s.blocks)
        s.KT = s.T // 128


class Buf:
    __slots__ = ("t", "w", "r", "name")

    def __init__(s, t, name=""):
        s.t, s.w, s.r, s.name = t, None, {}, name

    def __getitem__(s, idx):
        return V(s.t[idx], (s,))

    def sub(s):
        return Buf(s.t, s.name)


class V:
    __slots__ = ("ap", "bufs")

    def __init__(s, ap, bufs):
        s.ap, s.bufs = ap, bufs

    def __getitem__(s, idx):
        return V(s.ap[idx], s.bufs)

    def rearrange(s, pat, **kw):
        return V(s.ap.rearrange(pat, **kw), s.bufs)


class K:
    ND = 24

    def __init__(s, nc, st):
        s.nc, s.st = nc, st
        s.E = {"pe": nc.tensor, "act": nc.scalar, "dve": nc.vector, "pool": nc.gpsimd, "sp": nc.sync}
        s.esem = {e: st.enter_context(nc.semaphore("es_" + e)) for e in ("pe", "act", "dve", "pool")}
        s.cnt = {e: 0 for e in s.esem}
        s.known = {e: {} for e in s.E}
        s.dsl = {q: [[st.enter_context(nc.semaphore("ds_%s%d" % (q, i))), 0] for i in range(s.ND)] for q in ("sp", "pool", "act")}
        s.dnext = {"sp": 0, "pool": 0, "act": 0}
        s.nbuf = 0
        s.psb = None
        s.psi = 0
        s.held = []

    def sb(s, shape, dt=F32, name=None, stack=None):
        s.nbuf += 1
        nm = "%s_%d" % (name or "b", s.nbuf)
        t = (stack or s.st).enter_context(s.nc.sbuf_tensor(nm, list(shape), dt))
        return Buf(t, nm)

    def dram(s, shape, dt=F32, name=None):
        s.nbuf += 1
        nm = "%s_%d" % (name or "d", s.nbuf)
        return Buf(s.nc.dram_tensor(nm, list(shape), dt), nm)

    def init_psum(s):
        s.psb = [Buf(s.st.enter_context(s.nc.psum_tensor("ps%d" % i, [128, 512], F32)), "ps%d" % i) for i in range(8)]

    def ps(s, hold=False):
        while True:
            b = s.psb[s.psi]
            s.psi = (s.psi + 1) % 8
            if b not in s.held:
                break
        if hold:
            s.held.append(b)
        return b

    def release(s, b):
        s.held.remove(b)

    def _toks(s, reads, writes):
        toks = []
        for v in reads:
            for b in v.bufs:
                if b.w is not None:
                    toks.append(b.w)
        for v in writes:
            for b in v.bufs:
                if b.w is not None:
                    toks.append(b.w)
                toks.extend(b.r.values())
        return toks

    def _wait(s, eng, toks):
        kn = s.known[eng]
        for tok in toks:
            key = tok[0]
            val = tok[1]
            if key == eng and eng == "pe":
                continue
            if kn.get(key, 0) >= val:
                continue
            sem = s.esem[key] if isinstance(key, str) else s.dsl[key[0]][key[1]][0]
            s.E[eng].wait_ge(sem, val)
            kn[key] = val

    def _mark(s, tok, rkey, reads, writes):
        for v in writes:
            for b in v.bufs:
                b.w = tok
                b.r = {}
        for v in reads:
            for b in v.bufs:
                if b.w is tok:
                    continue
                b.r[rkey] = tok

    def op(s, eng, fn, reads, writes):
        reads = [v for v in reads if isinstance(v, V)]
        s._wait(eng, s._toks(reads, writes))
        ins = fn(s.E[eng])
        s.cnt[eng] += 1
        ins.then_inc(s.esem[eng], 1)
        tok = (eng, s.cnt[eng])
        s._mark(tok, eng, reads, writes)

    def dma(s, q, out, in_):
        si = s.dnext[q]
        s.dnext[q] = (si + 1) % s.ND
        slot = s.dsl[q][si]
        key = (q, si)
        toks = s._toks([in_], [out])
        if slot[1] > 0:
            toks.append((key, slot[1]))
        s._wait(q, toks)
        ins = s.E[q].dma_start(out=out.ap, in_=in_.ap)
        slot[1] += 16
        ins.then_inc(slot[0], 16)
        tok = (key, slot[1])
        s._mark(tok, key, [in_], [out])

    def barrier(s):
        toks = [(e, c) for e, c in s.cnt.items() if c > 0]
        for q in s.dsl:
            for i, (sem, val) in enumerate(s.dsl[q]):
                if val > 0:
                    toks.append(((q, i), val))
        for eng in s.E:
            s._wait(eng, toks)

    def scope(s):
        k = s

        class _Scope(ExitStack):
            def __exit__(self, *a):
                if a[0] is None:
                    k.barrier()
                return super().__exit__(*a)

        return _Scope()

    def finish(s, outs):
        toks = []
        for b in outs:
            if b.w is not None:
                toks.append(b.w)
        s._wait("sp", toks)

    @staticmethod
    def _a(x):
        return x.ap if isinstance(x, V) else x

    def mm(s, out, lhsT, rhs, start=True, stop=True):
        s.op("pe", lambda e: e.matmul(out.ap, lhsT.ap, rhs.ap, start=start, stop=stop), [lhsT, rhs] + ([] if start else [out]), [out])

    def tr(s, out, in_, ident):
        s.op("pe", lambda e: e.transpose(out.ap, in_.ap, ident.ap), [in_, ident], [out])

    def act(s, out, in_, func, bias=0.0, scale=1.0, accum=None):
        kw = {}
        if accum is not None:
            kw["accum_out"] = accum.ap
        s.op("act", lambda e: e.activation(out=out.ap, in_=in_.ap, func=func, bias=s._a(bias), scale=s._a(scale), **kw),
             [in_, bias, scale], [out] + ([accum] if accum is not None else []))

    def tt(s, out, a, b, op, eng="dve"):
        s.op(eng, lambda e: e.tensor_tensor(out=out.ap, in0=a.ap, in1=b.ap, op=op), [a, b], [out])

    def ts(s, out, a, s1, op0, s2=None, op1=None, eng="dve", accum=None):
        kw = {}
        if accum is not None:
            kw["accum_out"] = accum.ap
        if op1 is None:
            s.op(eng, lambda e: e.tensor_scalar(out=out.ap, in0=a.ap, scalar1=s._a(s1), scalar2=None, op0=op0, **kw), [a, s1], [out])
        else:
            s.op(eng, lambda e: e.tensor_scalar(out=out.ap, in0=a.ap, scalar1=s._a(s1), scalar2=s._a(s2), op0=op0, op1=op1, **kw),
                 [a, s1, s2], [out] + ([accum] if accum is not None else []))

    def stt(s, out, a, sc, b, op0, op1):
        s.op("dve", lambda e: e.scalar_tensor_tensor(out=out.ap, in0=a.ap, scalar=s._a(sc), in1=b.ap, op0=op0, op1=op1), [a, sc, b], [out])

    def copy(s, out, in_, eng="dve"):
        if eng == "act":
            s.op("act", lambda e: e.copy(out=out.ap, in_=in_.ap), [in_], [out])
        else:
            s.op(eng, lambda e: e.tensor_copy(out=out.ap, in_=in_.ap), [in_], [out])

    def memset(s, out, val, eng="dve"):
        s.op(eng, lambda e: e.memset(out.ap, val), [], [out])

    def recip(s, out, in_):
        s.op("dve", lambda e: e.reciprocal(out=out.ap, in_=in_.ap), [in_], [out])

    def scan(s, out, d0, d1, init, op0=ALU.mult, op1=ALU.add):
        s.op("dve", lambda e: e.tensor_tensor_scan(out.ap, d0.ap, d1.ap, s._a(init), op0, op1), [d0, d1, init], [out])

    def rmax(s, out, in_):
        s.op("dve", lambda e: e.reduce_max(out=out.ap, in_=in_.ap, axis=AX.X), [in_], [out])

    def rsum(s, out, in_):
        s.op("dve", lambda e: e.reduce_sum(out=out.ap, in_=in_.ap, axis=AX.X), [in_], [out])

    def rstd(s, out, ss, n, tmp):
        s.ts(tmp, ss, 1.0 / n, ALU.mult, NORM_EPS, ALU.add)
        s.act(tmp, tmp, AF.Sqrt)
        s.recip(out, tmp)


def chunks(n, c=128):
    return [(i, min(c, n - i)) for i in range(0, n, c)]


def build_program(cfg, debug=()):
    nc = bass.Bass("TRN2", target_bir_lowering=False)
    D, N, LC, T, KC = cfg.D, cfg.N, cfg.LC, cfg.T, cfg.KC
    L = cfg.DEPTH
    dbg = {}

    def ext(name, shape, dt=F32):
        return Buf(nc.dram_tensor(name, list(shape), dt, kind="ExternalInput"), name)

    I = {}
    I["x"] = ext("x", [N, D])
    I["ctx"] = ext("ctx", [LC, D])
    I["c"] = ext("c", [KC, 128])
    I["c_ctx"] = ext("c_ctx", [KC, 128])
    I["ada_w"] = ext("ada_w", [L, D, 6 * D])
    I["ada_b"] = ext("ada_b", [L, 6 * D])
    I["norm_mix"] = ext("norm_mix", [L, D])
    I["norm_ffn"] = ext("norm_ffn", [L, D])
    I["w_in"] = ext("w_in", [L, D, cfg.INW])
    I["w_out"] = ext("w_out", [L, D, D])
    I["diff_lambda"] = ext("diff_lambda", [L, 256])
    I["diff_subln"] = ext("diff_subln", [L, 128])
    I["s5_a_re"] = ext("s5_a_re", [L, 2, cfg.S5_G * 64])
    I["s5_a_im"] = ext("s5_a_im", [L, 2, cfg.S5_G * 64])
    I["s5_log_dt"] = ext("s5_log_dt", [L, 2, cfg.S5_G])
    I["s5_b_re"] = ext("s5_b_re", [L, 2, cfg.S5_G * 64, 16])
    I["s5_b_im"] = ext("s5_b_im", [L, 2, cfg.S5_G * 64, 16])
    I["s5_c_re"] = ext("s5_c_re", [L, 2, cfg.S5_G * 16, 64])
    I["s5_c_im"] = ext("s5_c_im", [L, 2, cfg.S5_G * 16, 64])
    I["s5_d"] = ext("s5_d", [L, cfg.GW])
    I["s5_glu_w"] = ext("s5_glu_w", [L, cfg.GW, cfg.GW])
    I["s5_glu_b"] = ext("s5_glu_b", [L, cfg.GW])
    I["mla_q_norm"] = ext("mla_q_norm", [L, cfg.QR])
    I["mla_kv_norm"] = ext("mla_kv_norm", [L, cfg.KVR])
    I["mla_w_uq"] = ext("mla_w_uq", [L, cfg.QR, cfg.MLA_HEADS * 192])
    I["mla_w_ukv"] = ext("mla_w_ukv", [L, cfg.KVR, cfg.MLA_HEADS * 256])
    I["ret_decay"] = ext("ret_decay", [L, 2 * cfg.RET_HEADS])
    I["ret_norm"] = ext("ret_norm", [L, cfg.GW])
    I["moe_wr"] = ext("moe_wr", [L, D, 20])
    I["moe_br"] = ext("moe_br", [L, 20])
    I["moe_w_gate"] = ext("moe_w_gate", [L, 16, D, cfg.FF])
    I["moe_w_up"] = ext("moe_w_up", [L, 16, D, cfg.FF])
    I["moe_w_down"] = ext("moe_w_down", [L, 16, cfg.FF, D])
    I["final_norm"] = ext("final_norm", [1, D])
    I["ident"] = ext("ident", [128, 128])
    I["rperm"] = ext("rperm", [128, 128])
    I["ropecs"] = ext("ropecs", [2, 128, N])
    OUT = Buf(nc.dram_tensor("out", [N, D], F32, kind="ExternalOutput"), "out")

    with ExitStack() as st:
        k = K(nc, st)
        k.init_psum()
        ident = k.sb([128, 128], F32, "ident")
        identb = k.sb([128, 128], BF16, "identb")
        ones = k.sb([128, 128], F32, "ones")
        onesb = k.sb([128, 128], BF16, "onesb")
        rperm = k.sb([128, 128], BF16, "rperm")
        k.dma("sp", ident[:, :], I["ident"][:, :])
        k.dma("pool", identb[:, :], I["ident"][:, :])
        k.dma("pool", rperm[:, :], I["rperm"][:, :])
        k.memset(ones[:, :], 1.0)
        k.memset(onesb[:, :], 1.0)

        xres = k.dram([T, D], F32, "xres")
        xres_t = [xres.sub() for _ in range(T // 128)]
        modbc = [[[k.dram([128, D], F32, "modbc") for j in range(6)] for r in range(2)] for l in range(L)]
        hT = k.dram([cfg.NTB, 128, KC, 512], BF16, "hT")
        hT_b = [hT.sub() for _ in range(cfg.NTB)]
        dwd = k.dram([T, 16], F32, "dw")
        dwd_t = [dwd.sub() for _ in range(T // 128)]

        for i in range(N // 128):
            k.dma("sp", V(xres.t[i * 128:(i + 1) * 128, :], (xres_t[i],)), I["x"][i * 128:(i + 1) * 128, :])
        for i in range(LC // 128):
            j = N // 128 + i
            k.dma("sp", V(xres.t[j * 128:(j + 1) * 128, :], (xres_t[j],)), I["ctx"][i * 128:(i + 1) * 128, :])

        def stage_ada():
            with k.scope() as ls:
                rep = [k.sb([128, KC, 128], F32, "rep", ls) for r in range(2)]
                crow = k.sb([KC, 128], F32, "crow", ls)
                colv = k.sb([128, KC], F32, "colv", ls)
                for r, src in enumerate((I["c"], I["c_ctx"])):
                    k.dma("sp", crow[:, :], src[:, :])
                    k.act(crow[:, :], crow[:, :], AF.Silu)
                    p = k.ps()
                    k.tr(p[:, 0:KC], crow[:, :], ident[0:KC, 0:KC])
                    k.copy(colv[:, :], p[:, 0:KC])
                    for c in range(KC):
                        k.ts(rep[r][:, c, :], ones[:, :], colv[:, c:c + 1], ALU.mult, eng=("dve" if c % 2 else "pool"))
                KB = min(8, KC)
                wt = [k.sb([128, KB, 512], F32, "adaw", ls) for _ in range(3)]
                brow = [k.sb([1, 512], F32, "brow", ls) for _ in range(2)]
                grow = [k.sb([1, 512], F32, "grow", ls) for _ in range(2)]
                stg = [k.sb([128, 512], F32, "stg", ls) for _ in range(2)]
                gbc = k.sb([128, 512], F32, "gbc", ls)
                wi = 0
                si = 0
                for l in range(L):
                    wv = I["ada_w"].t[l].rearrange("(c p) n -> p c n", p=128)
                    for ct in range(6 * D // 512):
                        j = (ct * 512) // D
                        col = ct * 512 - j * D
                        pp = [k.ps(), k.ps()]
                        br_ = brow[ct % 2]
                        k.dma("sp", br_[:, :], I["ada_b"][l:l + 1, ct * 512:(ct + 1) * 512])
                        for kb in range(0, KC, KB):
                            w = wt[wi % 3]
                            wi += 1
                            k.dma("sp", w[:, :, :], V(wv[:, kb:kb + KB, ct * 512:(ct + 1) * 512], (I["ada_w"],)))
                            for c in range(KB):
                                for r in range(2):
                                    k.mm(pp[r][:, :], rep[r][:, kb + c, :], w[:, c, :], start=(kb + c == 0), stop=False)
                        for r in range(2):
                            k.mm(pp[r][:, :], ones[0:1, :], br_[0:1, :], start=False, stop=True)
                        if j in (1, 4):
                            pg = k.ps()
                            gr_ = grow[ct % 2]
                            k.dma("sp", gr_[:, :], (I["norm_mix"] if j == 1 else I["norm_ffn"])[l:l + 1, col:col + 512])
                            k.mm(pg[:, :], ones[0:1, :], gr_[0:1, :])
                            k.copy(gbc[:, :], pg[:, :], eng="act")
                        for r in range(2):
                            sg = stg[si % 2]
                            si += 1
                            if j in (1, 4):
                                k.stt(sg[:, :], pp[r][:, :], 1.0, gbc[:, :], ALU.add, ALU.mult)
                            else:
                                k.copy(sg[:, :], pp[r][:, :], eng="act")
                            k.dma("sp", modbc[l][r][j][:, col:col + 512], sg[:, :])

        def stage_norm(l, which, blocks, router):
            jS, jA = (0, 1) if which == 0 else (3, 4)
            with k.scope() as ls:
                A = k.sb([128, D], F32, "A", ls)
                S = k.sb([128, D], F32, "S", ls)
                xt = [k.sb([128, D], F32, "xt", ls) for _ in range(2)]
                junk = k.sb([128, D], BF16, "junk", ls)
                hb = [k.sb([128, KC, 512], BF16, "hb", ls) for _ in range(2)]
                sm = k.sb([128, 8], F32, "sm", ls)
                if router:
                    hf = k.sb([128, KC * 128], F32, "hf", ls)
                    wr = k.sb([128, KC, 20], F32, "wr", ls)
                    brr = k.sb([1, 20], F32, "brr", ls)
                    k.dma("sp", wr[:, :, :], V(I["moe_wr"].t[l].rearrange("(c p) n -> p c n", p=128), (I["moe_wr"],)))
                    k.dma("sp", brr[:, :], I["moe_br"][l:l + 1, :])
                    rt = k.sb([128, 64], F32, "rt", ls)
                    dwt = k.sb([128, 16], F32, "dwt", ls)
                cur_r = None
                xi = 0
                for bi in blocks:
                    t0, bw, isc = cfg.blocks[bi]
                    r = 1 if isc else 0
                    if r != cur_r:
                        k.dma("sp", A[:, :], modbc[l][r][jA][:, :])
                        k.dma("sp", S[:, :], modbc[l][r][jS][:, :])
                        cur_r = r
                    hbb = hb[bi % 2]
                    for sub in range(bw // 128):
                        ti = (t0 + sub * 128) // 128
                        x = xt[xi % 2]
                        xi += 1
                        k.dma("sp", x[:, :], V(xres.t[ti * 128:(ti + 1) * 128, :], (xres_t[ti],)))
                        k.act(junk[:, :], x[:, :], AF.Square, accum=sm[:, 0:1])
                        k.rstd(sm[:, 1:2], sm[:, 0:1], D, sm[:, 2:3])
                        k.stt(x[:, :], x[:, :], sm[:, 1:2], A[:, :], ALU.mult, ALU.mult)
                        k.tt(x[:, :], x[:, :], S[:, :], ALU.add, eng="pool")
                        for c4 in range(0, KC, 4):
                            p = k.ps()
                            for c in range(c4, min(c4 + 4, KC)):
                                k.tr(p[:, (c - c4) * 128:(c - c4 + 1) * 128], x[:, c * 128:(c + 1) * 128], ident[:, :])
                            nn = min(4, KC - c4)
                            pv = p[:, 0:nn * 128].rearrange("p (c t) -> p c t", c=nn)
                            k.copy(hbb[:, c4:c4 + nn, sub * 128:(sub + 1) * 128], pv, eng=("dve" if router or (c4 // 4) % 2 else "act"))
                            if router:
                                k.ts(hf[:, c4 * 128:(c4 + nn) * 128], p[:, 0:nn * 128], 1.0, ALU.mult)
                        if router:
                            pr = k.ps()
                            for c in range(KC):
                                k.mm(pr[:, 0:20], hf[:, c * 128:(c + 1) * 128], wr[:, c, :], start=(c == 0), stop=False)
                            k.mm(pr[:, 0:20], ones[0:1, :], brr[0:1, :], start=False, stop=True)
                            lg = rt[:, 0:20]
                            k.copy(lg, pr[:, 0:20])
                            gmx = rt[:, 20:21]
                            k.rmax(gmx, rt[:, 0:4])
                            k.ts(rt[:, 24:28], rt[:, 0:4], gmx, ALU.subtract)
                            k.act(rt[:, 28:32], rt[:, 24:28], AF.Exp, accum=rt[:, 21:22])
                            k.recip(rt[:, 22:23], rt[:, 21:22])
                            k.ts(rt[:, 24:28], rt[:, 24:28], 0.0, ALU.is_ge)
                            k.ts(rt[:, 28:32], rt[:, 24:28], 1.0, ALU.subtract, 1e30, ALU.mult)
                            for g in range(4):
                                k.ts(rt[:, 32 + 4 * g:36 + 4 * g], rt[:, 4 + 4 * g:8 + 4 * g], rt[:, 28 + g:29 + g], ALU.add)
                            em = rt[:, 32:48]
                            k.rmax(rt[:, 48:49], em)
                            k.ts(dwt[:, :], em, rt[:, 48:49], ALU.is_ge)
                            k.stt(rt[:, 4:20], dwt[:, :], -1e30, em, ALU.mult, ALU.add)
                            k.rmax(rt[:, 49:50], rt[:, 4:20])
                            k.ts(rt[:, 32:48], rt[:, 4:20], rt[:, 49:50], ALU.is_ge)
                            k.tt(rt[:, 50:51], rt[:, 49:50], rt[:, 48:49], ALU.subtract)
                            k.act(rt[:, 51:52], rt[:, 50:51], AF.Exp)
                            k.ts(rt[:, 52:53], rt[:, 51:52], 1.0, ALU.add)
                            k.recip(rt[:, 52:53], rt[:, 52:53])
                            k.tt(rt[:, 53:54], rt[:, 52:53], rt[:, 22:23], ALU.mult)
                            k.tt(rt[:, 54:55], rt[:, 22:23], rt[:, 53:54], ALU.subtract)
                            k.ts(dwt[:, :], dwt[:, :], rt[:, 53:54], ALU.mult)
                            k.stt(dwt[:, :], rt[:, 32:48], rt[:, 54:55], dwt[:, :], ALU.mult, ALU.add)
                            k.dma("sp", V(dwd.t[ti * 128:(ti + 1) * 128, :], (dwd_t[ti],)), dwt[:, :])
                    k.dma("sp", V(hT.t[bi][:, :, 0:bw], (hT_b[bi],)), hbb[:, :, 0:bw])

        def wtiles_in():
            tl = []
            for nm in cfg.names:
                g0, gw = cfg.off[nm]
                for c0 in range(0, gw, 256):
                    tl.append((nm, c0, g0 + c0, min(256, gw - c0)))
            return tl

        WIN_T = wtiles_in()
        WIN_IDX = {(nm, c0): i for i, (nm, c0, a, w) in enumerate(WIN_T)}
        FC_ = cfg.FF // 128
        NFT = (cfg.FF + 255) // 256
        winb, woutb, wgb, wub, wdb = [], [], [], [], []
        for l in range(L):
            d = k.dram([len(WIN_T), 128, KC, 256], BF16, "winb")
            winb.append((d, [d.sub() for _ in WIN_T]))
            d = k.dram([D // 256, 128, KC, 256], BF16, "woutb")
            woutb.append((d, [d.sub() for _ in range(D // 256)]))
            d = k.dram([16, NFT, 128, KC, 256], BF16, "wgb")
            wgb.append((d, [[d.sub() for _ in range(NFT)] for _ in range(16)]))
            d = k.dram([16, NFT, 128, KC, 256], BF16, "wub")
            wub.append((d, [[d.sub() for _ in range(NFT)] for _ in range(16)]))
            d = k.dram([16, D // 512, 128, FC_, 512], BF16, "wdb")
            wdb.append((d, [[d.sub() for _ in range(D // 512)] for _ in range(16)]))

        bgq = []

        def bg_step(n):
            for _ in range(min(n, len(bgq))):
                bgq.pop(0)()

        def bg_flush():
            bg_step(len(bgq))

        def conv_list(l, with_win):
            out = []
            if with_win:
                wv0 = I["w_in"].t[l].rearrange("(c p) n -> p c n", p=128)
                for i, (nm, c0, a0, w) in enumerate(WIN_T):
                    out.append(lambda i=i, a0=a0, w=w, wv0=wv0: k.dma("pool", V(winb[l][0].t[i][:, :, 0:w], (winb[l][1][i],)), V(wv0[:, :, a0:a0 + w], (I["w_in"],))))
            wv1 = I["w_out"].t[l].rearrange("(c p) n -> p c n", p=128)
            for ct in range(D // 256):
                out.append(lambda ct=ct, wv1=wv1: k.dma("pool", V(woutb[l][0].t[ct], (woutb[l][1][ct],)), V(wv1[:, :, ct * 256:(ct + 1) * 256], (I["w_out"],))))
            for e in range(16):
                wgv = I["moe_w_gate"].t[l, e].rearrange("(c p) f -> p c f", p=128)
                wuv = I["moe_w_up"].t[l, e].rearrange("(c p) f -> p c f", p=128)
                wdv = I["moe_w_down"].t[l, e].rearrange("(c p) n -> p c n", p=128)
                for j in range(NFT):
                    f0 = j * 256
                    fw = min(256, cfg.FF - f0)
                    out.append(lambda e=e, j=j, f0=f0, fw=fw, wgv=wgv: k.dma("pool", V(wgb[l][0].t[e, j][:, :, 0:fw], (wgb[l][1][e][j],)), V(wgv[:, :, f0:f0 + fw], (I["moe_w_gate"],))))
                    out.append(lambda e=e, j=j, f0=f0, fw=fw, wuv=wuv: k.dma("pool", V(wub[l][0].t[e, j][:, :, 0:fw], (wub[l][1][e][j],)), V(wuv[:, :, f0:f0 + fw], (I["moe_w_up"],))))
                for ct in range(D // 512):
                    out.append(lambda e=e, ct=ct, wdv=wdv: k.dma("pool", V(wdb[l][0].t[e, ct], (wdb[l][1][e][ct],)), V(wdv[:, :, ct * 512:(ct + 1) * 512], (I["moe_w_down"],))))
            return out

        def fm_alloc(nrows, name, dt=BF16):
            nch = (nrows + 127) // 128
            d = k.dram([nch, cfg.NTB, 128, 512], dt, name)
            return d, [[d.sub() for _ in range(cfg.NTB)] for _ in range(nch)]

        FM = {}
        for nm in ("dq", "dk", "su", "cq", "ckv", "kr", "rq", "rk", "rg"):
            FM[nm] = fm_alloc(cfg.off[nm][1], "fm_" + nm)

        def fmv(nm, ch, tb, rows=128, w=512):
            d, subs = FM[nm]
            return V(d.t[ch, tb][0:rows, 0:w], (subs[ch][tb],))

        TMV = {}
        for nm, nh in (("dv", cfg.DIFF_HEADS), ("rv", cfg.RET_HEADS)):
            d = k.dram([nh, 128, cfg.KT, 128], BF16, "tm_" + nm)
            TMV[nm] = (d, [[d.sub() for _ in range(cfg.KT)] for _ in range(nh)])
        rq_bc = k.dram([cfg.NTB, 128, 512], F32, "rq_bc")
        rq_bc_s = [rq_bc.sub() for _ in range(cfg.NTB)]
        rkv_bc = k.dram([cfg.NTB, 128, 512], F32, "rkv_bc")
        rkv_bc_s = [rkv_bc.sub() for _ in range(cfg.NTB)]
        rkv_tm = k.dram([128, cfg.KT], F32, "rkv_tm")
        rkv_tm_s = [rkv_tm.sub() for _ in range(cfg.NTB)]

        def row_to_cols(dst, src_row, n, tmp_row):
            k.dma("sp", tmp_row[0:1, 0:n], src_row)
            p = k.ps()
            for i, (c0, cw) in enumerate(chunks(n)):
                k.tr(p[0:cw, i:i + 1], tmp_row[0:1, c0:c0 + cw], ident[0:1, 0:1])
                k.copy(dst[0:cw, i:i + 1], p[0:cw, i:i + 1])

        def stage_proj(l):
            wv = I["w_in"].t[l].rearrange("(c p) n -> p c n", p=128)
            csv = I["ropecs"].t.rearrange("a p n -> p a n")
            with k.scope() as ls:
                hb = [k.sb([128, KC, 512], BF16, "hb", ls) for _ in range(2)]
                wt = [k.sb([128, KC, 256], BF16, "wt", ls) for _ in range(3)]
                cs = [k.sb([128, 2, 512], F32, "cs", ls) for _ in range(2)]
                stg = [k.sb([128, 512], BF16, "stg", ls) for _ in range(3)]
                xs = [k.sb([128, 512], BF16, "xs", ls) for _ in range(2)]
                t1 = [k.sb([128, 512], F32, "t1", ls) for _ in range(2)]
                t2 = [k.sb([128, 512], F32, "t2", ls) for _ in range(2)]
                sqf = [k.sb([128, 512], F32, "sqf", ls) for _ in range(2)]
                rbc = k.sb([128, 512], F32, "rbc", ls)
                rtmp = k.sb([128, 512], F32, "rtmp", ls)
                qn = k.sb([128, 16], F32, "qn", ls)
                rtm = k.sb([128, 4], F32, "rtm", ls)
                trow = k.sb([1, max(cfg.QR, 128)], F32, "trow", ls)
                row_to_cols(qn[:, 0:8], I["mla_q_norm"][l:l + 1, :], cfg.QR, trow)
                row_to_cols(qn[:, 8:16], I["mla_kv_norm"][l:l + 1, :], cfg.KVR, trow)
                cnt = {"w": 0, "s": 0, "x": 0}

                def load_w(c0, w, nm_=None, c0rel=None):
                    t = wt[cnt["w"] % 3]
                    cnt["w"] += 1
                    i = WIN_IDX[(nm_, c0rel)]
                    k.dma("sp" if cnt["w"] % 2 else "pool", t[:, :, 0:w], V(winb[l][0].t[i][:, :, 0:w], (winb[l][1][i],)))
                    return t

                for bi, (t0, bw, isc) in enumerate(cfg.blocks):
                    h = hb[bi % 2]
                    k.dma("sp", h[:, :, 0:bw], V(hT.t[bi][:, :, 0:bw], (hT_b[bi],)))
                    c_ = cs[bi % 2]
                    if not isc:
                        k.dma("sp", c_[:, :, :], V(csv[:, :, t0:t0 + 512], (I["ropecs"],)))
                    for nm in cfg.names:
                        g0, gw = cfg.off[nm]
                        if nm in ("dv", "rv"):
                            d, subs = TMV[nm]
                            for c0 in range(0, gw, 256):
                                w = min(256, gw - c0)
                                t = load_w(g0 + c0, w, nm, c0)
                                for sub in range(bw // 128):
                                    p = k.ps()
                                    for c in range(KC):
                                        k.mm(p[:, 0:w], h[:, c, sub * 128:(sub + 1) * 128], t[:, c, 0:w], start=(c == 0), stop=(c == KC - 1))
                                    sg = stg[cnt["s"] % 3]
                                    cnt["s"] += 1
                                    k.copy(sg[:, 0:w], p[:, 0:w], eng=("act" if cnt["s"] % 2 else "dve"))
                                    kt = (t0 + sub * 128) // 128
                                    for hh in range(w // 128):
                                        head = (c0 + hh * 128) // 128
                                        k.dma("sp", V(d.t[head][:, kt, :], (subs[head][kt],)), sg[:, hh * 128:(hh + 1) * 128])
                            continue
                        nch = (gw + 127) // 128
                        pss = k.ps(hold=True) if nm in ("cq", "ckv") else None
                        for c0 in range(0, gw, 256):
                            w = min(256, gw - c0)
                            t = load_w(g0 + c0, w, nm, c0)
                            for j0 in range(0, w, 128):
                                cw = min(128, w - j0)
                                ch = (c0 + j0) // 128
                                p = k.ps()
                                for c in range(KC):
                                    k.mm(p[0:cw, 0:bw], t[:, c, j0:j0 + cw], h[:, c, 0:bw], start=(c == 0), stop=(c == KC - 1))
                                sg = stg[cnt["s"] % 3]
                                cnt["s"] += 1
                                if nm in ("dq", "dk", "kr", "rq", "rk") and not isc:
                                    x_ = xs[cnt["x"] % 2]
                                    a1 = t1[cnt["x"] % 2]
                                    a2 = t2[cnt["x"] % 2]
                                    cnt["x"] += 1
                                    k.copy(x_[0:cw, 0:bw], p[0:cw, 0:bw], eng="act")
                                    p2 = k.ps()
                                    k.mm(p2[0:cw, 0:bw], rperm[0:cw, 0:cw], x_[0:cw, 0:bw])
                                    k.tt(a1[0:cw, 0:bw], x_[0:cw, 0:bw], c_[0:cw, 0, 0:bw], ALU.mult, eng="pool")
                                    k.tt(a2[0:cw, 0:bw], p2[0:cw, 0:bw], c_[0:cw, 1, 0:bw], ALU.mult)
                                    k.tt(sg[0:cw, 0:bw], a1[0:cw, 0:bw], a2[0:cw, 0:bw], ALU.add, eng="pool")
                                elif nm in ("cq", "ckv"):
                                    qi = (0 if nm == "cq" else 8) + ch
                                    k.act(sg[0:cw, 0:bw], p[0:cw, 0:bw], AF.Copy, scale=qn[0:cw, qi:qi + 1])
                                    sq = sqf[ch % 2]
                                    k.act(sq[0:cw, 0:bw], p[0:cw, 0:bw], AF.Square)
                                    k.mm(pss[:, 0:bw], ones[0:cw, :], sq[0:cw, 0:bw], start=(ch == 0), stop=(ch == nch - 1))
                                elif nm == "rg":
                                    k.act(sg[0:cw, 0:bw], p[0:cw, 0:bw], AF.Silu)
                                else:
                                    k.copy(sg[0:cw, 0:bw], p[0:cw, 0:bw], eng=("act" if cnt["s"] % 2 else "dve"))
                                k.dma("sp", fmv(nm, ch, bi, cw, bw), sg[0:cw, 0:bw])
                        if pss is not None:
                            k.rstd(rbc[:, 0:bw], pss[:, 0:bw], gw, rtmp[:, 0:bw])
                            k.release(pss)
                            if nm == "cq":
                                k.dma("sp", V(rq_bc.t[bi][:, 0:bw], (rq_bc_s[bi],)), rbc[:, 0:bw])
                            else:
                                k.dma("sp", V(rkv_bc.t[bi][:, 0:bw], (rkv_bc_s[bi],)), rbc[:, 0:bw])
                                pt = k.ps()
                                ns = bw // 128
                                for sub in range(ns):
                                    k.tr(pt[:, sub:sub + 1], rbc[0:1, sub * 128:(sub + 1) * 128], ident[0:1, 0:1])
                                k.copy(rtm[:, 0:ns], pt[:, 0:ns])
                                kt0 = t0 // 128
                                k.dma("sp", V(rkv_tm.t[:, kt0:kt0 + ns], (rkv_tm_s[bi],)), rtm[:, 0:ns])

        GWc = cfg.GW // 128
        FM["mix"] = fm_alloc(D, "fm_mix")

        def bcast_col(dst, src11):
            p = k.ps()
            k.mm(p[:, 0:1], ones[0:1, :], src11)
            k.copy(dst, p[:, 0:1])

        def attn_core(ls_bufs, q_parts, k_parts, vT, ktiles, scale, bw):
            pT = ls_bufs
            po = k.ps(hold=True)
            pd = k.ps(hold=True)
            n = len(ktiles)
            for i, kt in enumerate(ktiles):
                p = k.ps()
                for j, (qv, kf) in enumerate(zip(q_parts, k_parts)):
                    k.mm(p[:, 0:bw], kf(kt), qv, start=(j == 0), stop=(j == len(q_parts) - 1))
                pt = pT[i % len(pT)]
                k.act(pt[:, 0:bw], p[:, 0:bw], AF.Exp, scale=scale)
                k.mm(po[:, 0:bw], vT[:, kt, :], pt[:, 0:bw], start=(i == 0), stop=(i == n - 1))
                k.mm(pd[:, 0:bw], onesb[:, :], pt[:, 0:bw], start=(i == 0), stop=(i == n - 1))
            return po, pd

        def q_blocks(need_ctx):
            return [(bi, t0, bw, isc) for bi, (t0, bw, isc) in enumerate(cfg.blocks) if (need_ctx or not isc)]

        def key_tiles(isc):
            return list(range(N // 128, cfg.KT)) if isc else list(range(cfg.KT))

        def stage_diff(l, need_ctx):
            lam_init = 0.8 - 0.6 * math.exp(-0.3 * l)
            with k.scope() as ls:
                kT = k.sb([128, T], BF16, "kT", ls)
                vT = k.sb([128, cfg.KT, 128], BF16, "vT", ls)
                qT = [k.sb([128, 512], BF16, "qT", ls) for _ in range(2)]
                pT = [k.sb([128, 512], BF16, "pT", ls) for _ in range(3)]
                a0 = k.sb([128, 512], F32, "a0", ls)
                a1 = k.sb([128, 512], F32, "a1", ls)
                rr = k.sb([128, 512], F32, "rr", ls)
                og = k.sb([128, 512], BF16, "og", ls)
                lr = k.sb([1, 256], F32, "lr", ls)
                lt = k.sb([1, 128], F32, "lt", ls)
                sc_ = k.sb([128, 8], F32, "sc", ls)
                trow = k.sb([1, 128], F32, "trow", ls)
                k.dma("sp", lr[:, :], I["diff_lambda"][l:l + 1, :])
                k.tt(lt[0:1, 0:64], lr[0:1, 0:64], lr[0:1, 64:128], ALU.mult)
                k.tt(lt[0:1, 64:128], lr[0:1, 128:192], lr[0:1, 192:256], ALU.mult)
                k.rsum(lr[0:1, 0:1], lt[0:1, 0:64])
                k.rsum(lr[0:1, 1:2], lt[0:1, 64:128])
                k.act(lr[0:1, 2:4], lr[0:1, 0:2], AF.Exp)
                k.tt(lr[0:1, 4:5], lr[0:1, 3:4], lr[0:1, 2:3], ALU.subtract)
                k.ts(lr[0:1, 5:6], lr[0:1, 4:5], -lam_init, ALU.add)
                bcast_col(sc_[:, 0:1], lr[0:1, 5:6])
                row_to_cols(sc_[:, 1:2], I["diff_subln"][l:l + 1, :], 128, trow)
                k.ts(sc_[:, 2:3], sc_[:, 1:2], 1.0 - lam_init, ALU.mult)
                qi = 0
                for h in range(cfg.DIFF_HEADS):
                    for bi, (t0, bw, isc) in enumerate(cfg.blocks):
                        k.dma("sp", kT[:, t0:t0 + bw], fmv("dk", h, bi, 128, bw))
                    dv, dvs = TMV["dv"]
                    k.dma("sp", vT[:, :, :], V(dv.t[h], tuple(dvs[h])))
                    for bi, t0, bw, isc in q_blocks(need_ctx):
                        q = qT[qi % 2]
                        qi += 1
                        k.dma("sp", q[:, 0:bw], fmv("dq", h, bi, 128, bw))
                        kts = key_tiles(isc)
                        for j in range(2):
                            r0, r1 = j * 64, (j + 1) * 64
                            po, pd = attn_core(pT, [q[r0:r1, 0:bw]], [lambda kt, r0=r0, r1=r1: kT[r0:r1, kt * 128:(kt + 1) * 128]],
                                               vT, kts, 64 ** -0.5, bw)
                            dst = a0 if j == 0 else a1
                            k.recip(rr[:, 0:bw], pd[:, 0:bw])
                            k.tt(dst[:, 0:bw], po[:, 0:bw], rr[:, 0:bw], ALU.mult)
                            k.release(po)
                            k.release(pd)
                        k.stt(a0[:, 0:bw], a1[:, 0:bw], sc_[:, 0:1], a0[:, 0:bw], ALU.mult, ALU.add)
                        k.act(a1[:, 0:bw], a0[:, 0:bw], AF.Square)
                        pss = k.ps()
                        k.mm(pss[:, 0:bw], ones[:, :], a1[:, 0:bw])
                        k.rstd(rr[:, 0:bw], pss[:, 0:bw], 128, a1[:, 0:bw])
                        k.stt(og[:, 0:bw], a0[:, 0:bw], sc_[:, 2:3], rr[:, 0:bw], ALU.mult, ALU.mult)
                        k.dma("sp", fmv("mix", h, bi, 128, bw), og[:, 0:bw])
                        bg_step(2)

        MH = cfg.MLA_HEADS
        mqn = fm_alloc(MH * 128, "mqn")
        mqr = fm_alloc(MH * 128, "mqr")
        mkn = fm_alloc(MH * 128, "mkn")
        mvd = k.dram([MH, 128, cfg.KT, 128], BF16, "mv")
        mvs = [[mvd.sub() for _ in range(cfg.KT)] for _ in range(MH)]

        def stage_mla(l, need_ctx):
            qch = chunks(cfg.QR)
            kch = chunks(cfg.KVR)
            csv = I["ropecs"].t.rearrange("a p n -> p a n")
            with k.scope() as ls:
                wq = k.sb([128, len(qch), MH * 192], BF16, "wq", ls)
                wkv = k.sb([128, len(kch), MH * 256], BF16, "wkv", ls)
                for ci, (c0, cw) in enumerate(qch):
                    k.dma("pool", wq[0:cw, ci, :], I["mla_w_uq"][l, c0:c0 + cw, :])
                for ci, (c0, cw) in enumerate(kch):
                    k.dma("pool", wkv[0:cw, ci, :], I["mla_w_ukv"][l, c0:c0 + cw, :])
                rtm_sb = k.sb([128, cfg.KT], F32, "rtm_sb", ls)
                k.dma("sp", rtm_sb[:, :], V(rkv_tm.t[:, :], tuple(rkv_tm_s)))
                cqs = k.sb([128, len(qch), 512], BF16, "cqs", ls)
                cks = k.sb([128, len(kch), 512], BF16, "cks", ls)
                rqt = k.sb([128, 512], F32, "rqt", ls)
                rkt = k.sb([128, 512], F32, "rkt", ls)
                cs_ = k.sb([128, 2, 512], F32, "cs", ls)
                stg = [k.sb([128, 512], BF16, "stg", ls) for _ in range(3)]
                xs = k.sb([128, 512], BF16, "xs", ls)
                b1 = k.sb([128, 512], F32, "b1", ls)
                b2 = k.sb([128, 512], F32, "b2", ls)
                si = 0
                for bi, (t0, bw, isc) in enumerate(cfg.blocks):
                    for ci, (c0, cw) in enumerate(qch):
                        k.dma("sp", cqs[0:cw, ci, 0:bw], fmv("cq", ci, bi, cw, bw))
                    for ci, (c0, cw) in enumerate(kch):
                        k.dma("sp", cks[0:cw, ci, 0:bw], fmv("ckv", ci, bi, cw, bw))
                    k.dma("sp", rqt[:, 0:bw], V(rq_bc.t[bi][:, 0:bw], (rq_bc_s[bi],)))
                    k.dma("sp", rkt[:, 0:bw], V(rkv_bc.t[bi][:, 0:bw], (rkv_bc_s[bi],)))
                    if not isc:
                        k.dma("sp", cs_[:, :, :], V(csv[:, :, t0:t0 + 512], (I["ropecs"],)))
                    for h in range(MH):
                        if need_ctx or not isc:
                            p = k.ps()
                            for ci, (c0, cw) in enumerate(qch):
                                k.mm(p[:, 0:bw], wq[0:cw, ci, h * 192:h * 192 + 128], cqs[0:cw, ci, 0:bw], start=(ci == 0), stop=(ci == len(qch) - 1))
                            sg = stg[si % 3]
                            si += 1
                            k.tt(sg[:, 0:bw], p[:, 0:bw], rqt[:, 0:bw], ALU.mult)
                            k.dma("sp", V(mqn[0].t[h, bi][:, 0:bw], (mqn[1][h][bi],)), sg[:, 0:bw])
                            p = k.ps()
                            for ci, (c0, cw) in enumerate(qch):
                                k.mm(p[0:64, 0:bw], wq[0:cw, ci, h * 192 + 128:h * 192 + 192], cqs[0:cw, ci, 0:bw], start=(ci == 0), stop=(ci == len(qch) - 1))
                            sg = stg[si % 3]
                            si += 1
                            if isc:
                                k.tt(sg[0:64, 0:bw], p[0:64, 0:bw], rqt[0:64, 0:bw], ALU.mult)
                            else:
                                k.tt(xs[0:64, 0:bw], p[0:64, 0:bw], rqt[0:64, 0:bw], ALU.mult)
                                p2 = k.ps()
                                k.mm(p2[0:64, 0:bw], rperm[0:64, 0:64], xs[0:64, 0:bw])
                                k.tt(b1[0:64, 0:bw], xs[0:64, 0:bw], cs_[0:64, 0, 0:bw], ALU.mult, eng="pool")
                                k.tt(b2[0:64, 0:bw], p2[0:64, 0:bw], cs_[0:64, 1, 0:bw], ALU.mult)
                                k.tt(sg[0:64, 0:bw], b1[0:64, 0:bw], b2[0:64, 0:bw], ALU.add, eng="pool")
                            k.dma("sp", V(mqr[0].t[h, bi][0:64, 0:bw], (mqr[1][h][bi],)), sg[0:64, 0:bw])
                        p = k.ps()
                        for ci, (c0, cw) in enumerate(kch):
                            k.mm(p[:, 0:bw], wkv[0:cw, ci, h * 256:h * 256 + 128], cks[0:cw, ci, 0:bw], start=(ci == 0), stop=(ci == len(kch) - 1))
                        sg = stg[si % 3]
                        si += 1
                        k.tt(sg[:, 0:bw], p[:, 0:bw], rkt[:, 0:bw], ALU.mult)
                        k.dma("sp", V(mkn[0].t[h, bi][:, 0:bw], (mkn[1][h][bi],)), sg[:, 0:bw])
                        for sub in range(bw // 128):
                            kt = t0 // 128 + sub
                            p = k.ps()
                            for ci, (c0, cw) in enumerate(kch):
                                k.mm(p[:, 0:128], cks[0:cw, ci, sub * 128:(sub + 1) * 128], wkv[0:cw, ci, h * 256 + 128:h * 256 + 256], start=(ci == 0), stop=(ci == len(kch) - 1))
                            sg = stg[si % 3]
                            si += 1
                            k.ts(sg[:, 0:128], p[:, 0:128], rtm_sb[:, kt:kt + 1], ALU.mult)
                            k.dma("sp", V(mvd.t[h][:, kt, :], (mvs[h][kt],)), sg[:, 0:128])
            with k.scope() as ls:
                kTn = k.sb([128, T], BF16, "kTn", ls)
                kTr = k.sb([64, T], BF16, "kTr", ls)
                vT = k.sb([128, cfg.KT, 128], BF16, "vT", ls)
                qn = [k.sb([128, 512], BF16, "qn", ls) for _ in range(2)]
                qr = [k.sb([64, 512], BF16, "qr", ls) for _ in range(2)]
                pT = [k.sb([128, 512], BF16, "pT", ls) for _ in range(3)]
                rr = k.sb([128, 512], F32, "rr", ls)
                og = k.sb([128, 512], BF16, "og", ls)
                for bi, (t0, bw, isc) in enumerate(cfg.blocks):
                    k.dma("sp", kTr[:, t0:t0 + bw], fmv("kr", 0, bi, 64, bw))
                qi = 0
                for h in range(MH):
                    for bi, (t0, bw, isc) in enumerate(cfg.blocks):
                        k.dma("sp", kTn[:, t0:t0 + bw], V(mkn[0].t[h, bi][:, 0:bw], (mkn[1][h][bi],)))
                    k.dma("sp", vT[:, :, :], V(mvd.t[h], tuple(mvs[h])))
                    for bi, t0, bw, isc in q_blocks(need_ctx):
                        qa, qb_ = qn[qi % 2], qr[qi % 2]
                        qi += 1
                        k.dma("sp", qa[:, 0:bw], V(mqn[0].t[h, bi][:, 0:bw], (mqn[1][h][bi],)))
                        k.dma("sp", qb_[:, 0:bw], V(mqr[0].t[h, bi][0:64, 0:bw], (mqr[1][h][bi],)))
                        po, pd = attn_core(pT, [qa[:, 0:bw], qb_[0:64, 0:bw]],
                                           [lambda kt: kTn[:, kt * 128:(kt + 1) * 128], lambda kt: kTr[0:64, kt * 128:(kt + 1) * 128]],
                                           vT, key_tiles(isc), 192 ** -0.5, bw)
                        k.recip(rr[:, 0:bw], pd[:, 0:bw])
                        k.tt(og[:, 0:bw], po[:, 0:bw], rr[:, 0:bw], ALU.mult)
                        k.release(po)
                        k.release(pd)
                        k.dma("sp", fmv("mix", 2 * GWc + h, bi, 128, bw), og[:, 0:bw])
                        bg_step(2)

        def stage_ret(l, need_ctx):
            RH = cfg.RET_HEADS
            ksc = 64 ** -0.5
            NI = cfg.KT + 8
            with k.scope() as ls:
                dr_ = k.sb([1, 2 * RH], F32, "dr", ls)
                lgb_ = k.sb([128, 2 * RH], F32, "lg", ls)
                nlg = k.sb([128, 2 * RH], F32, "nlg", ls)
                d0i = k.sb([128, 512], mybir.dt.int32, "d0i", ls)
                D0 = k.sb([128, 512], F32, "D0", ls)
                ioi = k.sb([128, NI], mybir.dt.int32, "ioi", ls)
                iof = k.sb([128, NI], F32, "iof", ls)
                ctf = k.sb([128, NI], F32, "ctf", ls)
                ctb = k.sb([128, NI], F32, "ctb", ls)
                Ef = k.sb([128, 512], F32, "Ef", ls)
                Eb = k.sb([128, 512], F32, "Eb", ls)
                Wd = [k.sb([128, 512], F32, "Wd", ls) for _ in range(4)]
                w1 = k.sb([128, 512], F32, "w1", ls)
                w2 = k.sb([128, 512], F32, "w2", ls)
                w3 = k.sb([128, 512], F32, "w3", ls)
                kT = k.sb([128, T], BF16, "kT", ls)
                vT = k.sb([128, cfg.KT, 128], BF16, "vT", ls)
                qT = [k.sb([128, 512], BF16, "qT", ls) for _ in range(2)]
                gT = [k.sb([128, 512], BF16, "gT", ls) for _ in range(2)]
                pT = [k.sb([128, 512], BF16, "pT", ls) for _ in range(3)]
                ya = k.sb([128, 512], F32, "ya", ls)
                yb = k.sb([128, 512], F32, "yb", ls)
                rr = k.sb([128, 512], F32, "rr", ls)
                og = k.sb([128, 512], BF16, "og", ls)
                gcol = k.sb([128, GWc], F32, "gcol", ls)
                trow = k.sb([1, cfg.GW], F32, "trow", ls)
                row_to_cols(gcol, I["ret_norm"][l:l + 1, :], cfg.GW, trow)
                k.dma("sp", dr_[:, :], I["ret_decay"][l:l + 1, :])
                k.act(dr_[:, :], dr_[:, :], AF.Exp, scale=-1.0)
                k.ts(dr_[:, :], dr_[:, :], 1.0, ALU.add)
                k.act(dr_[:, :], dr_[:, :], AF.Ln)
                p = k.ps()
                k.mm(p[:, 0:2 * RH], ones[0:1, :], dr_[0:1, :])
                k.ts(lgb_[:, :], p[:, 0:2 * RH], -1.0, ALU.mult)
                k.ts(nlg[:, :], lgb_[:, :], -1.0, ALU.mult)
                k.op("pool", lambda e: e.iota(d0i.t[:, :], [[1, 512]], base=0, channel_multiplier=-1), [], [d0i[:, :]])
                k.copy(D0[:, :], d0i[:, :])
                k.op("pool", lambda e: e.iota(ioi.t[:, :], [[128, NI]], base=0, channel_multiplier=0), [], [ioi[:, :]])
                k.copy(iof[:, :], ioi[:, :])
                lnk = math.log(ksc)
                qi = 0
                for h in range(RH):
                    lf, lb = lgb_[:, h:h + 1], lgb_[:, RH + h:RH + h + 1]
                    nlb = nlg[:, RH + h:RH + h + 1]
                    k.act(ctf[:, :], iof[:, :], AF.Exp, scale=lf)
                    k.act(ctb[:, :], iof[:, :], AF.Exp, scale=lb)
                    k.act(Ef[:, :], D0[:, :], AF.Exp, scale=lf, bias=lnk)
                    k.act(Eb[:, :], D0[:, :], AF.Exp, scale=nlb, bias=lnk)
                    for oi in range(4):
                        off = float(oi * 128)
                        k.ts(w1[:, :], D0[:, :], -off, ALU.add, 0.0, ALU.max)
                        k.act(w1[:, :], w1[:, :], AF.Exp, scale=lf, bias=lnk)
                        k.ts(w2[:, :], D0[:, :], -off, ALU.add, 0.0, ALU.min)
                        k.act(w2[:, :], w2[:, :], AF.Exp, scale=nlb, bias=lnk)
                        k.ts(w3[:, :], D0[:, :], -off, ALU.add, 0.0, ALU.is_ge)
                        k.tt(w1[:, :], w1[:, :], w2[:, :], ALU.subtract)
                        k.tt(w1[:, :], w1[:, :], w3[:, :], ALU.mult)
                        k.tt(Wd[oi][:, :], w1[:, :], w2[:, :], ALU.add)
                    rch, rro = h // 2, (h % 2) * 64
                    for bi, (t0, bw, isc) in enumerate(cfg.blocks):
                        k.dma("sp", kT[:, t0:t0 + bw], fmv("rk", rch, bi, 128, bw))
                    rv_, rvs = TMV["rv"]
                    k.dma("sp", vT[:, :, :], V(rv_.t[h], tuple(rvs[h])))
                    for bi, t0, bw, isc in q_blocks(need_ctx):
                        q, g = qT[qi % 2], gT[qi % 2]
                        qi += 1
                        k.dma("sp", q[:, 0:bw], fmv("rq", rch, bi, 128, bw))
                        k.dma("sp", g[:, 0:bw], fmv("rg", h, bi, 128, bw))
                        kts = key_tiles(isc)
                        po = k.ps(hold=True)
                        for i, kt in enumerate(kts):
                            p = k.ps()
                            k.mm(p[:, 0:bw], kT[rro:rro + 64, kt * 128:(kt + 1) * 128], q[rro:rro + 64, 0:bw])
                            pt = pT[i % 3]
                            kctx = kt >= N // 128
                            if kctx == isc:
                                s0 = (kt * 128 - N) if isc else kt * 128
                                tq = 0 if isc else t0
                                if s0 + 128 <= tq:
                                    ci_ = (tq - s0) // 128
                                    k.stt(pt[:, 0:bw], p[:, 0:bw], ctf[:, ci_:ci_ + 1], Ef[:, 0:bw], ALU.mult, ALU.mult)
                                elif s0 >= tq + bw:
                                    ci_ = (s0 - tq) // 128
                                    k.stt(pt[:, 0:bw], p[:, 0:bw], ctb[:, ci_:ci_ + 1], Eb[:, 0:bw], ALU.mult, ALU.mult)
                                else:
                                    k.tt(pt[:, 0:bw], p[:, 0:bw], Wd[(s0 - tq) // 128][:, 0:bw], ALU.mult)
                            else:
                                c0 = kt * 128 - N
                                cf = (t0 - (c0 - LC)) // 128
                                cb = (N + c0 - t0) // 128
                                k.ts(w1[:, 0:bw], Ef[:, 0:bw], ctf[:, cf:cf + 1], ALU.mult)
                                k.stt(w1[:, 0:bw], Eb[:, 0:bw], ctb[:, cb:cb + 1], w1[:, 0:bw], ALU.mult, ALU.add)
                                k.tt(pt[:, 0:bw], p[:, 0:bw], w1[:, 0:bw], ALU.mult)
                            k.mm(po[:, 0:bw], vT[:, kt, :], pt[:, 0:bw], start=(i == 0), stop=(i == len(kts) - 1))
                        k.copy(ya[:, 0:bw], po[:, 0:bw])
                        k.release(po)
                        k.act(yb[:, 0:bw], ya[:, 0:bw], AF.Square)
                        pss = k.ps()
                        k.mm(pss[:, 0:bw], ones[:, :], yb[:, 0:bw])
                        k.rstd(rr[:, 0:bw], pss[:, 0:bw], 128, yb[:, 0:bw])
                        k.stt(ya[:, 0:bw], ya[:, 0:bw], gcol[:, h:h + 1], rr[:, 0:bw], ALU.mult, ALU.mult)
                        k.tt(og[:, 0:bw], ya[:, 0:bw], g[:, 0:bw], ALU.mult)
                        k.dma("sp", fmv("mix", 3 * GWc + h, bi, 128, bw), og[:, 0:bw])
                        bg_step(2)

        s5g = fm_alloc(cfg.GW, "s5g")
        PI = math.pi

        def stage_s5(l, need_ctx):
            G = cfg.S5_G
            NP = G // 2
            lat_b = [(bi, t0, bw) for bi, (t0, bw, isc) in enumerate(cfg.blocks) if not isc]
            ctx_b = [(bi, t0, bw) for bi, (t0, bw, isc) in enumerate(cfg.blocks) if isc]
            with k.scope() as ls:
                trow = k.sb([1, max(G * 64, cfg.GW)], F32, "trow", ls)
                bc = k.sb([128, G], F32, "bc", ls)
                are = k.sb([128, 2, NP], F32, "are", ls)
                aim = k.sb([128, 2, NP], F32, "aim", ls)
                dtc = k.sb([128, 2, NP], F32, "dtc", ls)
                rr = k.sb([128, 2, NP], F32, "rr", ls)
                th = k.sb([128, 2, NP], F32, "th", ls)
                cr = k.sb([128, 2, NP], F32, "cr", ls)
                ci = k.sb([128, 2, NP], F32, "ci", ls)
                ncr = k.sb([128, 2, NP], F32, "ncr", ls)
                q1 = k.sb([128, 2, NP], F32, "q1", ls)
                q2 = k.sb([128, 2, NP], F32, "q2", ls)
                q3 = k.sb([128, 2, NP], F32, "q3", ls)
                q4 = k.sb([128, 2, NP], F32, "q4", ls)
                dcol = k.sb([32, NP], F32, "dcol", ls)
                jfi = k.sb([128, 512], mybir.dt.int32, "jfi", ls)
                jf = k.sb([128, 512], F32, "jf", ls)

                def sincos(out_s, out_c, ang, tmp, tmpi):
                    k.ts(tmp, ang, 1.0 / (2 * PI), ALU.mult)
                    k.copy(tmpi, tmp)
                    k.copy(tmp, tmpi)
                    k.stt(tmp, tmp, -2 * PI, ang, ALU.mult, ALU.add)
                    k.ts(out_c, tmp, PI, ALU.is_gt)
                    k.stt(tmp, out_c, -2 * PI, tmp, ALU.mult, ALU.add)
                    k.ts(out_c, tmp, -PI, ALU.is_lt)
                    k.stt(tmp, out_c, 2 * PI, tmp, ALU.mult, ALU.add)
                    k.act(out_s, tmp, AF.Sin)
                    k.ts(tmp, tmp, 0.5 * PI, ALU.add)
                    k.ts(out_c, tmp, PI, ALU.is_gt)
                    k.stt(tmp, out_c, -2 * PI, tmp, ALU.mult, ALU.add)
                    k.act(out_c, tmp, AF.Sin)

                negpi = k.sb([128, 1], F32, "negpi", ls)
                k.memset(negpi[:, :], -PI)
                k.op("pool", lambda e: e.iota(jfi.t[:, :], [[1, 512]], base=1, channel_multiplier=0), [], [jfi[:, :]])
                k.copy(jf[:, :], jfi[:, :])
                for dr in range(2):
                    row_to_cols(are[:, dr, :], I["s5_a_re"][l, dr:dr + 1, :], G * 64, trow)
                    row_to_cols(aim[:, dr, :], I["s5_a_im"][l, dr:dr + 1, :], G * 64, trow)
                    k.dma("sp", trow[0:1, 0:G], I["s5_log_dt"][l, dr:dr + 1, :])
                    k.act(trow[0:1, 0:G], trow[0:1, 0:G], AF.Exp)
                    p = k.ps()
                    k.mm(p[:, 0:G], ones[0:1, :], trow[0:1, 0:G])
                    k.copy(bc[:, :], p[:, 0:G])
                    bcv = bc[:, :].rearrange("p (n two) -> p n two", two=2)
                    k.copy(dtc[0:64, dr, :], bcv[0:64, :, 0])
                    k.copy(dtc[64:128, dr, :], bcv[64:128, :, 1])
                k.dma("sp", trow[0:1, 0:cfg.GW], I["s5_d"][l:l + 1, :])
                p = k.ps()
                for i in range(NP):
                    k.tr(p[0:32, i:i + 1], trow[0:1, i * 32:(i + 1) * 32], ident[0:1, 0:1])
                k.copy(dcol[:, :], p[0:32, 0:NP])
                fl = lambda b: b[:, :, :].rearrange("p a n -> p (a n)")
                k.tt(fl(q1), fl(are), fl(dtc), ALU.mult)
                k.act(fl(rr), fl(q1), AF.Exp)
                k.tt(fl(th), fl(aim), fl(dtc), ALU.mult)
                qi32 = k.sb([128, 2 * NP], mybir.dt.int32, "qi32", ls)
                sincos(fl(q1), fl(q2), fl(th), fl(q3), qi32[:, :])
                k.tt(fl(q1), fl(q1), fl(rr), ALU.mult)
                k.tt(fl(q2), fl(q2), fl(rr), ALU.mult)
                k.ts(fl(q2), fl(q2), -1.0, ALU.add)
                k.tt(fl(q3), fl(are), fl(are), ALU.mult)
                k.tt(fl(q4), fl(aim), fl(aim), ALU.mult)
                k.tt(fl(q3), fl(q3), fl(q4), ALU.add)
                k.recip(fl(q3), fl(q3))
                k.tt(fl(cr), fl(q2), fl(are), ALU.mult)
                k.tt(fl(q4), fl(q1), fl(aim), ALU.mult)
                k.tt(fl(cr), fl(cr), fl(q4), ALU.add)
                k.tt(fl(cr), fl(cr), fl(q3), ALU.mult)
                k.tt(fl(ci), fl(q1), fl(are), ALU.mult)
                k.tt(fl(q4), fl(q2), fl(aim), ALU.mult)
                k.tt(fl(ci), fl(ci), fl(q4), ALU.subtract)
                k.tt(fl(ci), fl(ci), fl(q3), ALU.mult)
                k.ts(fl(ncr), fl(cr), -1.0, ALU.mult)

                ang = k.sb([128, 512], F32, "ang", ls)
                atmp = k.sb([128, 512], F32, "atmp", ls)
                cosJ = k.sb([128, 512], F32, "cosJ", ls)
                sinJ = k.sb([128, 512], F32, "sinJ", ls)
                tre = k.sb([128, 512], F32, "tre", ls)
                tim = k.sb([128, 512], F32, "tim", ls)
                rJ = k.sb([128, 512], F32, "rJ", ls)
                z1 = k.sb([128, 512], F32, "z1", ls)
                z2 = k.sb([128, 512], F32, "z2", ls)
                zr = k.sb([128, 512], F32, "zr", ls)
                zi = k.sb([128, 512], F32, "zi", ls)
                xr = k.sb([128, 512], F32, "xr", ls)
                xi = k.sb([128, 512], F32, "xi", ls)
                zp = k.sb([128, 2], F32, "zp", ls)
                Bw = [k.sb([128, 32], F32, "Bw", ls) for _ in range(2)]
                Bl = [k.sb([32, 128], F32, "Bl", ls) for _ in range(2)]
                Cw = [k.sb([32, 128], F32, "Cw", ls) for _ in range(2)]
                Cl = [k.sb([128, 32], F32, "Cl", ls) for _ in range(2)]
                uf = k.sb([32, T], F32, "uf", ls)
                ya = k.sb([32, T], F32, "ya", ls)
                g1 = k.sb([32, T], F32, "g1", ls)
                gb = k.sb([32, T], BF16, "gb", ls)
                for bw_ in Bw:
                    k.memset(bw_[:, :], 0.0)
                for cw_ in Cw:
                    k.memset(cw_[:, :], 0.0)
                GC = math.sqrt(2.0 / math.pi) * 2.0
                for pk in range(NP):
                    ch, ro = (pk * 32) // 128, (pk * 32) % 128
                    for bi, (t0, bw, isc) in enumerate(cfg.blocks):
                        d_, subs_ = FM["su"]
                        k.dma("pool", uf[:, t0:t0 + bw], V(d_.t[ch, bi][ro:ro + 32, 0:bw], (subs_[ch][bi],)))
                    for dr in range(2):
                        for ri, nm in enumerate(("s5_b_re", "s5_b_im")):
                            src = I[nm]
                            k.dma("sp", Bw[ri][0:64, 0:16], src[l, dr, pk * 128:pk * 128 + 64, :])
                            k.dma("sp", Bw[ri][64:128, 16:32], src[l, dr, pk * 128 + 64:pk * 128 + 128, :])
                            p = k.ps()
                            k.tr(p[0:32, 0:128], Bw[ri][:, :], ident[:, :])
                            k.copy(Bl[ri][:, :], p[0:32, 0:128])
                        for ri, nm in enumerate(("s5_c_re", "s5_c_im")):
                            src = I[nm]
                            k.dma("sp", Cw[ri][0:16, 0:64], src[l, dr, pk * 32:pk * 32 + 16, :])
                            k.dma("sp", Cw[ri][16:32, 64:128], src[l, dr, pk * 32 + 16:pk * 32 + 32, :])
                            p = k.ps()
                            k.tr(p[:, 0:32], Cw[ri][:, :], ident[0:32, 0:32])
                            if ri == 0:
                                k.copy(Cl[ri][:, :], p[:, 0:32])
                            else:
                                k.ts(Cl[ri][:, :], p[:, 0:32], -1.0, ALU.mult)
                        k.ts(ang[:, :], jf[:, :], th[:, dr, pk:pk + 1], ALU.mult)
                        sincos(sinJ[:, :], cosJ[:, :], ang[:, :], atmp[:, :], jfi[:, :])
                        k.ts(tre[:, :], cosJ[:, :], cr[:, dr, pk:pk + 1], ALU.mult)
                        k.stt(tre[:, :], sinJ[:, :], ci[:, dr, pk:pk + 1], tre[:, :], ALU.mult, ALU.add)
                        k.ts(tim[:, :], cosJ[:, :], ci[:, dr, pk:pk + 1], ALU.mult)
                        k.stt(tim[:, :], sinJ[:, :], ncr[:, dr, pk:pk + 1], tim[:, :], ALU.mult, ALU.add)
                        k.memset(rJ[:, :], 1.0)
                        k.ts(rJ[:, :], rJ[:, :], rr[:, dr, pk:pk + 1], ALU.mult)
                        k.memset(zp[:, :], 0.0)
                        seq = (ctx_b + lat_b) if dr == 0 else (ctx_b[::-1] + lat_b[::-1])
                        for bi, t0, bw in seq:
                            isc = cfg.blocks[bi][2]
                            rv = (lambda v: v[:, ::-1]) if dr == 1 else (lambda v: v)
                            pbr = k.ps()
                            k.mm(pbr[:, 0:bw], Bl[0][:, :], uf[:, t0:t0 + bw])
                            pbi = k.ps()
                            k.mm(pbi[:, 0:bw], Bl[1][:, :], uf[:, t0:t0 + bw])
                            br_, bi_ = rv(pbr[:, 0:bw]), rv(pbi[:, 0:bw])
                            k.tt(z1[:, 0:bw], tre[:, 0:bw], br_, ALU.mult)
                            k.tt(z2[:, 0:bw], tim[:, 0:bw], bi_, ALU.mult)
                            k.tt(zr[:, 0:bw], z1[:, 0:bw], z2[:, 0:bw], ALU.subtract, eng="pool")
                            k.tt(z1[:, 0:bw], tre[:, 0:bw], bi_, ALU.mult)
                            k.tt(z2[:, 0:bw], tim[:, 0:bw], br_, ALU.mult)
                            k.tt(zi[:, 0:bw], z1[:, 0:bw], z2[:, 0:bw], ALU.add, eng="pool")
                            k.scan(zr[:, 0:bw], rJ[:, 0:bw], zr[:, 0:bw], zp[:, 0:1])
                            k.scan(zi[:, 0:bw], rJ[:, 0:bw], zi[:, 0:bw], zp[:, 1:2])
                            k.tt(z1[:, 0:bw], cosJ[:, 0:bw], zr[:, 0:bw], ALU.mult)
                            k.tt(z2[:, 0:bw], sinJ[:, 0:bw], zi[:, 0:bw], ALU.mult, eng="pool")
                            k.tt(xr[:, 0:bw], z1[:, 0:bw], z2[:, 0:bw], ALU.subtract)
                            k.tt(z1[:, 0:bw], sinJ[:, 0:bw], zr[:, 0:bw], ALU.mult, eng="pool")
                            k.tt(z2[:, 0:bw], cosJ[:, 0:bw], zi[:, 0:bw], ALU.mult)
                            k.tt(xi[:, 0:bw], z1[:, 0:bw], z2[:, 0:bw], ALU.add, eng="pool")
                            k.copy(zp[:, 0:1], xr[:, bw - 1:bw])
                            k.copy(zp[:, 1:2], xi[:, bw - 1:bw])
                            if isc and not need_ctx:
                                continue
                            py = k.ps()
                            k.mm(py[0:32, 0:bw], Cl[0][:, :], xr[:, 0:bw], start=True, stop=False)
                            k.mm(py[0:32, 0:bw], Cl[1][:, :], xi[:, 0:bw], start=False, stop=True)
                            if dr == 0:
                                k.stt(ya[:, t0:t0 + bw], uf[:, t0:t0 + bw], dcol[:, pk:pk + 1], py[0:32, 0:bw], ALU.mult, ALU.add)
                            else:
                                k.tt(ya[:, t0:t0 + bw], ya[:, t0:t0 + bw], py[0:32, 0:bw][:, ::-1], ALU.add)
                    tr_ = [(bi, t0, bw) for bi, (t0, bw, isc) in enumerate(cfg.blocks) if (need_ctx or not isc)]
                    t_lo, t_hi = tr_[0][1], tr_[-1][1] + tr_[-1][2]
                    yv = ya[:, t_lo:t_hi]
                    gv = g1[:, t_lo:t_hi]
                    k.tt(gv, yv, yv, ALU.mult)
                    k.ts(gv, gv, 0.044715, ALU.mult, 1.0, ALU.add)
                    k.tt(gv, gv, yv, ALU.mult, eng="pool")
                    k.act(gv, gv, AF.Sigmoid, scale=GC)
                    k.tt(gb[:, t_lo:t_hi], gv, yv, ALU.mult)
                    for bi, t0, bw in tr_:
                        k.dma("sp", V(s5g[0].t[ch, bi][ro:ro + 32, 0:bw], (s5g[1][ch][bi],)), gb[:, t0:t0 + bw])
            with k.scope() as ls:
                gw = k.sb([128, GWc, cfg.GW], BF16, "gw", ls)
                k.dma("pool", gw[:, :, :], V(I["s5_glu_w"].t[l].rearrange("(c p) n -> p c n", p=128), (I["s5_glu_w"],)))
                bcol = k.sb([128, GWc], F32, "bcol", ls)
                trow = k.sb([1, cfg.GW], F32, "trow", ls)
                row_to_cols(bcol, I["s5_glu_b"][l:l + 1, :], cfg.GW, trow)
                gin = [k.sb([128, GWc, 512], BF16, "gin", ls) for _ in range(2)]
                gt = [k.sb([128, 512], F32, "gt", ls) for _ in range(2)]
                og = [k.sb([128, 512], BF16, "og", ls) for _ in range(2)]
                n = 0
                for bi, t0, bw, isc in q_blocks(need_ctx):
                    gi = gin[bi % 2]
                    for c in range(GWc):
                        k.dma("sp", gi[:, c, 0:bw], V(s5g[0].t[c, bi][:, 0:bw], (s5g[1][c][bi],)))
                    for oc in range(GWc):
                        p = k.ps()
                        for c in range(GWc):
                            k.mm(p[:, 0:bw], gw[:, c, oc * 128:(oc + 1) * 128], gi[:, c, 0:bw], start=(c == 0), stop=(c == GWc - 1))
                        g_, o_ = gt[n % 2], og[n % 2]
                        n += 1
                        k.act(g_[:, 0:bw], p[:, 0:bw], AF.Sigmoid, bias=bcol[:, oc:oc + 1])
                        k.tt(o_[:, 0:bw], g_[:, 0:bw], gi[:, oc, 0:bw], ALU.mult)
                        k.dma("sp", fmv("mix", GWc + oc, bi, 128, bw), o_[:, 0:bw])

        def xv(ti):
            return V(xres.t[ti * 128:(ti + 1) * 128, :], (xres_t[ti],))

        def stage_wout(l, blocks):
            wv = I["w_out"].t[l].rearrange("(c p) n -> p c n", p=128)
            with k.scope() as ls:
                mb = k.sb([128, KC, 512], BF16, "mb", ls)
                xt = [k.sb([128, D], F32, "xt", ls) for _ in range(4)]
                wt = [k.sb([128, KC, 256], BF16, "wt", ls) for _ in range(3)]
                G1 = k.sb([128, D], F32, "G1", ls)
                tmp = [k.sb([128, 256], F32, "tmp", ls) for _ in range(2)]
                cur_r = None
                wi = 0
                n = 0
                for bi in blocks:
                    t0, bw, isc = cfg.blocks[bi]
                    r = 1 if isc else 0
                    if r != cur_r:
                        k.dma("sp", G1[:, :], modbc[l][r][2][:, :])
                        cur_r = r
                    for c in range(KC):
                        k.dma("sp", mb[:, c, 0:bw], fmv("mix", c, bi, 128, bw))
                    ns = bw // 128
                    for sub in range(ns):
                        k.dma("sp", xt[sub][:, :], xv(t0 // 128 + sub))
                    for ct in range(D // 256):
                        w = wt[wi % 3]
                        wi += 1
                        k.dma("sp" if wi % 2 else "pool", w[:, :, :], V(woutb[l][0].t[ct], (woutb[l][1][ct],)))
                        for sub in range(ns):
                            p = k.ps()
                            for c in range(KC):
                                k.mm(p[:, 0:256], mb[:, c, sub * 128:(sub + 1) * 128], w[:, c, :], start=(c == 0), stop=(c == KC - 1))
                            tm = tmp[n % 2]
                            n += 1
                            k.tt(tm[:, :], p[:, 0:256], G1[:, ct * 256:(ct + 1) * 256], ALU.mult)
                            k.tt(xt[sub][:, ct * 256:(ct + 1) * 256], xt[sub][:, ct * 256:(ct + 1) * 256], tm[:, :], ALU.add, eng="pool")
                    for sub in range(ns):
                        k.dma("sp", xv(t0 // 128 + sub), xt[sub][:, :])

        def stage_moe(l, blocks):
            FC = cfg.FF // 128
            with k.scope() as ls:
                hb = k.sb([128, KC, 512], BF16, "hb", ls)
                acc = [k.sb([128, D], F32, "acc", ls) for _ in range(4)]
                wt = [k.sb([128, KC, 256], BF16, "wt", ls) for _ in range(4)]
                hid = k.sb([128, FC, 512], BF16, "hid", ls)
                wd = [k.sb([128, FC, 512], BF16, "wd", ls) for _ in range(3)]
                HC = min(1024, D)
                G2 = k.sb([128, HC], F32, "G2", ls)
                xh = k.sb([128, HC], F32, "xh", ls)
                sl = [k.sb([128, 512], F32, "sl", ls) for _ in range(2)]
                dws = k.sb([128, 4, 16], F32, "dws", ls)
                wi = 0
                di = 0
                n = 0
                for bi in blocks:
                    t0, bw, isc = cfg.blocks[bi]
                    r = 1 if isc else 0
                    ns = bw // 128
                    k.dma("sp", hb[:, :, 0:bw], V(hT.t[bi][:, :, 0:bw], (hT_b[bi],)))
                    for sub in range(ns):
                        ti = t0 // 128 + sub
                        k.dma("sp", dws[:, sub, :], V(dwd.t[ti * 128:(ti + 1) * 128, :], (dwd_t[ti],)))
                    for e in range(16):
                        bg_step(3)
                        wgv = I["moe_w_gate"].t[l, e].rearrange("(c p) f -> p c f", p=128)
                        wuv = I["moe_w_up"].t[l, e].rearrange("(c p) f -> p c f", p=128)
                        wdv = I["moe_w_down"].t[l, e].rearrange("(c p) n -> p c n", p=128)
                        for f0 in range(0, cfg.FF, 256):
                            fw = min(256, cfg.FF - f0)
                            wg_ = wt[wi % 4]
                            wu_ = wt[(wi + 1) % 4]
                            wi += 2
                            jt = f0 // 256
                            k.dma("sp", wg_[:, :, 0:fw], V(wgb[l][0].t[e, jt][:, :, 0:fw], (wgb[l][1][e][jt],)))
                            k.dma("sp", wu_[:, :, 0:fw], V(wub[l][0].t[e, jt][:, :, 0:fw], (wub[l][1][e][jt],)))
                            for j0 in range(0, fw, 128):
                                fc = (f0 + j0) // 128
                                pg = k.ps()
                                for c in range(KC):
                                    k.mm(pg[:, 0:bw], wg_[:, c, j0:j0 + 128], hb[:, c, 0:bw], start=(c == 0), stop=(c == KC - 1))
                                pu = k.ps()
                                for c in range(KC):
                                    k.mm(pu[:, 0:bw], wu_[:, c, j0:j0 + 128], hb[:, c, 0:bw], start=(c == 0), stop=(c == KC - 1))
                                s_ = sl[n % 2]
                                n += 1
                                k.act(s_[:, 0:bw], pg[:, 0:bw], AF.Silu)
                                k.tt(hid[:, fc, 0:bw], pu[:, 0:bw], s_[:, 0:bw], ALU.mult)
                        for ct in range(D // 512):
                            w = wd[di % 3]
                            di += 1
                            k.dma("sp", w[:, :, :], V(wdb[l][0].t[e, ct], (wdb[l][1][e][ct],)))
                            for sub in range(ns):
                                p = k.ps()
                                for fc in range(FC):
                                    k.mm(p[:, :], hid[:, fc, sub * 128:(sub + 1) * 128], w[:, fc, :], start=(fc == 0), stop=(fc == FC - 1))
                                av = acc[sub][:, ct * 512:(ct + 1) * 512]
                                if e == 0:
                                    k.ts(av, p[:, :], dws[:, sub, e:e + 1], ALU.mult)
                                else:
                                    k.stt(av, p[:, :], dws[:, sub, e:e + 1], av, ALU.mult, ALU.add)
                    for hc in range(0, D, HC):
                        k.dma("sp", G2[:, :], modbc[l][r][5][:, hc:hc + HC])
                        for sub in range(ns):
                            ti = t0 // 128 + sub
                            k.dma("sp", xh[:, :], V(xres.t[ti * 128:(ti + 1) * 128, hc:hc + HC], (xres_t[ti],)))
                            k.tt(acc[sub][:, hc:hc + HC], acc[sub][:, hc:hc + HC], G2[:, :], ALU.mult, eng="pool")
                            k.tt(xh[:, :], xh[:, :], acc[sub][:, hc:hc + HC], ALU.add)
                            k.dma("sp", V(xres.t[ti * 128:(ti + 1) * 128, hc:hc + HC], (xres_t[ti],)), xh[:, :])

        def stage_final():
            with k.scope() as ls:
                gb_ = k.sb([128, D], F32, "gfin", ls)
                grow = k.sb([1, D], F32, "grow", ls)
                xt = [k.sb([128, D], F32, "xt", ls) for _ in range(2)]
                junk = k.sb([128, D], BF16, "junk", ls)
                sm = k.sb([128, 4], F32, "sm", ls)
                k.dma("sp", grow[:, :], I["final_norm"][0:1, :])
                for ct in range(D // 512):
                    p = k.ps()
                    k.mm(p[:, :], ones[0:1, :], grow[0:1, ct * 512:(ct + 1) * 512])
                    k.copy(gb_[:, ct * 512:(ct + 1) * 512], p[:, :])
                for ti in range(N // 128):
                    x = xt[ti % 2]
                    k.dma("sp", x[:, :], xv(ti))
                    k.act(junk[:, :], x[:, :], AF.Square, accum=sm[:, 0:1])
                    k.rstd(sm[:, 1:2], sm[:, 0:1], D, sm[:, 2:3])
                    k.stt(x[:, :], x[:, :], sm[:, 1:2], gb_[:, :], ALU.mult, ALU.mult)
                    k.dma("sp", OUT[ti * 128:(ti + 1) * 128, :], x[:, :])

        def run_all():
            for fn in conv_list(0, True)[:len(WIN_T)]:
                fn()
            stage_ada()
            bgq.extend(conv_list(0, False))
            allb = list(range(cfg.NTB))
            for l in range(L):
                need_ctx = l < L - 1
                ob = [bi for bi in allb if (need_ctx or not cfg.blocks[bi][2])]
                stage_norm(l, 0, allb, False)
                stage_proj(l)
                stage_diff(l, need_ctx)
                stage_s5(l, need_ctx)
                stage_mla(l, need_ctx)
                stage_ret(l, need_ctx)
                bg_flush()
                stage_wout(l, ob)
                stage_norm(l, 1, ob, True)
                if l + 1 < L:
                    bgq.extend(conv_list(l + 1, True))
                stage_moe(l, ob)
                bg_flush()
            stage_final()

        def dbg_out(name, d, subs, dt):
            shape = list(d.t.shape)
            o = Buf(nc.dram_tensor("o_" + name, shape, dt, kind="ExternalOutput"), "o_" + name)
            idx = tuple(slice(None) for _ in shape)
            k.dma("sp", V(o.t[idx], (o,)), V(d.t[idx], tuple(subs)))
            return o

        outs = [OUT]
        run_all()
        k.finish(outs)
    return nc


def host_consts(cfg):
    ident = np.eye(128, dtype=np.float32)
    R = np.zeros((64, 64), np.float32)
    for i in range(16):
        R[i + 16, i] = -1.0
        R[i, i + 16] = 1.0
        R[i + 48, i + 32] = -1.0
        R[i + 32, i + 48] = 1.0
    rp = np.zeros((128, 128), np.float32)
    rp[:64, :64] = R
    rp[64:, 64:] = R
    rows = cfg.N // 64
    row = np.repeat(np.arange(rows, dtype=np.float32), 64)
    col = np.tile(np.arange(64, dtype=np.float32), rows)
    inv = (10000.0 ** (-np.arange(16, dtype=np.float32) / 16)).astype(np.float32)
    ar = row[:, None] * inv
    ac = col[:, None] * inv
    ang = np.concatenate([ar, ar, ac, ac], axis=-1)
    cs = np.stack([np.cos(ang).T, np.sin(ang).T]).astype(np.float32)
    cs = np.concatenate([cs, cs], axis=1)
    return {"ident": ident, "rperm": rp, "ropecs": np.ascontiguousarray(cs)}


def make_in_maps(cfg, inp, ncores):
    L = cfg.DEPTH
    f = lambda a: np.ascontiguousarray(np.asarray(a, dtype=np.float32))
    shared = {
        "c_ctx": f(inp["c_ctx"]).reshape(cfg.KC, 128),
        "ada_w": f(inp["ada_w"]), "ada_b": f(inp["ada_b"]),
        "norm_mix": f(inp["norm_mix"]), "norm_ffn": f(inp["norm_ffn"]),
        "w_in": f(inp["w_in"]), "w_out": f(inp["w_out"]),
        "diff_lambda": f(inp["diff_lambda"]).reshape(L, 256), "diff_subln": f(inp["diff_subln"]),
        "s5_a_re": f(inp["s5_a_re"]).reshape(L, 2, -1), "s5_a_im": f(inp["s5_a_im"]).reshape(L, 2, -1),
        "s5_log_dt": f(inp["s5_log_dt"]),
        "s5_b_re": f(inp["s5_b_re"]).reshape(L, 2, -1, 16), "s5_b_im": f(inp["s5_b_im"]).reshape(L, 2, -1, 16),
        "s5_c_re": f(inp["s5_c_re"]).reshape(L, 2, -1, 64), "s5_c_im": f(inp["s5_c_im"]).reshape(L, 2, -1, 64),
        "s5_d": f(inp["s5_d"]).reshape(L, -1), "s5_glu_w": f(inp["s5_glu_w"]), "s5_glu_b": f(inp["s5_glu_b"]),
        "mla_q_norm": f(inp["mla_q_norm"]), "mla_kv_norm": f(inp["mla_kv_norm"]),
        "mla_w_uq": f(inp["mla_w_uq"]), "mla_w_ukv": f(inp["mla_w_ukv"]),
        "ret_decay": f(inp["ret_decay"]).reshape(L, -1), "ret_norm": f(inp["ret_norm"]),
        "moe_wr": np.ascontiguousarray(np.concatenate([f(inp["moe_wg"]), f(inp["moe_we"])], axis=-1)),
        "moe_br": np.ascontiguousarray(np.concatenate([f(inp["moe_bg"]), f(inp["moe_be"])], axis=-1)),
        "moe_w_gate": f(inp["moe_w_gate"]), "moe_w_up": f(inp["moe_w_up"]), "moe_w_down": f(inp["moe_w_down"]),
        "final_norm": f(inp["final_norm"]).reshape(1, -1),
    }
    shared.update(host_consts(cfg))
    maps = []
    for b in range(ncores):
        m = dict(shared)
        m["x"] = f(inp["x"][b])
        m["ctx"] = f(inp["ctx"][b])
        m["c"] = f(inp["c"][b]).reshape(cfg.KC, 128)
        maps.append(m)
    return maps


def kernel(**inputs):
    x = np.asarray(inputs["x"])
    B, N, D = x.shape
    cfg = Cfg(D=D, N=N, LC=np.asarray(inputs["ctx"]).shape[1], DEPTH=np.asarray(inputs["ada_w"]).shape[0], B=B)
    nc = build_program(cfg)
    maps = make_in_maps(cfg, inputs, B)
    res = run_bass_kernel_spmd(nc, maps, core_ids=list(range(B)))
    return np.stack([np.asarray(r["out"]) for r in res.results]).astype(np.float32)
```
